# Optimizing a Trainium2 kernel written in Bass

```python
import math, functools
import jax, jax.numpy as jnp
from jax import lax
import numpy as np

D_MODEL = 1024
BATCH = 8
SEQ = 4096
DEPTH = 4

GRID_W = 64
CTX_LEN = 256
HEAD_DIM = 64
N_Q_HEADS = 8
N_KV_HEADS = 2
GQA_GROUP = N_Q_HEADS // N_KV_HEADS
D_ATTN = N_Q_HEADS * HEAD_DIM
KV_W = N_KV_HEADS * HEAD_DIM
Q_BLOCK = 128
ATTN_SCALE = HEAD_DIM ** -0.5
ROPE_THETA = 10000.0
ROPE_AXIS_DIM = HEAD_DIM // 2
QK_EPS = 1e-6
D_HYENA = 256
HYENA_ORDER = 2
HYENA_BANDS = 16
HYENA_EMB = 1 + 2 * HYENA_BANDS
HYENA_FILTER_HIDDEN = 64
HYENA_DECAY_TARGET = 1e-2
HYENA_FAST_DECAY_PCT = 0.3
HYENA_SLOW_DECAY_PCT = 1.5
D_POOL = 256
POOL_WINDOWS = (2, 4, 8, 16)
POOL_GROUP = D_POOL // len(POOL_WINDOWS)
Q_END = D_ATTN
K_END = Q_END + KV_W
V_END = K_END + KV_W
HY_END = V_END + 3 * D_HYENA
D_IN = HY_END + D_POOL
D_MIX = D_ATTN + D_HYENA + D_POOL
D_FF = 2816
N_EXPERTS = 8
TOP_K = 2
D_FF_EXPERT = 3584
N_DENSE = (DEPTH + 1) // 2
N_MOE = DEPTH // 2
DEEPNORM_ALPHA = (2.0 * DEPTH) ** 0.25
DEEPNORM_BETA = (8.0 * DEPTH) ** -0.25
LN_EPS = 1e-5
ADALN_EPS = 1e-6

kernel_name = 'hybrid_attn_hyena_pool_moe_dit'


def ln_plain(h, eps=ADALN_EPS):
    hf = h.astype(jnp.float32)
    mu = jnp.mean(hf, -1, keepdims=True)
    var = jnp.mean(jnp.square(hf - mu), -1, keepdims=True)
    return ((hf - mu) * lax.rsqrt(var + eps)).astype(h.dtype)


def ln_affine(h, g, b):
    hf = h.astype(jnp.float32)
    mu = jnp.mean(hf, -1, keepdims=True)
    var = jnp.mean(jnp.square(hf - mu), -1, keepdims=True)
    y = (hf - mu) * lax.rsqrt(var + LN_EPS) * g.astype(jnp.float32) + b.astype(jnp.float32)
    return y.astype(h.dtype)


def rms_norm(h, g):
    hf = h.astype(jnp.float32)
    y = hf * lax.rsqrt(jnp.mean(jnp.square(hf), -1, keepdims=True) + QK_EPS) * g.astype(jnp.float32)
    return y.astype(h.dtype)


def adaln(cond, w, b):
    return jnp.split(jax.nn.silu(cond) @ w + b, 6, axis=-1)


def modulate(h, shift, scale):
    return ln_plain(h) * (1.0 + scale[:, None]) + shift[:, None]


def deepnorm_update(h, gate, f, g, b):
    return ln_affine(DEEPNORM_ALPHA * h + gate[:, None] * f, g, b)


def axial_rope_tables(L, dtype):
    rows = L // GRID_W
    row = jnp.repeat(jnp.arange(rows), GRID_W).astype(jnp.float32)
    col = jnp.tile(jnp.arange(GRID_W), rows).astype(jnp.float32)
    inv = ROPE_THETA ** (-jnp.arange(0, ROPE_AXIS_DIM, 2, dtype=jnp.float32) / ROPE_AXIS_DIM)
    ang = jnp.concatenate([row[:, None] * inv, col[:, None] * inv], axis=-1)
    return jnp.cos(ang).astype(dtype), jnp.sin(ang).astype(dtype)


def apply_axial_rope(h, cos, sin):
    half = ROPE_AXIS_DIM // 2
    bshape = (h.shape[1],) + (1,) * (h.ndim - 3) + (2, half)
    cos = cos.reshape(bshape)
    sin = sin.reshape(bshape)
    hr = h.reshape(h.shape[:-1] + (2, 2, half))
    a, b = hr[..., 0, :], hr[..., 1, :]
    return jnp.stack([a * cos - b * sin, b * cos + a * sin], axis=-2).reshape(h.shape)


def head_groups(proj, q_gain, k_gain):
    B, L, _ = proj.shape
    q = proj[..., :Q_END].reshape(B, L, N_KV_HEADS, GQA_GROUP, HEAD_DIM)
    k = proj[..., Q_END:K_END].reshape(B, L, N_KV_HEADS, HEAD_DIM)
    v = proj[..., K_END:V_END].reshape(B, L, N_KV_HEADS, HEAD_DIM)
    return (rms_norm(q, q_gain), rms_norm(k, k_gain), v, proj[..., V_END:HY_END], proj[..., HY_END:])


def attend(q, keys, vals):
    s = jnp.einsum('bqhgd,bkhd->bhgqk', q, keys, preferred_element_type=jnp.float32) * ATTN_SCALE
    p = jax.nn.softmax(s, axis=-1).astype(vals.dtype)
    return jnp.einsum('bhgqk,bkhd->bqhgd', p, vals)


def latent_attention(q, k, v, k_ctx, v_ctx):
    B, S = q.shape[:2]
    keys = jnp.concatenate([k, k_ctx], axis=1)
    vals = jnp.concatenate([v, v_ctx], axis=1)
    qb = q.reshape((B, S // Q_BLOCK, Q_BLOCK) + q.shape[2:]).swapaxes(0, 1)
    o = lax.map(lambda qi: attend(qi, keys, vals), qb)
    return o.swapaxes(0, 1).reshape(B, S, D_ATTN)


def short_conv(z, w, b):
    zp = jnp.pad(z, ((0, 0), (1, 1), (0, 0)))
    return zp[:, :-2] * w[0] + zp[:, 1:-1] * w[1] + zp[:, 2:] * w[2] + b


def hyena_filter_spectra(L, w1, b1, freq, w2, b2, w3):
    f32 = jnp.float32
    t = jnp.linspace(0.0, 1.0, L, dtype=f32)[:, None]
    omega = 2.0 * math.pi * jnp.arange(L, dtype=f32)[:, None] / L
    bands = jnp.linspace(1e-4, HYENA_BANDS - 1, HYENA_BANDS, dtype=f32)[None, :]
    feats = jnp.concatenate([t, jnp.cos(omega * bands), -jnp.sin(omega * bands)], axis=-1)
    fr = freq.astype(f32)
    h = jnp.sin(fr * (feats @ w1.astype(f32) + b1.astype(f32)))
    h = jnp.sin(fr * (h @ w2.astype(f32) + b2.astype(f32)))
    h = (h @ w3.astype(f32)).reshape(L, HYENA_ORDER, 2, D_HYENA)
    max_decay = math.log(HYENA_DECAY_TARGET) / HYENA_FAST_DECAY_PCT
    min_decay = math.log(HYENA_DECAY_TARGET) / HYENA_SLOW_DECAY_PCT
    deltas = jnp.linspace(min_decay, max_decay, D_HYENA, dtype=f32)
    h = h * jnp.exp(-t[:, :, None, None] * jnp.abs(deltas))
    h = h / jnp.sum(jnp.abs(h), axis=(0, 2), keepdims=True)
    fwd, bwd = h[:, :, 0], h[:, :, 1]
    buf = jnp.concatenate([fwd, jnp.zeros((1,) + fwd.shape[1:], f32), bwd[:0:-1]], axis=0)
    return jnp.fft.rfft(buf, axis=0)


def long_conv(z, spec):
    L = z.shape[1]
    Z = jnp.fft.rfft(z, n=2 * L, axis=1)
    return jnp.fft.irfft(Z * spec[None], n=2 * L, axis=1)[:, :L]


def hyena_mixer(z_in, conv_w, conv_b, spec, d):
    zc = short_conv(z_in, conv_w, conv_b).astype(jnp.float32)
    v, g1, g2 = jnp.split(zc, 3, axis=-1)
    df = d.astype(jnp.float32)
    z1 = g1 * (long_conv(v, spec[:, 0]) + df[0] * v)
    y = g2 * (long_conv(z1, spec[:, 1]) + df[1] * z1)
    return y.astype(z_in.dtype)


def pool_mixer(p, w, scale):
    B, L, _ = p.shape
    pf = p.astype(jnp.float32)
    cs = jnp.pad(jnp.cumsum(pf, axis=1), ((0, 0), (1, 0), (0, 0)))
    t = jnp.arange(L)
    outs = []
    for g, win in enumerate(POOL_WINDOWS):
        lo = jnp.clip(t - win // 2, 0, L)
        hi = jnp.clip(t + win - win // 2, 0, L)
        sl = slice(g * POOL_GROUP, (g + 1) * POOL_GROUP)
        mean = (cs[:, hi, sl] - cs[:, lo, sl]) / (hi - lo).astype(jnp.float32)[:, None]
        outs.append(mean - pf[:, :, sl])
    y = jnp.stack(outs, axis=2)
    y = jnp.einsum('blgc,gcd->blgd', y, w.astype(jnp.float32)).reshape(B, L, D_POOL) * scale.astype(jnp.float32)
    return y.astype(p.dtype)


def swiglu(u, w_gate, w_up, w_down):
    return (jax.nn.silu(u @ w_gate) * (u @ w_up)) @ w_down


def moe_swiglu(u, router_w, w_gate, w_up, w_down):
    shp = u.shape
    tok = u.reshape(-1, shp[-1])
    logits = jnp.dot(tok, router_w, preferred_element_type=jnp.float32)
    top_v, top_i = lax.top_k(logits, TOP_K)
    gates = jax.nn.softmax(top_v, axis=-1)
    comb = jnp.sum(jax.nn.one_hot(top_i, N_EXPERTS, dtype=jnp.float32) * gates[..., None], axis=1)
    y = jnp.zeros(tok.shape, jnp.float32)
    for e in range(N_EXPERTS):
        y = y + comb[:, e:e + 1] * swiglu(tok, w_gate[e], w_up[e], w_down[e]).astype(jnp.float32)
    return y.astype(u.dtype).reshape(shp)


def setup_inputs(seed: int = 0) -> dict:
    key = jax.random.key(seed)
    ks = jax.random.split(key, 32)
    f32 = jnp.float32
    D = D_MODEL

    def nrm(k, shape, scale):
        return jax.random.normal(k, shape, f32) * scale

    def gain(k, shape):
        return 1.0 + nrm(k, shape, 0.02)

    return {
        'x': nrm(ks[0], (BATCH, SEQ, D), 1.0),
        'c': nrm(ks[1], (BATCH, D), 1.0),
        'ctx': nrm(ks[2], (BATCH, CTX_LEN, D), 1.0),
        'c_ctx': nrm(ks[3], (D,), 1.0),
        'w_mod': nrm(ks[4], (DEPTH, D, 6 * D), 0.5 * D ** -0.5),
        'b_mod': nrm(ks[5], (DEPTH, 6 * D), 0.02),
        'w_in': nrm(ks[6], (DEPTH, D, D_IN), D ** -0.5),
        'q_gain': gain(ks[7], (DEPTH, HEAD_DIM)),
        'k_gain': gain(ks[8], (DEPTH, HEAD_DIM)),
        'hy_conv_w': nrm(ks[9], (DEPTH, 3, 3 * D_HYENA), 3 ** -0.5),
        'hy_conv_b': nrm(ks[10], (DEPTH, 3 * D_HYENA), 0.02),
        'hy_f_w1': nrm(ks[11], (DEPTH, HYENA_EMB, HYENA_FILTER_HIDDEN), HYENA_EMB ** -0.5),
        'hy_f_b1': nrm(ks[12], (DEPTH, HYENA_FILTER_HIDDEN), 0.1),
        'hy_f_freq': gain(ks[13], (DEPTH, HYENA_FILTER_HIDDEN)),
        'hy_f_w2': nrm(ks[14], (DEPTH, HYENA_FILTER_HIDDEN, HYENA_FILTER_HIDDEN), HYENA_FILTER_HIDDEN ** -0.5),
        'hy_f_b2': nrm(ks[15], (DEPTH, HYENA_FILTER_HIDDEN), 0.1),
        'hy_f_w3': nrm(ks[16], (DEPTH, HYENA_FILTER_HIDDEN, HYENA_ORDER * 2 * D_HYENA), HYENA_FILTER_HIDDEN ** -0.5),
        'hy_d': nrm(ks[17], (DEPTH, HYENA_ORDER, D_HYENA), 1.0),
        'pool_w': nrm(ks[18], (DEPTH, len(POOL_WINDOWS), POOL_GROUP, POOL_GROUP), POOL_GROUP ** -0.5),
        'pool_scale': gain(ks[19], (DEPTH, D_POOL)),
        'w_out': nrm(ks[20], (DEPTH, D_MIX, D), DEEPNORM_BETA * D_MIX ** -0.5),
        'ln1_g': gain(ks[21], (DEPTH, D)),
        'ln1_b': nrm(ks[22], (DEPTH, D), 0.02),
        'ln2_g': gain(ks[23], (DEPTH, D)),
        'ln2_b': nrm(ks[24], (DEPTH, D), 0.02),
        'ffn_w_gate': nrm(ks[25], (N_DENSE, D, D_FF), D ** -0.5),
        'ffn_w_up': nrm(ks[26], (N_DENSE, D, D_FF), D ** -0.5),
        'ffn_w_down': nrm(ks[27], (N_DENSE, D_FF, D), DEEPNORM_BETA * D_FF ** -0.5),
        'router_w': nrm(ks[28], (N_MOE, D, N_EXPERTS), D ** -0.5),
        'moe_w_gate': nrm(ks[29], (N_MOE, N_EXPERTS, D, D_FF_EXPERT), D ** -0.5),
        'moe_w_up': nrm(ks[30], (N_MOE, N_EXPERTS, D, D_FF_EXPERT), D ** -0.5),
        'moe_w_down': nrm(ks[31], (N_MOE, N_EXPERTS, D_FF_EXPERT, D), DEEPNORM_BETA * D_FF_EXPERT ** -0.5),
    }


def reference(x, c, ctx, c_ctx, w_mod, b_mod, w_in, q_gain, k_gain, hy_conv_w, hy_conv_b, hy_f_w1, hy_f_b1, hy_f_freq, hy_f_w2, hy_f_b2, hy_f_w3, hy_d, pool_w, pool_scale, w_out, ln1_g, ln1_b, ln2_g, ln2_b, ffn_w_gate, ffn_w_up, ffn_w_down, router_w, moe_w_gate, moe_w_up, moe_w_down):
    B, S, _ = x.shape
    C = ctx.shape[1]
    cos, sin = axial_rope_tables(S, x.dtype)
    h_ctx = ctx
    for l in range(DEPTH):
        last = l == DEPTH - 1
        m = adaln(c, w_mod[l], b_mod[l])
        mc = adaln(c_ctx[None, :], w_mod[l], b_mod[l])
        filt = (hy_f_w1[l], hy_f_b1[l], hy_f_freq[l], hy_f_w2[l], hy_f_b2[l], hy_f_w3[l])
        if l % 2 == 0:
            j = l // 2
            ffn = functools.partial(swiglu, w_gate=ffn_w_gate[j], w_up=ffn_w_up[j], w_down=ffn_w_down[j])
        else:
            j = l // 2
            ffn = functools.partial(moe_swiglu, router_w=router_w[j], w_gate=moe_w_gate[j], w_up=moe_w_up[j], w_down=moe_w_down[j])

        uc = modulate(h_ctx, mc[0], mc[1])
        if last:
            kv = uc @ w_in[l][:, Q_END:V_END]
            kc = rms_norm(kv[..., :KV_W].reshape(B, C, N_KV_HEADS, HEAD_DIM), k_gain[l])
            vc = kv[..., KV_W:].reshape(B, C, N_KV_HEADS, HEAD_DIM)
        else:
            qc, kc, vc, hyc, plc = head_groups(uc @ w_in[l], q_gain[l], k_gain[l])
            att_c = attend(qc, kc, vc).reshape(B, C, D_ATTN)
            hyo_c = hyena_mixer(hyc, hy_conv_w[l], hy_conv_b[l], hyena_filter_spectra(C, *filt), hy_d[l])
            plo_c = pool_mixer(plc, pool_w[l], pool_scale[l])
            mix_c = jnp.concatenate([att_c, hyo_c, plo_c], axis=-1) @ w_out[l]

        u = modulate(x, m[0], m[1])
        q, k, v, hy, pl = head_groups(u @ w_in[l], q_gain[l], k_gain[l])
        q = apply_axial_rope(q, cos, sin)
        k = apply_axial_rope(k, cos, sin)
        att = latent_attention(q, k, v, kc, vc)
        hyo = hyena_mixer(hy, hy_conv_w[l], hy_conv_b[l], hyena_filter_spectra(S, *filt), hy_d[l])
        plo = pool_mixer(pl, pool_w[l], pool_scale[l])
        mix = jnp.concatenate([att, hyo, plo], axis=-1) @ w_out[l]
        x = deepnorm_update(x, m[2], mix, ln1_g[l], ln1_b[l])
        x = deepnorm_update(x, m[5], ffn(modulate(x, m[3], m[4])), ln2_g[l], ln2_b[l])

        if not last:
            h_ctx = deepnorm_update(h_ctx, mc[2], mix_c, ln1_g[l], ln1_b[l])
            h_ctx = deepnorm_update(h_ctx, mc[5], ffn(modulate(h_ctx, mc[3], mc[4])), ln2_g[l], ln2_b[l])
    return x
```

```python
import math
from contextlib import ExitStack
import numpy as np
import ml_dtypes
import concourse.bass as bass
import concourse.mybir as mybir
from concourse.bass_utils import run_bass_kernel_spmd

F32 = mybir.dt.float32
BF16 = mybir.dt.bfloat16
AF = mybir.ActivationFunctionType
ALU = mybir.AluOpType
AX = mybir.AxisListType
NPBF = ml_dtypes.bfloat16

PE, ACT, DVE, POOL, SP = "pe", "act", "dve", "pool", "sp"
ENGINES = (PE, ACT, DVE, POOL, SP)

D = 1024
S = 4096
CTX = 256
TOK = S + CTX
NT = TOK // 128
DEPTH = 4
D_IN = 1792
D_FF = 2816
D_FFE = 3584
NEXP = 8
ALPHA = (2.0 * DEPTH) ** 0.25
LN_EPS = 1e-5
ADA_EPS = 1e-6
QK_EPS = 1e-6
BLOCKS = [(i * 512, 4) for i in range(8)] + [(4096, 2)]
MAGIC = 12582912.0
TWO_PI = 2.0 * math.pi


class Buf:
    __slots__ = ("name", "w", "r")

    def __init__(self, name=""):
        self.name = name
        self.w = None
        self.r = {}


class AutoSync:
    def __init__(self, nc, ring_sizes=None):
        self.nc = nc
        self.q = {e: [] for e in ENGINES}
        self.cnt = {}
        self.sems = {}
        self.seen = {e: {} for e in ENGINES}
        for e in (PE, ACT, DVE, POOL):
            self._mksem(e)
        ring_sizes = ring_sizes or {SP: 40, POOL: 24, ACT: 8}
        self.ring = {}
        self.ring_pos = {}
        for qn, n in ring_sizes.items():
            keys = []
            for i in range(n):
                k = f"dma_{qn}_{i}"
                self._mksem(k)
                keys.append(k)
            self.ring[qn] = keys
            self.ring_pos[qn] = 0
        self.n_instr = 0

    def _mksem(self, key):
        self.sems[key] = self.nc.alloc_semaphore(f"s_{key}")
        self.cnt[key] = 0

    def _collect(self, eng, reads, writes, skip_self):
        need = {}

        def add(d):
            if d is None:
                return
            k, v = d
            if skip_self and k == eng:
                return
            if need.get(k, 0) < v:
                need[k] = v
        for b in reads:
            add(b.w)
        for b in writes:
            add(b.w)
            for k, v in b.r.items():
                add((k, v))
        seen = self.seen[eng]
        for k, v in need.items():
            if seen.get(k, 0) < v:
                seen[k] = v
                self.q[eng].append(("wait", k, v))

    def _mark(self, me, reads, writes):
        k, v = me
        for b in reads:
            if b.r.get(k, 0) < v:
                b.r[k] = v
        for b in writes:
            b.w = me
            b.r = {}

    def op(self, eng, fn, reads=(), writes=()):
        self._collect(eng, reads, writes, skip_self=(eng == PE))
        self.cnt[eng] += 1
        me = (eng, self.cnt[eng])
        self.q[eng].append(("op", fn, eng, 1))
        self._mark(me, reads, writes)
        self.n_instr += 1

    def dma(self, qn, fn, reads=(), writes=()):
        ring = self.ring[qn]
        key = ring[self.ring_pos[qn]]
        self.ring_pos[qn] = (self.ring_pos[qn] + 1) % len(ring)
        prev = self.cnt[key]
        if prev and self.seen[qn].get(key, 0) < prev:
            self.seen[qn][key] = prev
            self.q[qn].append(("wait", key, prev))
        self._collect(qn, reads, writes, skip_self=False)
        self.cnt[key] += 16
        me = (key, self.cnt[key])
        self.q[qn].append(("op", fn, key, 16))
        self._mark(me, reads, writes)
        self.n_instr += 1

    def barrier(self):
        for e in ENGINES:
            for k, v in self.cnt.items():
                if v and self.seen[e].get(k, 0) < v and not (k == e and e == PE):
                    self.seen[e][k] = v
                    self.q[e].append(("wait", k, v))

    def emit(self):
        nc = self.nc
        sems = self.sems

        def run(q):
            def body(e):
                for it in q:
                    if it[0] == "wait":
                        e.wait_ge(sems[it[1]], it[2])
                    else:
                        it[1](e).then_inc(sems[it[2]], it[3])
            return body

        with nc.Block() as block:
            block.tensor(run(self.q[PE]))
            block.scalar(run(self.q[ACT]))
            block.vector(run(self.q[DVE]))
            block.gpsimd(run(self.q[POOL]))
            block.sync(run(self.q[SP]))


_CONST = None


def _trig_tables(L):
    N = 2 * L
    nt = L // 128
    f = np.arange(L, dtype=np.float64) + 0.5
    s = np.arange(L, dtype=np.float64)
    k = (np.outer(2 * np.arange(L, dtype=np.int64) + 1, np.arange(L, dtype=np.int64))) % (2 * N)
    ang = k.astype(np.float64) * (math.pi / N)
    c = np.cos(ang)
    sn = -np.sin(ang)
    del k, ang
    fwd = np.empty((nt, 128, 2, nt, 128), dtype=NPBF)
    inv = np.empty((nt, 128, 2, nt, 128), dtype=NPBF)
    for i, m in enumerate((c, sn)):
        m4 = m.reshape(nt, 128, nt, 128)
        fwd[:, :, i] = m4.transpose(0, 3, 2, 1).astype(NPBF)
        mi = (m * (2.0 / N)).reshape(nt, 128, nt, 128)
        inv[:, :, i] = mi.transpose(2, 1, 0, 3).astype(NPBF)
    return (np.ascontiguousarray(fwd.reshape(nt, 128, 2 * nt * 128)),
            np.ascontiguousarray(inv.reshape(nt, 128, 2 * nt * 128)))


def _hy_feats(L):
    t = np.linspace(0.0, 1.0, L, dtype=np.float32)[:, None]
    omega = (2.0 * math.pi * np.arange(L, dtype=np.float32)[:, None] / L).astype(np.float32)
    bands = np.linspace(1e-4, 16 - 1, 16, dtype=np.float32)[None, :]
    feats = np.concatenate([t, np.cos(omega * bands), -np.sin(omega * bands)], axis=-1).astype(np.float32)
    max_decay = math.log(1e-2) / 0.3
    min_decay = math.log(1e-2) / 1.5
    deltas = np.linspace(min_decay, max_decay, 256, dtype=np.float32)
    decay = np.exp(-t * np.abs(deltas)[None, :]).astype(np.float32)
    nt = L // 128
    decay_t = np.ascontiguousarray(decay.reshape(nt, 128, 256).transpose(1, 0, 2))
    return np.ascontiguousarray(feats.T), decay_t


def _pool_band():
    out = np.zeros((128, 20, 128), dtype=np.float64)
    L = 1024
    for g, win in enumerate((2, 4, 8, 16)):
        A = np.zeros((L, L))
        for t in range(L):
            lo = max(t - win // 2, 0)
            hi = min(t + win - win // 2, L)
            A[t, lo:hi] = 1.0 / (hi - lo)
            A[t, t] -= 1.0
        last = L - 128
        out[:, g * 5 + 0, :] = A[0:128, 0:128].T
        out[:, g * 5 + 1, :] = A[256:384, 256:384].T
        out[:, g * 5 + 2, :] = A[last:, last:].T
        out[:, g * 5 + 3, :] = A[256:384, 128:256].T
        out[:, g * 5 + 4, :] = A[256:384, 384:512].T
    return out.astype(NPBF)


def _rope_tables():
    rows = S // 64
    row = np.repeat(np.arange(rows), 64).astype(np.float32)
    col = np.tile(np.arange(64), rows).astype(np.float32)
    inv = (10000.0 ** (-np.arange(0, 32, 2, dtype=np.float32) / 32)).astype(np.float32)
    ang = np.concatenate([row[:, None] * inv, col[:, None] * inv], axis=-1)
    cos = np.cos(ang).astype(np.float32).reshape(S, 2, 1, 16)
    sin = np.sin(ang).astype(np.float32).reshape(S, 2, 1, 16)
    cosF = np.broadcast_to(cos, (S, 2, 2, 16)).reshape(S, 1, 64)
    sinF = np.concatenate([-sin, sin], axis=2).reshape(S, 1, 64)
    cosF = np.ascontiguousarray(np.broadcast_to(cosF, (S, 10, 64)).reshape(S, 640))
    sinF = np.ascontiguousarray(np.broadcast_to(sinF, (S, 10, 64)).reshape(S, 640))
    return cosF, sinF


def _consts():
    global _CONST
    if _CONST is None:
        fwd, inv = _trig_tables(S)
        fwdc, invc = _trig_tables(CTX)
        featsT, decay_t = _hy_feats(S)
        featsTc, decay_tc = _hy_feats(CTX)
        cosF, sinF = _rope_tables()
        _CONST = dict(
            k_fwd=fwd, k_inv=inv, k_fwdc=fwdc, k_invc=invc,
            k_feats=featsT, k_decay=decay_t, k_featsc=featsTc, k_decayc=decay_tc,
            k_cosF=cosF, k_sinF=sinF, k_band=_pool_band(),
            k_identb=np.eye(128).astype(NPBF), k_identf=np.eye(128, dtype=np.float32),
        )
    return _CONST


INPUT_SHAPES = dict(
    x=[S, D], c=[D], ctx=[CTX, D], c_ctx=[D], w_mod=[DEPTH, D, 6 * D], b_mod=[DEPTH, 6 * D],
    w_in=[DEPTH, D, D_IN], q_gain=[DEPTH, 64], k_gain=[DEPTH, 64], hy_conv_w=[DEPTH, 3, 768],
    hy_conv_b=[DEPTH, 768], hy_f_w1=[DEPTH, 33, 64], hy_f_b1=[DEPTH, 64], hy_f_freq=[DEPTH, 64],
    hy_f_w2=[DEPTH, 64, 64], hy_f_b2=[DEPTH, 64], hy_f_w3=[DEPTH, 64, 1024], hy_d=[DEPTH, 2, 256],
    pool_w=[DEPTH, 4, 64, 64], pool_scale=[DEPTH, 256], w_out=[DEPTH, D, D], ln1_g=[DEPTH, D],
    ln1_b=[DEPTH, D], ln2_g=[DEPTH, D], ln2_b=[DEPTH, D], ffn_w_gate=[2, D, D_FF], ffn_w_up=[2, D, D_FF],
    ffn_w_down=[2, D_FF, D], router_w=[2, D, NEXP], moe_w_gate=[2, NEXP, D, D_FFE],
    moe_w_up=[2, NEXP, D, D_FFE], moe_w_down=[2, NEXP, D_FFE, D],
)


def build(n_layers=DEPTH, dbg=False):
    nc = bass.Bass("TRN2", target_bir_lowering=False)
    A = AutoSync(nc)
    cst = _consts()
    I = {}
    for k, shp in INPUT_SHAPES.items():
        I[k] = nc.dram_tensor(k, shp, F32, kind="ExternalInput").ap()
    for k, v in cst.items():
        I[k] = nc.dram_tensor(k, list(v.shape), BF16 if v.dtype == NPBF else F32, kind="ExternalInput").ap()
    out_d = nc.dram_tensor("out", [S, D], F32, kind="ExternalOutput").ap()
    DBG = {}
    if dbg:
        DBG["cat"] = nc.dram_tensor("dbg_cat", [8, 128, TOK], BF16, kind="ExternalOutput").ap()
        DBG["x1"] = nc.dram_tensor("dbg_x1", [TOK, D], F32, kind="ExternalOutput").ap()
        DBG["x2"] = nc.dram_tensor("dbg_x2", [TOK, D], F32, kind="ExternalOutput").ap()
        DBG["mod"] = nc.dram_tensor("dbg_mod", [128, DEPTH * 96], F32, kind="ExternalOutput").ap()
        DBG["uT"] = nc.dram_tensor("dbg_uT", [8, 128, TOK], BF16, kind="ExternalOutput").ap()
        DBG["hy"] = nc.dram_tensor("dbg_hy", [6, 128, TOK], F32, kind="ExternalOutput").ap()

    xres = nc.dram_tensor("xres", [TOK, D], F32).ap()
    uT_d = nc.dram_tensor("uT_d", [8, 128, TOK], BF16).ap()
    hyraw = nc.dram_tensor("hyraw", [6, 128, TOK], F32).ap()
    catT = nc.dram_tensor("catT", [8, 128, TOK], BF16).ap()
    gate_d = nc.dram_tensor("gate_d", [DEPTH, 4, D], F32).ap()
    invn_d = nc.dram_tensor("invn_d", [2, 512], F32).ap()
    Bxres = [Buf(f"xres{i}") for i in range(NT)]
    BuTd = [Buf(f"uTd{i}") for i in range(9)]
    Bhyraw = Buf("hyraw")
    Bcat = [Buf(f"cat{i}") for i in range(9)]
    Bgate = Buf("gate_d")
    Binvn = Buf("invn")
    Bout = Buf("out")
    Bdbg = Buf("dbg")

    ps = nc.alloc_psum_tensor("ps", [128, 8, 512], F32)
    Bps = [Buf(f"ps{i}") for i in range(8)]

    def psb(bank):
        return ps[:, bank, :].bitcast(BF16)

    glob = ExitStack()
    _uid = [0]
    _orig_sbuf = nc.sbuf_tensor

    def _sbuf_unique(name, shape, dt, **kw):
        _uid[0] += 1
        return _orig_sbuf(f"{name}_{_uid[0]}", shape, dt, **kw)

    def GT(name, shape, dt):
        return glob.enter_context(_sbuf_unique(name, shape, dt)), Buf(name)

    identb, Bidb = GT("identb", [128, 128], BF16)
    identf, Bidf = GT("identf", [128, 128], F32)
    modT, BmodT = GT("modT", [128, DEPTH, 48, 2], F32)
    ones_b, Bones = GT("ones_b", [128, 1], BF16)
    A.dma(SP, lambda e: e.dma_start(out=identb[:], in_=I["k_identb"]), writes=[Bidb])
    A.dma(SP, lambda e: e.dma_start(out=identf[:], in_=I["k_identf"]), writes=[Bidf])
    A.op(DVE, lambda e: e.memset(ones_b[:], 1.0), writes=[Bones])

    def load_cols(T, dst, Bdst, vec, n):
        tmp, Btmp = T("lc_tmp", [n, 128], F32)
        A.dma(SP, lambda e: e.dma_start(out=tmp[:], in_=vec.rearrange("(n p) -> n p", p=128)), writes=[Btmp])
        A.op(PE, lambda e: e.transpose(out=ps[:, 7, 0:n], in_=tmp[:], identity=identf[0:n, 0:n]),
             reads=[Btmp, Bidf], writes=[Bps[7]])
        A.op(DVE, lambda e: e.tensor_copy(out=dst, in_=ps[:, 7, 0:n]), reads=[Bps[7]], writes=[Bdst])

    def ln_stats(T, xt, Bx, eps, tag):
        st, Bst = T(f"st_{tag}", [128, 2, 6], F32)
        mv, Bmv = T(f"mv_{tag}", [128, 2], F32)
        rs, Brs = T(f"rs_{tag}", [128, 1], F32)
        return (st, Bst, mv, Bmv, rs, Brs)

    def ln_compute(stt, xt, Bx, eps):
        st, Bst, mv, Bmv, rs, Brs = stt
        for h in range(2):
            A.op(DVE, lambda e, h=h: e.bn_stats(out=st[:, h, :], in_=xt[:, h * 512:(h + 1) * 512]),
                 reads=[Bx], writes=[Bst])
        A.op(DVE, lambda e: e.bn_aggr(out=mv[:], in_=st[:].rearrange("p a b -> p (a b)")), reads=[Bst], writes=[Bmv])
        A.op(ACT, lambda e: e.activation(out=rs[:], in_=mv[:, 1:2], func=AF.Sqrt, bias=float(eps), scale=1.0),
             reads=[Bmv], writes=[Brs])
        A.op(DVE, lambda e: e.reciprocal(out=rs[:], in_=rs[:]), reads=[Brs], writes=[Brs])

    def modulate_block(uh_tiles, nti, l, j, base, uT, BuT):
        for ti, (uh, Buh) in enumerate(uh_tiles):
            for k in range(8):
                A.op(PE, lambda e, k=k, ti=ti, uh=uh: e.transpose(
                    out=psb(k // 2)[:, (k % 2) * 512 + ti * 128:(k % 2) * 512 + (ti + 1) * 128],
                    in_=uh[:, k * 128:(k + 1) * 128], identity=identb[:]),
                    reads=[Buh, Bidb], writes=[Bps[k // 2]])
        n = nti * 128
        for k in range(8):
            eng = ACT if k % 2 == 0 else DVE
            src = psb(k // 2)[:, (k % 2) * 512:(k % 2) * 512 + n]
            sc = modT[:, l, base + 8 + k, j:j + 1]
            sh = modT[:, l, base + k, j:j + 1]
            if eng == ACT:
                A.op(ACT, lambda e, k=k, src=src, sc=sc, sh=sh: e.activation(
                    out=uT[:, k, 0:n], in_=src, func=AF.Identity, scale=sc, bias=sh),
                    reads=[Bps[k // 2], BmodT], writes=[BuT])
            else:
                A.op(DVE, lambda e, k=k, src=src, sc=sc, sh=sh: e.tensor_scalar(
                    out=uT[:, k, 0:n], in0=src, scalar1=sc, scalar2=sh, op0=ALU.mult, op1=ALU.add),
                    reads=[Bps[k // 2], BmodT], writes=[BuT])

    with ExitStack() as stk:
        def T(name, shape, dt):
            return stk.enter_context(_sbuf_unique(name, shape, dt)), Buf(name)
        cT, BcT = T("cT", [128, 8, 2], F32)
        sT, BsT = T("sT", [128, 8, 2], BF16)
        load_cols(T, cT[:, :, 0], BcT, I["c"], 8)
        load_cols(T, cT[:, :, 1], BcT, I["c_ctx"], 8)
        A.op(ACT, lambda e: e.activation(out=sT[:], in_=cT[:], func=AF.Silu), reads=[BcT], writes=[BsT])
        wm = [T(f"wm{i}", [128, 8, 512], BF16) for i in range(3)]
        bT, BbT = T("bT", [128, 48], F32)
        for l in range(n_layers):
            load_cols(T, bT[:], BbT, I["b_mod"][l], 48)
            for nb in range(12):
                w, Bw = wm[nb % 3]
                A.dma(POOL, lambda e, w=w, l=l, nb=nb: e.dma_start(
                    out=w[:], in_=I["w_mod"][l, :, nb * 512:(nb + 1) * 512].rearrange("(k p) n -> p k n", p=128)),
                    writes=[Bw])
                for jj in range(4):
                    nch = nb * 4 + jj
                    for k in range(8):
                        A.op(PE, lambda e, w=w, k=k, jj=jj, nch=nch: e.matmul(
                            ps[:, 6, nch * 2:nch * 2 + 2], lhsT=w[:, k, jj * 128:(jj + 1) * 128], rhs=sT[:, k, :],
                            start=(k == 0), stop=(k == 7)), reads=[Bw, BsT], writes=[Bps[6]])
            for j in range(2):
                A.op(DVE, lambda e, l=l, j=j: e.tensor_tensor(
                    out=modT[:, l, :, j], in0=ps[:, 6, 0:96].rearrange("p (n t) -> p n t", t=2)[:, :, j],
                    in1=bT[:], op=ALU.add), reads=[Bps[6], BbT], writes=[BmodT])
            for base in (8, 32):
                A.op(DVE, lambda e, l=l, base=base: e.tensor_scalar(
                    out=modT[:, l, base:base + 8, :], in0=modT[:, l, base:base + 8, :], scalar1=1.0, scalar2=None,
                    op0=ALU.add), reads=[BmodT], writes=[BmodT])
            for base in (16, 40):
                A.op(DVE, lambda e, l=l, base=base: e.tensor_scalar(
                    out=modT[:, l, base:base + 8, :], in0=modT[:, l, base:base + 8, :], scalar1=1.0 / ALPHA,
                    scalar2=None, op0=ALU.mult), reads=[BmodT], writes=[BmodT])
            for gi, base in enumerate((16, 40)):
                for j in range(2):
                    A.dma(SP, lambda e, l=l, gi=gi, base=base, j=j: e.dma_start(
                        out=gate_d[l, gi * 2 + j].rearrange("(k p) -> p k", p=128),
                        in_=modT[:, l, base:base + 8, j], allow_slow_non_contiguous=True),
                        reads=[BmodT], writes=[Bgate])
        if dbg:
            A.dma(SP, lambda e: e.dma_start(out=DBG["mod"], in_=modT[:].rearrange("p l n t -> p (l n t)")),
                  reads=[BmodT], writes=[Bdbg])
        A.barrier()

    def layer(l):
        last = (l == DEPTH - 1)
        moe = (l % 2 == 1)
        jf = l // 2
        nblk = 8 if last else 9
        xsrc = (lambda t0, n: (I["x"][t0:t0 + n, :] if t0 < S else I["ctx"][t0 - S:t0 - S + n, :])) if l == 0 \
            else (lambda t0, n: xres[t0:t0 + n, :])

        with ExitStack() as stk:
            def T(name, shape, dt):
                return stk.enter_context(_sbuf_unique(name, shape, dt)), Buf(name)
            qT, BqT = T("qT", [128, 4, TOK], BF16)
            kT, BkT = T("kT", [128, 4, TOK], BF16)
            vp, Bvp = T("vx", [128, NT, 2, 128], BF16)
            pl_tm, Bpl = T("pl_tm", [128, NT, 256], BF16)
            A.op(DVE, lambda e: e.memset(vp[:], 1.0), writes=[Bvp])
            with ExitStack() as stk1:
                def T1(name, shape, dt):
                    return stk1.enter_context(_sbuf_unique(name, shape, dt)), Buf(name)
                wtm, Bwtm = T1("wtm", [128, 8, 1024], BF16)
                whm, Bwhm = T1("whm", [128, 8, 768], BF16)
                wv = I["w_in"][l].rearrange("(k p) n -> p k n", p=128)
                A.dma(POOL, lambda e: e.dma_start(out=wtm[:, :, 0:768], in_=wv[:, :, 0:768]), writes=[Bwtm])
                A.dma(POOL, lambda e: e.dma_start(out=wtm[:, :, 768:1024], in_=wv[:, :, 1536:1792]), writes=[Bwtm])
                A.dma(POOL, lambda e: e.dma_start(out=whm[:], in_=wv[:, :, 768:1536]), writes=[Bwhm])
                gain, Bgain = T1("gain", [128, 640], F32)
                A.dma(SP, lambda e: e.dma_start(out=gain[:, 0:64], in_=I["q_gain"][l].partition_broadcast(128)), writes=[Bgain])
                A.dma(SP, lambda e: e.dma_start(out=gain[:, 512:576], in_=I["k_gain"][l].partition_broadcast(128)), writes=[Bgain])
                A.op(DVE, lambda e: e.tensor_scalar(out=gain[:, 0:64], in0=gain[:, 0:64], scalar1=0.125, scalar2=None,
                                                    op0=ALU.mult), reads=[Bgain], writes=[Bgain])
                for h in range(1, 8):
                    A.op(DVE, lambda e, h=h: e.tensor_copy(out=gain[:, h * 64:(h + 1) * 64], in_=gain[:, 0:64]),
                         reads=[Bgain], writes=[Bgain])
                A.op(DVE, lambda e: e.tensor_copy(out=gain[:, 576:640], in_=gain[:, 512:576]), reads=[Bgain], writes=[Bgain])
                xt_r = [T1(f"xt{i}", [128, D], F32) for i in range(3)]
                uh_r = [T1(f"uh{i}", [128, D], BF16) for i in range(6)]
                uT_r = [T1(f"uTb{i}", [128, 8, 512], BF16) for i in range(2)]
                stt = [ln_stats(T1, None, None, None, f"a{i}") for i in range(2)]
                qk_r = [T1(f"qk{i}", [128, 640], F32) for i in range(2)]
                sq, Bsq = T1("sq", [128, 640], F32)
                ssq, Bssq = T1("ssq", [128, 10], F32)
                cosr = [T1(f"cos{i}", [128, 640], F32) for i in range(2)]
                sinr = [T1(f"sin{i}", [128, 640], F32) for i in range(2)]
                t1, Bt1 = T1("t1", [128, 640], F32)
                t2, Bt2 = T1("t2", [128, 640], F32)
                qkr_r = [T1(f"qkr{i}", [128, 1024], BF16) for i in range(2)]
                for qkr_, Bqkr_ in qkr_r:
                    A.op(POOL, lambda e, qkr_=qkr_: e.memset(qkr_[:, 512:1024], 0.0), writes=[Bqkr_])
                hys_r = [T1(f"hys{i}", [128, 512], F32) for i in range(2)]
                tile_ctr = 0
                for bi, (t0, nti) in enumerate(BLOCKS):
                    is_ctx = (t0 >= S)
                    jm = 1 if is_ctx else 0
                    n = nti * 128
                    uT, BuT = uT_r[bi % 2]
                    uhs = []
                    for ti in range(nti):
                        g = tile_ctr + ti
                        xt, Bx = xt_r[g % 3]
                        uh, Buh = uh_r[g % 6]
                        A.dma(SP, lambda e, xt=xt, t0=t0, ti=ti: e.dma_start(out=xt[:], in_=xsrc(t0 + ti * 128, 128)),
                              reads=[Bxres[t0 // 128 + ti]], writes=[Bx])
                        s_ = stt[g % 2]
                        ln_compute(s_, xt, Bx, ADA_EPS)
                        A.op(DVE, lambda e, xt=xt, uh=uh, s_=s_: e.tensor_scalar(
                            out=uh[:], in0=xt[:], scalar1=s_[2][:, 0:1], scalar2=s_[4][:], op0=ALU.subtract, op1=ALU.mult),
                            reads=[Bx, s_[3], s_[5]], writes=[Buh])
                        uhs.append((uh, Buh))
                    modulate_block(uhs, nti, l, jm, 0, uT, BuT)
                    if dbg:
                        A.dma(SP, lambda e, uT=uT, t0=t0, n=n: e.dma_start(
                            out=DBG["uT"][:, :, t0:t0 + n].rearrange("k p t -> p k t"), in_=uT[:, :, 0:n]),
                            reads=[BuT], writes=[Bdbg])
                    if not (last and is_ctx):
                        for hc in range(6):
                            bank = 6 + (hc % 2)
                            for k in range(8):
                                A.op(PE, lambda e, hc=hc, k=k, bank=bank, uT=uT, n=n: e.matmul(
                                    ps[:, bank, 0:n], lhsT=whm[:, k, hc * 128:(hc + 1) * 128], rhs=uT[:, k, 0:n],
                                    start=(k == 0), stop=(k == 7)), reads=[Bwhm, BuT], writes=[Bps[bank]])
                            hs, Bhs = hys_r[hc % 2]
                            A.op(ACT, lambda e, hs=hs, bank=bank, n=n: e.copy(out=hs[:, 0:n], in_=ps[:, bank, 0:n]),
                                 reads=[Bps[bank]], writes=[Bhs])
                            A.dma(SP, lambda e, hs=hs, hc=hc, t0=t0, n=n: e.dma_start(
                                out=hyraw[hc, :, t0:t0 + n], in_=hs[:, 0:n]), reads=[Bhs], writes=[Bhyraw])
                    for ti in range(nti):
                        g = tile_ctr + ti
                        tok = t0 + ti * 128
                        for nb in range(2):
                            bank = 4 + nb
                            for k in range(8):
                                A.op(PE, lambda e, nb=nb, k=k, bank=bank, uT=uT, ti=ti: e.matmul(
                                    ps[:, bank, :], lhsT=uT[:, k, ti * 128:(ti + 1) * 128],
                                    rhs=wtm[:, k, nb * 512:(nb + 1) * 512], start=(k == 0), stop=(k == 7)),
                                    reads=[Bwtm, BuT], writes=[Bps[bank]])
                        qk, Bqk = qk_r[g % 2]
                        A.op(ACT, lambda e, qk=qk: e.copy(out=qk[:, 0:512], in_=ps[:, 4, :]), reads=[Bps[4]], writes=[Bqk])
                        A.op(ACT, lambda e, qk=qk: e.copy(out=qk[:, 512:640], in_=ps[:, 5, 0:128]), reads=[Bps[5]], writes=[Bqk])
                        A.op(ACT, lambda e, g=g: e.copy(out=vp[:, g, :, 0:64],
                                                        in_=ps[:, 5, 128:256].rearrange("p (a b) -> p a b", b=64)),
                             reads=[Bps[5]], writes=[Bvp])
                        A.op(ACT, lambda e, g=g: e.copy(out=pl_tm[:, g, :], in_=ps[:, 5, 256:512]),
                             reads=[Bps[5]], writes=[Bpl])
                        A.op(DVE, lambda e, qk=qk: e.tensor_tensor(out=sq[:], in0=qk[:], in1=qk[:], op=ALU.mult),
                             reads=[Bqk], writes=[Bsq])
                        A.op(DVE, lambda e: e.tensor_reduce(out=ssq[:], in_=sq[:].rearrange("p (h d) -> p h d", d=64),
                                                            axis=AX.X, op=ALU.add), reads=[Bsq], writes=[Bssq])
                        A.op(ACT, lambda e: e.activation(out=ssq[:], in_=ssq[:], func=AF.Sqrt, bias=QK_EPS, scale=1.0 / 64),
                             reads=[Bssq], writes=[Bssq])
                        A.op(DVE, lambda e: e.reciprocal(out=ssq[:], in_=ssq[:]), reads=[Bssq], writes=[Bssq])
                        for h in range(10):
                            A.op(ACT, lambda e, h=h, qk=qk: e.activation(
                                out=qk[:, h * 64:(h + 1) * 64], in_=qk[:, h * 64:(h + 1) * 64], func=AF.Copy,
                                scale=ssq[:, h:h + 1]), reads=[Bqk, Bssq], writes=[Bqk])
                        qkr, Bqkr = qkr_r[g % 2]
                        kd4 = qkr[:, 512:1024].rearrange("p (a b c d) -> p a b c d", a=2, b=2, c=2)
                        if is_ctx:
                            A.op(DVE, lambda e, qk=qk, qkr=qkr: e.tensor_tensor(
                                out=qkr[:, 0:512], in0=qk[:, 0:512], in1=gain[:, 0:512], op=ALU.mult),
                                reads=[Bqk, Bgain], writes=[Bqkr])
                            for b2 in range(2):
                                A.op(DVE, lambda e, qk=qk, kd4=kd4, b2=b2: e.tensor_tensor(
                                    out=kd4[:, :, b2, b2, :], in0=qk[:, 512:640].rearrange("p (a d) -> p a d", d=64),
                                    in1=gain[:, 512:640].rearrange("p (a d) -> p a d", d=64), op=ALU.mult),
                                    reads=[Bqk, Bgain], writes=[Bqkr])
                        else:
                            cs_, Bcs = cosr[g % 2]
                            sn_, Bsn = sinr[g % 2]
                            A.dma(SP, lambda e, cs_=cs_, tok=tok: e.dma_start(out=cs_[:], in_=I["k_cosF"][tok:tok + 128, :]), writes=[Bcs])
                            A.dma(SP, lambda e, sn_=sn_, tok=tok: e.dma_start(out=sn_[:], in_=I["k_sinF"][tok:tok + 128, :]), writes=[Bsn])
                            A.op(DVE, lambda e, qk=qk: e.tensor_tensor(out=qk[:], in0=qk[:], in1=gain[:], op=ALU.mult),
                                 reads=[Bqk, Bgain], writes=[Bqk])
                            A.op(DVE, lambda e, qk=qk, cs_=cs_: e.tensor_tensor(out=t1[:], in0=qk[:], in1=cs_[:], op=ALU.mult),
                                 reads=[Bqk, Bcs], writes=[Bt1])
                            qv = qk[:].rearrange("p (h s d) -> p h s d", s=2, d=16)
                            sv = sn_[:].rearrange("p (h s d) -> p h s d", s=2, d=16)
                            tv = t2[:].rearrange("p (h s d) -> p h s d", s=2, d=16)
                            for s2 in range(2):
                                A.op(POOL, lambda e, s2=s2, qv=qv, sv=sv, tv=tv: e.tensor_tensor(
                                    out=tv[:, :, s2, :], in0=qv[:, :, 1 - s2, :], in1=sv[:, :, s2, :], op=ALU.mult),
                                    reads=[Bqk, Bsn], writes=[Bt2])
                            A.op(DVE, lambda e, qkr=qkr: e.tensor_tensor(out=qkr[:, 0:512], in0=t1[:, 0:512], in1=t2[:, 0:512], op=ALU.add),
                                 reads=[Bt1, Bt2], writes=[Bqkr])
                            for b2 in range(2):
                                A.op(DVE, lambda e, kd4=kd4, b2=b2: e.tensor_tensor(
                                    out=kd4[:, :, b2, b2, :], in0=t1[:, 512:640].rearrange("p (a d) -> p a d", d=64),
                                    in1=t2[:, 512:640].rearrange("p (a d) -> p a d", d=64), op=ALU.add),
                                    reads=[Bt1, Bt2], writes=[Bqkr])
                        for c6 in range(8):
                            A.op(PE, lambda e, c6=c6, qkr=qkr: e.transpose(
                                out=psb(6)[:, c6 * 128:(c6 + 1) * 128], in_=qkr[:, c6 * 128:(c6 + 1) * 128], identity=identb[:]),
                                reads=[Bqkr, Bidb], writes=[Bps[6]])
                        A.op(DVE, lambda e, tok=tok: e.tensor_copy(
                            out=qT[:, :, tok:tok + 128], in_=psb(6)[:, 0:512].rearrange("p (a t) -> p a t", t=128)),
                            reads=[Bps[6]], writes=[BqT])
                        A.op(DVE, lambda e, tok=tok: e.tensor_copy(
                            out=kT[:, :, tok:tok + 128], in_=psb(6)[:, 512:1024].rearrange("p (a t) -> p a t", t=128)),
                            reads=[Bps[6]], writes=[BkT])
                    tile_ctr += nti
                A.barrier()

            if True:
                with ExitStack() as stk2:
                    def T2(name, shape, dt):
                        return stk2.enter_context(_sbuf_unique(name, shape, dt)), Buf(name)
                    band, Bband = T2("band", [128, 20, 128], BF16)
                    pw, Bpw = T2("pw", [128, 2, 64], BF16)
                    psc, Bpsc = T2("psc", [128, 2], F32)
                    A.dma(SP, lambda e: e.dma_start(out=band[:], in_=I["k_band"]), writes=[Bband])
                    A.dma(POOL, lambda e: e.dma_start(out=pw[:], in_=I["pool_w"][l].rearrange("(a b) c d -> (b c) a d", b=2)), writes=[Bpw])
                    load_cols(T2, psc[:], Bpsc, I["pool_scale"][l], 2)
                    yp_r = [T2(f"yp{i}", [128, 2, 128], BF16) for i in range(2)]
                    po_r = [T2(f"po{i}", [128, 2, 512], BF16) for i in range(2)]
                    seqs = [(0, 32)] if last else [(0, 32), (32, 2)]
                    for (g0, ng) in seqs:
                        for tb in range(0, ng, 4):
                            nb4 = min(4, ng - tb)
                            po, Bpo = po_r[(tb // 4) % 2]
                            for tq in range(nb4):
                                tt = tb + tq
                                g = g0 + tt
                                yp, Byp = yp_r[g % 2]
                                for grp in range(4):
                                    srcs = []
                                    if tt > 0:
                                        srcs.append((g - 1, 3))
                                    srcs.append((g, 0 if tt == 0 else (2 if tt == ng - 1 else 1)))
                                    if tt < ng - 1:
                                        srcs.append((g + 1, 4))
                                    h2 = (grp % 2) * 64
                                    for si, (sg, kind) in enumerate(srcs):
                                        A.op(PE, lambda e, grp=grp, sg=sg, kind=kind, si=si, ns=len(srcs), h2=h2: e.matmul(
                                            ps[h2:h2 + 64, 6, (grp // 2) * 128:(grp // 2 + 1) * 128],
                                            lhsT=pl_tm[:, sg, grp * 64:(grp + 1) * 64], rhs=band[:, grp * 5 + kind, :],
                                            start=(si == 0), stop=(si == ns - 1), skip_group_check=True),
                                            reads=[Bpl, Bband], writes=[Bps[6]])
                                A.op(ACT, lambda e, yp=yp: e.copy(out=yp[:], in_=ps[:, 6, 0:256].rearrange("p (a t) -> p a t", t=128)),
                                     reads=[Bps[6]], writes=[Byp])
                                for grp in range(4):
                                    h2 = (grp % 2) * 64
                                    A.op(PE, lambda e, grp=grp, h2=h2, yp=yp: e.matmul(
                                        ps[h2:h2 + 64, 7, (grp // 2) * 128:(grp // 2 + 1) * 128],
                                        lhsT=pw[h2:h2 + 64, grp // 2, :], rhs=yp[h2:h2 + 64, grp // 2, :],
                                        start=True, stop=True, skip_group_check=True), reads=[Bpw, Byp], writes=[Bps[7]])
                                for a in range(2):
                                    A.op(ACT, lambda e, a=a, po=po, tq=tq: e.activation(
                                        out=po[:, a, tq * 128:(tq + 1) * 128], in_=ps[:, 7, a * 128:(a + 1) * 128],
                                        func=AF.Copy, scale=psc[:, a:a + 1]), reads=[Bps[7], Bpsc], writes=[Bpo])
                            tok0 = (g0 + tb) * 128
                            bi = min(tok0 // 512, 8)
                            A.dma(SP, lambda e, po=po, tok0=tok0, nb4=nb4: e.dma_start(
                                out=catT[6:8, :, tok0:tok0 + nb4 * 128].rearrange("k p t -> p k t"), in_=po[:, :, 0:nb4 * 128]),
                                reads=[Bpo], writes=[Bcat[bi]])
                    A.barrier()

            with ExitStack() as stk3:
                def T3(name, shape, dt):
                    return stk3.enter_context(_sbuf_unique(name, shape, dt)), Buf(name)
                ex_r = [T3(f"ex{i}", [128, 2, 512], BF16) for i in range(4)]
                rc_r = [T3(f"rc{i}", [64, 512], F32) for i in range(2)]
                ca_r = [T3(f"ca{i}", [128, 4, 512], BF16) for i in range(2)]
                SB = (0, 2, 6)
                qblocks = [(i * 512, 4, list(range(NT))) for i in range(8)]
                if not last:
                    qblocks.append((S, 2, [32, 33]))
                units = []
                for qi, (q0, nq, kts) in enumerate(qblocks):
                    for h in range(8):
                        pairs = [kts[i:i + 2] for i in range(0, len(kts), 2)]
                        for pi, pk in enumerate(pairs):
                            units.append((qi, q0, nq, h, pi, len(pairs), pk))

                def emit_qk(ui):
                    qi, q0, nq, h, pi, npairs, pk = units[ui]
                    nqt = nq * 128
                    kv, hf, pr = h // 4, (h % 2) * 64, h // 2
                    sb0 = SB[ui % 3]
                    for j2, kt in enumerate(pk):
                        A.op(PE, lambda e, kt=kt, j2=j2, sb0=sb0, h=h, kv=kv, pr=pr, q0=q0, nqt=nqt: e.matmul(
                            ps[:, sb0 + j2, 0:nqt], lhsT=kT[:, kv * 2 + (h % 2), kt * 128:(kt + 1) * 128],
                            rhs=qT[:, pr, q0:q0 + nqt], start=True, stop=True),
                            reads=[BkT, BqT], writes=[Bps[sb0 + j2]])

                LOOK = 2
                for ui in range(min(LOOK, len(units))):
                    emit_qk(ui)
                for ui, (qi, q0, nq, h, pi, npairs, pk) in enumerate(units):
                    nqt = nq * 128
                    kv, hf, pr = h // 4, (h % 2) * 64, h // 2
                    obank = 4 + (h % 2)
                    sb0 = SB[ui % 3]
                    npk = len(pk)
                    ex, Bex = ex_r[ui % 4]
                    A.op(ACT, lambda e, ex=ex, sb0=sb0, npk=npk, nqt=nqt: e.activation(
                        out=ex[:, 0:npk, 0:nqt], in_=ps[:, sb0:sb0 + npk, 0:nqt], func=AF.Exp),
                        reads=[Bps[sb0 + i] for i in range(npk)], writes=[Bex])
                    if ui + LOOK < len(units):
                        emit_qk(ui + LOOK)
                    for j2, kt in enumerate(pk):
                        first = (pi == 0 and j2 == 0)
                        lastmm = (pi == npairs - 1 and j2 == npk - 1)
                        A.op(PE, lambda e, ex=ex, j2=j2, kt=kt, kv=kv, obank=obank, first=first, lastmm=lastmm, nqt=nqt: e.matmul(
                            ps[:, obank, 0:nqt], lhsT=vp[:, kt, kv, :], rhs=ex[:, j2, 0:nqt],
                            start=first, stop=lastmm), reads=[Bex, Bvp], writes=[Bps[obank]])
                    if pi == npairs - 1:
                        ca, Bca = ca_r[qi % 2]
                        rc, Brc = rc_r[h % 2]
                        A.op(DVE, lambda e, rc=rc, obank=obank, nqt=nqt: e.reciprocal(
                            out=rc[0:64, 0:nqt], in_=ps[64:128, obank, 0:nqt]), reads=[Bps[obank]], writes=[Brc])
                        A.op(DVE, lambda e, rc=rc, ca=ca, obank=obank, nqt=nqt, hf=hf, pr=pr: e.tensor_tensor(
                            out=ca[hf:hf + 64, pr, 0:nqt], in0=ps[0:64, obank, 0:nqt], in1=rc[0:64, 0:nqt], op=ALU.mult),
                            reads=[Bps[obank], Brc], writes=[Bca])
                        if h == 7:
                            bi = min(q0 // 512, 8)
                            A.dma(SP, lambda e, ca=ca, q0=q0, nqt=nqt: e.dma_start(
                                out=catT[0:4, :, q0:q0 + nqt].rearrange("k p t -> p k t"), in_=ca[:, :, 0:nqt]),
                                reads=[Bca], writes=[Bcat[bi]])
                A.barrier()

        def hyena(seq_t0, L, fwd_d, inv_d, feats_d, decay_d):
            nt = L // 128
            nfb = max(L // 512, 1)
            fbw = min(L, 512)
            with ExitStack() as stk:
                def T(name, shape, dt):
                    return stk.enter_context(_sbuf_unique(name, shape, dt)), Buf(name)
                Ksp, BK = T("Ksp", [128, nt, 2, 512], BF16)
                slab_r = [T(f"slab{i}", [128, 2, nt, 128], BF16) for i in range(2)]
                slab_ctr = [0]

                def load_slab(src_d, idx):
                    sl, Bsl = slab_r[slab_ctr[0] % 2]
                    slab_ctr[0] += 1
                    A.dma(SP, lambda e, sl=sl, idx=idx: e.dma_start(
                        out=sl[:].rearrange("p a b c -> p (a b c)"), in_=src_d[idx]), writes=[Bsl])
                    return sl, Bsl

                with ExitStack() as stkf:
                    def TF(name, shape, dt):
                        return stkf.enter_context(_sbuf_unique(name, shape, dt)), Buf(name)
                    Ptm, BP = TF("Ptm", [128, nt, 512], BF16)
                    Qtm, BQ = TF("Qtm", [128, nt, 512], BF16)
                    fe_r = [TF(f"feats{i}", [33, 512], F32) for i in range(2)]
                    dc_r = [TF(f"decay{i}", [128, 256], F32) for i in range(2)]
                    w1, Bw1 = TF("w1", [33, 64], F32)
                    w2, Bw2 = TF("w2", [64, 64], F32)
                    w3, Bw3 = TF("w3", [64, 1024], F32)
                    fb, Bfb = TF("fb", [64, 3], F32)
                    h1, Bh1 = TF("h1", [64, 512], F32)
                    h2, Bh2 = TF("h2", [64, 512], F32)
                    arg, Barg = TF("arg", [64, 512], F32)
                    rr, Brr = TF("rr", [64, 512], F32)
                    A.dma(SP, lambda e: e.dma_start(out=w1[:], in_=I["hy_f_w1"][l]), writes=[Bw1])
                    A.dma(SP, lambda e: e.dma_start(out=w2[:], in_=I["hy_f_w2"][l]), writes=[Bw2])
                    A.dma(SP, lambda e: e.dma_start(out=w3[:], in_=I["hy_f_w3"][l]), writes=[Bw3])
                    for ci, nm in enumerate(("hy_f_b1", "hy_f_freq", "hy_f_b2")):
                        A.dma(SP, lambda e, ci=ci, nm=nm: e.dma_start(
                            out=fb[:, ci:ci + 1], in_=I[nm][l].rearrange("(p o) -> p o", o=1)), writes=[Bfb])

                    def sin_layer(wt, Bwt, kdim, src, Bsrc, bcol, dst, Bdst):
                        A.op(PE, lambda e: e.matmul(ps[0:64, 0, 0:fbw], lhsT=wt[0:kdim, :], rhs=src[0:kdim, 0:fbw],
                                                    start=True, stop=True), reads=[Bwt, Bsrc], writes=[Bps[0]])
                        A.op(DVE, lambda e: e.tensor_scalar(out=arg[:, 0:fbw], in0=ps[0:64, 0, 0:fbw], scalar1=fb[:, bcol:bcol + 1],
                                                            scalar2=fb[:, 1:2], op0=ALU.add, op1=ALU.mult),
                             reads=[Bps[0], Bfb], writes=[Barg])
                        A.op(DVE, lambda e: e.tensor_scalar(out=rr[:, 0:fbw], in0=arg[:, 0:fbw], scalar1=1.0 / TWO_PI, scalar2=MAGIC,
                                                            op0=ALU.mult, op1=ALU.add), reads=[Barg], writes=[Brr])
                        A.op(DVE, lambda e: e.tensor_scalar(out=rr[:, 0:fbw], in0=rr[:, 0:fbw], scalar1=-MAGIC, scalar2=-TWO_PI,
                                                            op0=ALU.add, op1=ALU.mult), reads=[Brr], writes=[Brr])
                        A.op(DVE, lambda e: e.tensor_tensor(out=arg[:, 0:fbw], in0=arg[:, 0:fbw], in1=rr[:, 0:fbw], op=ALU.add),
                             reads=[Barg, Brr], writes=[Barg])
                        A.op(DVE, lambda e: e.tensor_scalar(out=arg[:, 0:fbw], in0=arg[:, 0:fbw], scalar1=math.pi, scalar2=-math.pi,
                                                            op0=ALU.min, op1=ALU.max), reads=[Barg], writes=[Barg])
                        A.op(ACT, lambda e: e.activation(out=dst[:, 0:fbw], in_=arg[:, 0:fbw], func=AF.Sin),
                             reads=[Barg], writes=[Bdst])
                    hd_r = [TF(f"hd{i}", [128, 2, 2, 256], F32) for i in range(2)]
                    ab_r = [TF(f"ab{i}", [128, 1024], BF16) for i in range(2)]
                    for fbk in range(nfb):
                        feats, Bfe = fe_r[fbk % 2]
                        A.dma(SP, lambda e, feats=feats, fbk=fbk: e.dma_start(out=feats[:, 0:fbw], in_=feats_d[:, fbk * fbw:(fbk + 1) * fbw]),
                              writes=[Bfe])
                        sin_layer(w1, Bw1, 33, feats, Bfe, 0, h1, Bh1)
                        sin_layer(w2, Bw2, 64, h1, Bh1, 2, h2, Bh2)
                        for jj in range(fbw // 128):
                            j = fbk * (fbw // 128) + jj
                            hd, Bhd = hd_r[j % 2]
                            ab, Bab = ab_r[j % 2]
                            decay, Bdec = dc_r[j % 2]
                            A.dma(SP, lambda e, decay=decay, j=j: e.dma_start(out=decay[:], in_=decay_d[:, j, :]), writes=[Bdec])
                            for o in range(2):
                                A.op(PE, lambda e, o=o, jj=jj: e.matmul(ps[:, 1 + o, :], lhsT=h2[:, jj * 128:(jj + 1) * 128],
                                                                        rhs=w3[:, o * 512:(o + 1) * 512], start=True, stop=True),
                                     reads=[Bh2, Bw3], writes=[Bps[1 + o]])
                                for dr in range(2):
                                    A.op(DVE, lambda e, o=o, dr=dr, hd=hd, decay=decay: e.tensor_tensor(
                                        out=hd[:, o, dr, :], in0=ps[:, 1 + o, dr * 256:(dr + 1) * 256], in1=decay[:], op=ALU.mult),
                                        reads=[Bps[1 + o], Bdec], writes=[Bhd])
                            A.op(DVE, lambda e, hd=hd, ab=ab: e.scalar_tensor_tensor(
                                out=ab[:], in0=hd[:].rearrange("p a b c -> p (a b c)"), scalar=-1.0,
                                in1=hd[:].rearrange("p a b c -> p (a b c)"), op0=ALU.mult, op1=ALU.max),
                                reads=[Bhd], writes=[Bab])
                            for o in range(2):
                                A.op(PE, lambda e, o=o, j=j, ab=ab: e.matmul(ps[0:1, 3 + o, :], lhsT=ones_b[:, 0:1], rhs=ab[:, o * 512:(o + 1) * 512],
                                                                             start=(j == 0), stop=(j == nt - 1)),
                                     reads=[Bones, Bab], writes=[Bps[3 + o]])
                            A.op(POOL, lambda e, hd=hd, j=j: e.tensor_tensor(
                                out=Ptm[:, j, :].rearrange("p (o c) -> p o c", o=2), in0=hd[:, :, 0, :], in1=hd[:, :, 1, :], op=ALU.add),
                                reads=[Bhd], writes=[BP])
                            A.op(POOL, lambda e, hd=hd, j=j: e.tensor_tensor(
                                out=Qtm[:, j, :].rearrange("p (o c) -> p o c", o=2), in0=hd[:, :, 0, :], in1=hd[:, :, 1, :], op=ALU.subtract),
                                reads=[Bhd], writes=[BQ])
                            if j == 0:
                                for dst, Bd in ((Ptm, BP), (Qtm, BQ)):
                                    A.op(POOL, lambda e, hd=hd, dst=dst: e.tensor_copy(
                                        out=dst[0:1, 0, :].rearrange("p (o c) -> p o c", o=2), in_=hd[0:1, :, 0, :]),
                                        reads=[Bhd], writes=[Bd])
                    nrm, Bnrm = TF("nrm", [1, 2, 256], F32)
                    invn, Binv = TF("invn_bc", [128, 512], F32)
                    dbc, Bdbc = TF("d_bc", [128, 512], F32)
                    for o in range(2):
                        A.op(DVE, lambda e, o=o: e.tensor_copy(out=nrm[:, o, :], in_=ps[0:1, 3 + o, 0:256]), reads=[Bps[3 + o]], writes=[Bnrm])
                        A.op(DVE, lambda e, o=o: e.tensor_tensor(out=nrm[:, o, :], in0=nrm[:, o, :], in1=ps[0:1, 3 + o, 256:512], op=ALU.add),
                             reads=[Bps[3 + o], Bnrm], writes=[Bnrm])
                    A.op(DVE, lambda e: e.reciprocal(out=nrm[:], in_=nrm[:]), reads=[Bnrm], writes=[Bnrm])
                    isl = 0 if L == S else 1
                    A.dma(SP, lambda e: e.dma_start(out=invn_d[isl:isl + 1, :], in_=nrm[:].rearrange("p a b -> p (a b)")),
                          reads=[Bnrm], writes=[Binvn])
                    A.dma(SP, lambda e: e.dma_start(out=invn[:], in_=invn_d[isl].partition_broadcast(128)), reads=[Binvn], writes=[Binv])
                    A.dma(SP, lambda e: e.dma_start(out=dbc[:], in_=I["hy_d"][l].rearrange("a b -> (a b)").partition_broadcast(128)),
                          writes=[Bdbc])
                    for ft in range(nt):
                        sl, Bsl = load_slab(fwd_d, ft)
                        sbk = 4 + (ft % 2) * 2
                        for cs, (src, Bsrc) in enumerate(((Ptm, BP), (Qtm, BQ))):
                            bank = sbk + cs
                            for st_ in range(nt):
                                A.op(PE, lambda e, cs=cs, st_=st_, sl=sl, src=src, bank=bank: e.matmul(
                                    ps[:, bank, :], lhsT=sl[:, cs, st_, :], rhs=src[:, st_, :], start=(st_ == 0), stop=(st_ == nt - 1)),
                                    reads=[Bsl, Bsrc], writes=[Bps[bank]])
                        A.op(DVE, lambda e, ft=ft, sbk=sbk: e.tensor_tensor(out=Ksp[:, ft, 1, :], in0=ps[:, sbk + 1, :], in1=invn[:], op=ALU.mult),
                             reads=[Bps[sbk + 1], Binv], writes=[BK])
                        krt, Bkrt = hd_r[ft % 2]
                        krv = krt[:].rearrange("p a b c -> p (a b c)")[:, 0:512]
                        A.op(DVE, lambda e, krv=krv, sbk=sbk: e.tensor_tensor(out=krv, in0=ps[:, sbk, :], in1=invn[:], op=ALU.mult),
                             reads=[Bps[sbk], Binv], writes=[Bkrt])
                        A.op(POOL, lambda e, krv=krv, ft=ft: e.tensor_tensor(out=Ksp[:, ft, 0, :], in0=krv, in1=dbc[:], op=ALU.add),
                             reads=[Bkrt, Bdbc], writes=[BK])
                    A.barrier()

                tm3, Btm3 = T("tm3", [128, nt, 768], BF16)
                with ExitStack() as stks:
                    def TS(name, shape, dt):
                        return stks.enter_context(_sbuf_unique(name, shape, dt)), Buf(name)
                    cw, Bcw = TS("cw", [128, 18], F32)
                    cb, Bcb = TS("cb", [128, 6], F32)
                    load_cols(TS, cw[:], Bcw, I["hy_conv_w"][l].rearrange("a b -> (a b)"), 18)
                    load_cols(TS, cb[:], Bcb, I["hy_conv_b"][l], 6)
                    W = min(L, 1024)
                    zin_r = [TS(f"zin{i}", [128, W + 2], F32) for i in range(2)]
                    zt_r = [TS(f"zt{i}", [128, W], F32) for i in range(2)]
                    zc_r = [TS(f"zc{i}", [128, W], BF16) for i in range(2)]
                    ctr = 0
                    for hc in range(6):
                        for b0 in range(0, L, W):
                            zin, Bzin = zin_r[ctr % 2]
                            zt, Bzt = zt_r[ctr % 2]
                            zc, Bzc = zc_r[ctr % 2]
                            ctr += 1
                            lo = b0 - 1
                            hi = b0 + W + 1
                            clo = max(lo, 0)
                            chi = min(hi, L)
                            if lo < 0:
                                A.op(DVE, lambda e, zin=zin: e.memset(zin[:, 0:1], 0.0), writes=[Bzin])
                            if hi > L:
                                A.op(DVE, lambda e, zin=zin: e.memset(zin[:, W + 1:W + 2], 0.0), writes=[Bzin])
                            A.dma(SP, lambda e, zin=zin, hc=hc, clo=clo, chi=chi, lo=lo: e.dma_start(
                                out=zin[:, clo - lo:chi - lo], in_=hyraw[hc, :, seq_t0 + clo:seq_t0 + chi]),
                                reads=[Bhyraw], writes=[Bzin])
                            A.op(DVE, lambda e, zin=zin, zt=zt, hc=hc: e.tensor_scalar(
                                out=zt[:], in0=zin[:, 1:W + 1], scalar1=cw[:, 6 + hc:7 + hc], scalar2=cb[:, hc:hc + 1],
                                op0=ALU.mult, op1=ALU.add), reads=[Bzin, Bcw, Bcb], writes=[Bzt])
                            A.op(DVE, lambda e, zin=zin, zt=zt, hc=hc: e.scalar_tensor_tensor(
                                out=zt[:], in0=zin[:, 0:W], scalar=cw[:, hc:hc + 1], in1=zt[:], op0=ALU.mult, op1=ALU.add),
                                reads=[Bzin, Bcw, Bzt], writes=[Bzt])
                            A.op(DVE, lambda e, zin=zin, zt=zt, zc=zc, hc=hc: e.scalar_tensor_tensor(
                                out=zc[:], in0=zin[:, 2:W + 2], scalar=cw[:, 12 + hc:13 + hc], in1=zt[:], op0=ALU.mult, op1=ALU.add),
                                reads=[Bzin, Bcw, Bzt], writes=[Bzc])
                            for t4 in range(0, W // 128, 4):
                                n4 = min(4, W // 128 - t4)
                                bank = (t4 // 4) % 2
                                for q in range(n4):
                                    A.op(PE, lambda e, zc=zc, t4=t4, q=q, bank=bank: e.transpose(
                                        out=psb(bank)[:, q * 128:(q + 1) * 128], in_=zc[:, (t4 + q) * 128:(t4 + q + 1) * 128],
                                        identity=identb[:]), reads=[Bzc, Bidb], writes=[Bps[bank]])
                                tile0 = b0 // 128 + t4
                                A.op(ACT, lambda e, tile0=tile0, n4=n4, bank=bank, hc=hc: e.copy(
                                    out=tm3[:, tile0:tile0 + n4, hc * 128:(hc + 1) * 128],
                                    in_=psb(bank)[:, 0:n4 * 128].rearrange("p (a c) -> p a c", c=128)),
                                    reads=[Bps[bank]], writes=[Btm3])
                    A.barrier()

                with ExitStack() as stkc:
                    def TC(name, shape, dt):
                        return stkc.enter_context(_sbuf_unique(name, shape, dt)), Buf(name)
                    Ysb, BY = TC("Ysb", [128, nt, 2, 256], BF16)
                    z1, Bz1 = TC("z1", [128, nt, 256], BF16)
                    tA = [TC(f"tA{i}", [128, 256], F32) for i in range(4)]
                    yo_r = [TC(f"yo{i}", [128, 4, 256], BF16) for i in range(2)]
                    cy_r = [TC(f"cy{i}", [128, 2, 512], BF16) for i in range(2)]
                    for o in range(2):
                        for ft in range(nt):
                            sl, Bsl = load_slab(fwd_d, ft)
                            fb0 = (ft % 2) * 2
                            for cs in range(2):
                                bank = fb0 + cs
                                for st_ in range(nt):
                                    rhs = tm3[:, st_, 0:256] if o == 0 else z1[:, st_, :]
                                    A.op(PE, lambda e, cs=cs, st_=st_, sl=sl, rhs=rhs, bank=bank: e.matmul(
                                        ps[:, bank, 0:256], lhsT=sl[:, cs, st_, :], rhs=rhs, start=(st_ == 0), stop=(st_ == nt - 1)),
                                        reads=[Bsl, Btm3 if o == 0 else Bz1], writes=[Bps[bank]])
                            Kr = Ksp[:, ft, 0, o * 256:(o + 1) * 256]
                            Ki = Ksp[:, ft, 1, o * 256:(o + 1) * 256]
                            for i4, (zb, kk) in enumerate(((fb0, Kr), (fb0 + 1, Ki), (fb0, Ki), (fb0 + 1, Kr))):
                                A.op(DVE, lambda e, i4=i4, zb=zb, kk=kk: e.tensor_tensor(out=tA[i4][0][:], in0=ps[:, zb, 0:256], in1=kk, op=ALU.mult),
                                     reads=[Bps[zb], BK], writes=[tA[i4][1]])
                            A.op(POOL, lambda e, ft=ft: e.tensor_tensor(out=Ysb[:, ft, 0, :], in0=tA[0][0][:], in1=tA[1][0][:], op=ALU.subtract),
                                 reads=[tA[0][1], tA[1][1]], writes=[BY])
                            A.op(POOL, lambda e, ft=ft: e.tensor_tensor(out=Ysb[:, ft, 1, :], in0=tA[2][0][:], in1=tA[3][0][:], op=ALU.add),
                                 reads=[tA[2][1], tA[3][1]], writes=[BY])
                        for tt in range(nt):
                            sl, Bsl = load_slab(inv_d, tt)
                            bank = 4 + tt % 2
                            n_mm = 2 * nt
                            i_mm = 0
                            for cs in range(2):
                                for ft in range(nt):
                                    A.op(PE, lambda e, cs=cs, ft=ft, sl=sl, bank=bank, i_mm=i_mm: e.matmul(
                                        ps[:, bank, 0:256], lhsT=sl[:, cs, ft, :], rhs=Ysb[:, ft, cs, :],
                                        start=(i_mm == 0), stop=(i_mm == n_mm - 1)), reads=[Bsl, BY], writes=[Bps[bank]])
                                    i_mm += 1
                            if o == 0:
                                A.op(DVE, lambda e, tt=tt, bank=bank: e.tensor_tensor(
                                    out=z1[:, tt, :], in0=ps[:, bank, 0:256], in1=tm3[:, tt, 256:512], op=ALU.mult),
                                    reads=[Bps[bank], Btm3], writes=[Bz1])
                            else:
                                yo, Byo = yo_r[(tt // 4) % 2]
                                A.op(DVE, lambda e, tt=tt, bank=bank, yo=yo: e.tensor_tensor(
                                    out=yo[:, tt % 4, :], in0=ps[:, bank, 0:256], in1=tm3[:, tt, 512:768], op=ALU.mult),
                                    reads=[Bps[bank], Btm3], writes=[Byo])
                                if tt % 4 == 3 or tt == nt - 1:
                                    n4 = tt % 4 + 1
                                    cy, Bcy = cy_r[(tt // 4) % 2]
                                    for q in range(n4):
                                        for a in range(2):
                                            A.op(PE, lambda e, q=q, a=a, yo=yo: e.transpose(
                                                out=psb(6 + a)[:, q * 128:(q + 1) * 128], in_=yo[:, q, a * 128:(a + 1) * 128],
                                                identity=identb[:]), reads=[Byo, Bidb], writes=[Bps[6 + a]])
                                    for a in range(2):
                                        A.op(ACT, lambda e, a=a, cy=cy, n4=n4: e.copy(out=cy[:, a, 0:n4 * 128], in_=psb(6 + a)[:, 0:n4 * 128]),
                                             reads=[Bps[6 + a]], writes=[Bcy])
                                    tok0 = seq_t0 + (tt - n4 + 1) * 128
                                    bi = min(tok0 // 512, 8)
                                    A.dma(SP, lambda e, cy=cy, tok0=tok0, n4=n4: e.dma_start(
                                        out=catT[4:6, :, tok0:tok0 + n4 * 128].rearrange("k p t -> p k t"), in_=cy[:, :, 0:n4 * 128]),
                                        reads=[Bcy], writes=[Bcat[bi]])
                    A.barrier()

        hyena(0, S, I["k_fwd"], I["k_inv"], I["k_feats"], I["k_decay"])
        if not last:
            hyena(S, CTX, I["k_fwdc"], I["k_invc"], I["k_featsc"], I["k_decayc"])
        if dbg:
            A.dma(SP, lambda e: e.dma_start(out=DBG["cat"], in_=catT), reads=Bcat, writes=[Bdbg])
            A.dma(SP, lambda e: e.dma_start(out=DBG["hy"], in_=hyraw), reads=[Bhyraw], writes=[Bdbg])
            A.barrier()

        def load_bc(T, name, src_vec):
            t, Bt = T(name, [128, D], F32)
            A.dma(SP, lambda e: e.dma_start(out=t[:], in_=src_vec.partition_broadcast(128)), reads=[Bgate], writes=[Bt])
            return t, Bt

        def deepnorm_tile(T_, stt_, psbanks, gate_bc, Bgate_bc, xin, Bxin, g_bc, Bg, b_bc, Bb, yt, Byt, xo, Bxo):
            b0 = psbanks
            A.op(DVE, lambda e: e.tensor_tensor(out=yt[:].rearrange("p (a c) -> p a c", a=2), in0=ps[:, b0:b0 + 2, :],
                                                in1=gate_bc[:].rearrange("p (a c) -> p a c", a=2), op=ALU.mult),
                 reads=[Bps[b0], Bps[b0 + 1], Bgate_bc], writes=[Byt])
            A.op(POOL, lambda e: e.tensor_tensor(out=yt[:], in0=yt[:], in1=xin[:], op=ALU.add), reads=[Byt, Bxin], writes=[Byt])
            ln_compute(stt_, yt, Byt, LN_EPS / (ALPHA * ALPHA))
            A.op(DVE, lambda e: e.tensor_scalar(out=yt[:], in0=yt[:], scalar1=stt_[2][:, 0:1], scalar2=stt_[4][:],
                                                op0=ALU.subtract, op1=ALU.mult), reads=[Byt, stt_[3], stt_[5]], writes=[Byt])
            A.op(POOL, lambda e: e.tensor_tensor(out=yt[:], in0=yt[:], in1=g_bc[:], op=ALU.mult), reads=[Byt, Bg], writes=[Byt])
            A.op(POOL, lambda e: e.tensor_tensor(out=xo[:], in0=yt[:], in1=b_bc[:], op=ALU.add), reads=[Byt, Bb], writes=[Bxo])

        with ExitStack() as stk:
            def T(name, shape, dt):
                return stk.enter_context(_sbuf_unique(name, shape, dt)), Buf(name)
            wo, Bwo = T("wo", [128, 8, D], BF16)
            A.dma(POOL, lambda e: e.dma_start(out=wo[:], in_=I["w_out"][l].rearrange("(k p) n -> p k n", p=128)), writes=[Bwo])
            g1l = load_bc(T, "g1l", gate_d[l, 0])
            g1c = load_bc(T, "g1c", gate_d[l, 1])
            lg = load_bc(T, "lg", I["ln1_g"][l])
            lb = load_bc(T, "lb", I["ln1_b"][l])
            if moe:
                rw, Brw = T("rw", [128, 8, NEXP], BF16)
                A.dma(POOL, lambda e: e.dma_start(out=rw[:], in_=I["router_w"][jf].rearrange("(k p) n -> p k n", p=128)), writes=[Brw])
            cat_r = [T(f"catb{i}", [128, 8, 512], BF16) for i in range(2)]
            xin_r = [T(f"xin{i}", [128, D], F32) for i in range(4)]
            yt_r = [T(f"yt{i}", [128, D], F32) for i in range(4)]
            x1_r = [T(f"x1_{i}", [128, D], F32) for i in range(4)]
            nmr_r = [T(f"nmr5_{i}", [128, 2], F32) for i in range(4)]
            uh_r = [T(f"uh5_{i}", [128, D], BF16) for i in range(8)]
            uT_r = [T(f"uT5_{i}", [128, 8, 512], BF16) for i in range(2)]
            stt = [ln_stats(T, None, None, None, f"p5{i}") for i in range(8)]
            tile_ctr = 0
            for bi, (t0, nti) in enumerate(BLOCKS[:nblk]):
                is_ctx = t0 >= S
                jm = 1 if is_ctx else 0
                n = nti * 128
                cb_, Bcb_ = cat_r[bi % 2]
                A.dma(SP, lambda e, cb_=cb_, t0=t0, n=n: e.dma_start(
                    out=cb_[:, :, 0:n], in_=catT[:, :, t0:t0 + n].rearrange("k p t -> p k t")), reads=[Bcat[bi]], writes=[Bcb_])
                uT, BuT = uT_r[bi % 2]
                gt = g1c if is_ctx else g1l
                for ti in range(nti):
                    g = tile_ctr + ti
                    tok = t0 + ti * 128
                    xin, Bxin = xin_r[g % 4]
                    yt, Byt = yt_r[g % 4]
                    pb = 4 + 2 * (ti % 2)
                    A.dma(SP, lambda e, xin=xin, tok=tok: e.dma_start(out=xin[:], in_=xsrc(tok, 128)), reads=[Bxres[tok // 128]], writes=[Bxin])
                    for nb in range(2):
                        for k in range(8):
                            A.op(PE, lambda e, nb=nb, k=k, cb_=cb_, ti=ti, pb=pb: e.matmul(
                                ps[:, pb + nb, :], lhsT=cb_[:, k, ti * 128:(ti + 1) * 128], rhs=wo[:, k, nb * 512:(nb + 1) * 512],
                                start=(k == 0), stop=(k == 7)), reads=[Bcb_, Bwo], writes=[Bps[pb + nb]])
                    A.op(DVE, lambda e, yt=yt, pb=pb, gt=gt: e.tensor_tensor(
                        out=yt[:].rearrange("p (a c) -> p a c", a=2), in0=ps[:, pb:pb + 2, :],
                        in1=gt[0][:].rearrange("p (a c) -> p a c", a=2), op=ALU.mult),
                        reads=[Bps[pb], Bps[pb + 1], gt[1]], writes=[Byt])
                    A.op(POOL, lambda e, yt=yt, xin=xin: e.tensor_tensor(out=yt[:], in0=yt[:], in1=xin[:], op=ALU.add),
                         reads=[Byt, Bxin], writes=[Byt])
                for ti in range(nti):
                    g = tile_ctr + ti
                    yt, Byt = yt_r[g % 4]
                    s_ = stt[g % 4]
                    nmr, Bnmr = nmr_r[g % 4]
                    ln_compute(s_, yt, Byt, LN_EPS / (ALPHA * ALPHA))
                    A.op(DVE, lambda e, nmr=nmr, s_=s_: e.tensor_scalar(
                        out=nmr[:, 0:1], in0=s_[2][:, 0:1], scalar1=s_[4][:], scalar2=-1.0, op0=ALU.mult, op1=ALU.mult),
                        reads=[s_[3], s_[5]], writes=[Bnmr])
                    A.op(ACT, lambda e, yt=yt, s_=s_, nmr=nmr: e.activation(
                        out=yt[:], in_=yt[:], func=AF.Identity, scale=s_[4][:], bias=nmr[:, 0:1]),
                        reads=[Byt, s_[5], Bnmr], writes=[Byt])
                for ti in range(nti):
                    g = tile_ctr + ti
                    tok = t0 + ti * 128
                    yt, Byt = yt_r[g % 4]
                    x1, Bx1 = x1_r[g % 4]
                    A.op(DVE, lambda e, yt=yt: e.tensor_tensor(out=yt[:], in0=yt[:], in1=lg[0][:], op=ALU.mult), reads=[Byt, lg[1]], writes=[Byt])
                    A.op(POOL, lambda e, yt=yt, x1=x1: e.tensor_tensor(out=x1[:], in0=yt[:], in1=lb[0][:], op=ALU.add), reads=[Byt, lb[1]], writes=[Bx1])
                    A.dma(SP, lambda e, x1=x1, tok=tok: e.dma_start(out=xres[tok:tok + 128, :], in_=x1[:]), reads=[Bx1], writes=[Bxres[tok // 128]])
                    if dbg:
                        A.dma(SP, lambda e, x1=x1, tok=tok: e.dma_start(out=DBG["x1"][tok:tok + 128, :], in_=x1[:]), reads=[Bx1], writes=[Bdbg])
                uhs = []
                for ti in range(nti):
                    g = tile_ctr + ti
                    x1, Bx1 = x1_r[g % 4]
                    uh, Buh = uh_r[g % 8]
                    s_ = stt[4 + g % 4]
                    nmr, Bnmr = nmr_r[g % 4]
                    ln_compute(s_, x1, Bx1, ADA_EPS)
                    A.op(DVE, lambda e, nmr=nmr, s_=s_: e.tensor_scalar(
                        out=nmr[:, 1:2], in0=s_[2][:, 0:1], scalar1=s_[4][:], scalar2=-1.0, op0=ALU.mult, op1=ALU.mult),
                        reads=[s_[3], s_[5]], writes=[Bnmr])
                    A.op(ACT, lambda e, x1=x1, uh=uh, s_=s_, nmr=nmr: e.activation(
                        out=uh[:], in_=x1[:], func=AF.Identity, scale=s_[4][:], bias=nmr[:, 1:2]),
                        reads=[Bx1, s_[5], Bnmr], writes=[Buh])
                    uhs.append((uh, Buh))
                modulate_block(uhs, nti, l, jm, 24, uT, BuT)
                A.dma(SP, lambda e, uT=uT, t0=t0, n=n: e.dma_start(
                    out=uT_d[:, :, t0:t0 + n].rearrange("k p t -> p k t"), in_=uT[:, :, 0:n]), reads=[BuT], writes=[BuTd[bi]])
                tile_ctr += nti
            A.barrier()

        with ExitStack() as stk:
            def T(name, shape, dt):
                return stk.enter_context(_sbuf_unique(name, shape, dt)), Buf(name)
            NQ = 7
            if moe:
                pieces = [(ex_, q * 7, 7) for ex_ in range(NEXP) for q in range(4)]
                wsrc = lambda ex_: (I["moe_w_gate"][jf, ex_], I["moe_w_up"][jf, ex_], I["moe_w_down"][jf, ex_])
            else:
                pieces = [(0, 0, 6), (0, 6, 6), (0, 12, 5), (0, 17, 5)]
                wsrc = lambda ex_: (I["ffn_w_gate"][jf], I["ffn_w_up"][jf], I["ffn_w_down"][jf])
            comb, Bcomb = T("comb", [128, NT, NEXP], F32)
            if moe:
                with ExitStack() as stkr:
                    def TR(name, shape, dt):
                        return stkr.enter_context(_sbuf_unique(name, shape, dt)), Buf(name)
                    rw, Brw = TR("rw", [128, 8, NEXP], BF16)
                    A.dma(POOL, lambda e: e.dma_start(out=rw[:], in_=I["router_w"][jf].rearrange("(k p) n -> p k n", p=128)), writes=[Brw])
                    uT_r = [TR(f"uTr{i}", [128, 8, 512], BF16) for i in range(2)]
                    lg_r = [TR(f"lgt{i}", [128, 8], F32) for i in range(2)]
                    m8_r = [TR(f"m8{i}", [128, 8], F32) for i in range(2)]
                    gg_r = [TR(f"gg{i}", [128, 4], F32) for i in range(2)]
                    eq_r = [TR(f"eq{i}", [128, 8], F32) for i in range(2)]
                    tile_ctr = 0
                    for bi, (t0, nti) in enumerate(BLOCKS[:nblk]):
                        n = nti * 128
                        uT, BuT = uT_r[bi % 2]
                        A.dma(SP, lambda e, uT=uT, t0=t0, n=n: e.dma_start(
                            out=uT[:, :, 0:n], in_=uT_d[:, :, t0:t0 + n].rearrange("k p t -> p k t")), reads=[BuTd[bi]], writes=[BuT])
                        for ti in range(nti):
                            g = tile_ctr + ti
                            bank = g % 2
                            for k in range(8):
                                A.op(PE, lambda e, k=k, uT=uT, ti=ti, bank=bank: e.matmul(
                                    ps[:, bank, 0:NEXP], lhsT=uT[:, k, ti * 128:(ti + 1) * 128], rhs=rw[:, k, :],
                                    start=(k == 0), stop=(k == 7)), reads=[BuT, Brw], writes=[Bps[bank]])
                            lgt, Blg = lg_r[g % 2]
                            m8, Bm8 = m8_r[g % 2]
                            gg, Bgg = gg_r[g % 2]
                            eq, Beq = eq_r[g % 2]
                            A.op(DVE, lambda e, lgt=lgt, bank=bank: e.tensor_copy(out=lgt[:], in_=ps[:, bank, 0:NEXP]), reads=[Bps[bank]], writes=[Blg])
                            A.op(DVE, lambda e, lgt=lgt, m8=m8: e.max(out=m8[:], in_=lgt[:]), reads=[Blg], writes=[Bm8])
                            A.op(DVE, lambda e, m8=m8, gg=gg: e.tensor_tensor(out=gg[:, 0:1], in0=m8[:, 1:2], in1=m8[:, 0:1], op=ALU.subtract),
                                 reads=[Bm8], writes=[Bgg])
                            A.op(ACT, lambda e, gg=gg: e.activation(out=gg[:, 1:2], in_=gg[:, 0:1], func=AF.Exp), reads=[Bgg], writes=[Bgg])
                            A.op(DVE, lambda e, gg=gg: e.tensor_scalar(out=gg[:, 2:3], in0=gg[:, 1:2], scalar1=1.0, scalar2=None, op0=ALU.add),
                                 reads=[Bgg], writes=[Bgg])
                            A.op(DVE, lambda e, gg=gg: e.reciprocal(out=gg[:, 2:3], in_=gg[:, 2:3]), reads=[Bgg], writes=[Bgg])
                            A.op(DVE, lambda e, gg=gg: e.tensor_tensor(out=gg[:, 3:4], in0=gg[:, 1:2], in1=gg[:, 2:3], op=ALU.mult),
                                 reads=[Bgg], writes=[Bgg])
                            A.op(DVE, lambda e, lgt=lgt, m8=m8, gg=gg, eq=eq: e.tensor_scalar(
                                out=eq[:], in0=lgt[:], scalar1=m8[:, 0:1], scalar2=gg[:, 2:3], op0=ALU.is_equal, op1=ALU.mult),
                                reads=[Blg, Bm8, Bgg], writes=[Beq])
                            tg = t0 // 128 + ti
                            A.op(DVE, lambda e, lgt=lgt, m8=m8, gg=gg, tg=tg: e.tensor_scalar(
                                out=comb[:, tg, :], in0=lgt[:], scalar1=m8[:, 1:2], scalar2=gg[:, 3:4], op0=ALU.is_equal, op1=ALU.mult),
                                reads=[Blg, Bm8, Bgg], writes=[Bcomb])
                            A.op(DVE, lambda e, eq=eq, tg=tg: e.tensor_tensor(out=comb[:, tg, :], in0=comb[:, tg, :], in1=eq[:], op=ALU.add),
                                 reads=[Beq, Bcomb], writes=[Bcomb])
                        tile_ctr += nti
                    A.barrier()
            if last:
                sblocks = [[0, 1, 2], [3, 4, 5], [6, 7]]
            else:
                sblocks = [[0, 1, 2], [3, 4, 5], [6, 7, 8]]
            acc, _ = T("acc", [128, 12, D], F32)
            Bacc = [Buf(f"acc{i}") for i in range(12)]
            w_r = [(T(f"wgq{i}", [128, 8, NQ * 128], BF16), T(f"wuq{i}", [128, 8, NQ * 128], BF16), T(f"wdq{i}", [128, NQ, D], BF16)) for i in range(2)]
            uT_r = [T(f"uT7_{i}", [128, 8, 512], BF16) for i in range(2)]
            aT_r = [T(f"aT7_{i}", [128, NQ, 512], BF16) for i in range(2)]
            sg_r = [T(f"sg7_{i}", [128, 512], BF16) for i in range(2)]
            g2l = load_bc(T, "g2l7", gate_d[l, 2])
            g2c = load_bc(T, "g2c7", gate_d[l, 3])
            lg2 = load_bc(T, "lg27", I["ln2_g"][l])
            lb2 = load_bc(T, "lb27", I["ln2_b"][l])
            xin_r = [T(f"xin7_{i}", [128, D], F32) for i in range(2)]
            yt_r = [T(f"yt7_{i}", [128, D], F32) for i in range(2)]
            xo_r = [T(f"xo7_{i}", [128, D], F32) for i in range(2)]
            nmr_r = [T(f"nmr7_{i}", [128, 1], F32) for i in range(2)]
            stt = [ln_stats(T, None, None, None, f"p7{i}") for i in range(2)]

            def finalize_steps(sbl):
                tiles = []
                at = 0
                for bi in sbl:
                    t0, nti = BLOCKS[bi]
                    for ti in range(nti):
                        tiles.append((at, t0 + ti * 128, g2c if t0 >= S else g2l))
                        at += 1
                steps = []

                def s1(at, tok, g2):
                    xin, Bxin = xin_r[at % 2]
                    yt, Byt = yt_r[at % 2]
                    A.dma(SP, lambda e: e.dma_start(out=xin[:], in_=xres[tok:tok + 128, :]), reads=[Bxres[tok // 128]], writes=[Bxin])
                    A.op(DVE, lambda e: e.tensor_tensor(out=yt[:], in0=acc[:, at, :], in1=g2[0][:], op=ALU.mult),
                         reads=[Bacc[at], g2[1]], writes=[Byt])
                    A.op(POOL, lambda e: e.tensor_tensor(out=yt[:], in0=yt[:], in1=xin[:], op=ALU.add), reads=[Byt, Bxin], writes=[Byt])

                def s2(at, tok, g2):
                    yt, Byt = yt_r[at % 2]
                    stt_ = stt[at % 2]
                    nmr, Bnmr = nmr_r[at % 2]
                    ln_compute(stt_, yt, Byt, LN_EPS / (ALPHA * ALPHA))
                    A.op(DVE, lambda e: e.tensor_scalar(out=nmr[:], in0=stt_[2][:, 0:1], scalar1=stt_[4][:], scalar2=-1.0,
                                                        op0=ALU.mult, op1=ALU.mult), reads=[stt_[3], stt_[5]], writes=[Bnmr])
                    A.op(ACT, lambda e: e.activation(out=yt[:], in_=yt[:], func=AF.Identity, scale=stt_[4][:], bias=nmr[:]),
                         reads=[Byt, stt_[5], Bnmr], writes=[Byt])

                def s3(at, tok, g2):
                    yt, Byt = yt_r[at % 2]
                    xo, Bxo = xo_r[at % 2]
                    A.op(DVE, lambda e: e.tensor_tensor(out=yt[:], in0=yt[:], in1=lg2[0][:], op=ALU.mult), reads=[Byt, lg2[1]], writes=[Byt])
                    A.op(POOL, lambda e: e.tensor_tensor(out=xo[:], in0=yt[:], in1=lb2[0][:], op=ALU.add), reads=[Byt, lb2[1]], writes=[Bxo])
                    if last:
                        A.dma(SP, lambda e: e.dma_start(out=out_d[tok:tok + 128, :], in_=xo[:]), reads=[Bxo], writes=[Bout])
                    else:
                        A.dma(SP, lambda e: e.dma_start(out=xres[tok:tok + 128, :], in_=xo[:]), reads=[Bxo], writes=[Bxres[tok // 128]])
                    if dbg:
                        A.dma(SP, lambda e: e.dma_start(out=DBG["x2"][tok:tok + 128, :], in_=xo[:]), reads=[Bxo], writes=[Bdbg])

                for p0 in range(0, len(tiles), 2):
                    pair = tiles[p0:p0 + 2]
                    for stage in (s1, s2, s3):
                        for tl in pair:
                            steps.append(lambda stage=stage, tl=tl: stage(*tl))
                    while len(steps) % 6:
                        steps.append(lambda: None)
                return steps

            pending = []
            n_drained = [0]

            def drain(n):
                while n > 0 and pending:
                    pending.pop(0)()
                    n_drained[0] += 1
                    n -= 1

            wctr = 0
            bctr = 0
            for sbl in sblocks:
                for pi_, (ex_, ch0, nch) in enumerate(pieces):
                    (wgq, Bwgq), (wuq, Bwuq), (wdq, Bwdq) = w_r[wctr % 2]
                    wctr += 1
                    c0 = ch0 * 128
                    c1 = c0 + nch * 128
                    sg_, su_, sd_ = wsrc(ex_)
                    A.dma(POOL, lambda e, wgq=wgq, sg_=sg_, c0=c0, c1=c1, nch=nch: e.dma_start(
                        out=wgq[:, :, 0:nch * 128], in_=sg_[:, c0:c1].rearrange("(k p) n -> p k n", p=128)), writes=[Bwgq])
                    A.dma(POOL, lambda e, wuq=wuq, su_=su_, c0=c0, c1=c1, nch=nch: e.dma_start(
                        out=wuq[:, :, 0:nch * 128], in_=su_[:, c0:c1].rearrange("(k p) n -> p k n", p=128)), writes=[Bwuq])
                    A.dma(POOL, lambda e, wdq=wdq, sd_=sd_, c0=c0, c1=c1, nch=nch: e.dma_start(
                        out=wdq[:, 0:nch, :], in_=sd_[c0:c1, :].rearrange("(k p) n -> p k n", p=128)), writes=[Bwdq])
                    firstw = (pi_ == 0)
                    at = 0
                    for bi in sbl:
                        t0, nti = BLOCKS[bi]
                        n = nti * 128
                        uT, BuT = uT_r[bctr % 2]
                        aT, BaT = aT_r[bctr % 2]
                        bctr += 1
                        A.dma(SP, lambda e, uT=uT, t0=t0, n=n: e.dma_start(
                            out=uT[:, :, 0:n], in_=uT_d[:, :, t0:t0 + n].rearrange("k p t -> p k t")), reads=[BuTd[bi]], writes=[BuT])
                        for fc in range(nch):
                            gb, ub = (fc % 2) * 2, (fc % 2) * 2 + 1
                            for (wt_, Bwt_, bank) in ((wgq, Bwgq, gb), (wuq, Bwuq, ub)):
                                for k in range(8):
                                    A.op(PE, lambda e, wt_=wt_, k=k, fc=fc, bank=bank, uT=uT, n=n: e.matmul(
                                        ps[:, bank, 0:n], lhsT=wt_[:, k, fc * 128:(fc + 1) * 128], rhs=uT[:, k, 0:n],
                                        start=(k == 0), stop=(k == 7)), reads=[Bwt_, BuT], writes=[Bps[bank]])
                            sg, Bsg = sg_r[fc % 2]
                            A.op(ACT, lambda e, sg=sg, gb=gb, n=n: e.activation(out=sg[:, 0:n], in_=ps[:, gb, 0:n], func=AF.Silu),
                                 reads=[Bps[gb]], writes=[Bsg])
                            A.op(DVE, lambda e, sg=sg, ub=ub, aT=aT, fc=fc, n=n: e.tensor_tensor(
                                out=aT[:, fc, 0:n], in0=ps[:, ub, 0:n], in1=sg[:, 0:n], op=ALU.mult),
                                reads=[Bps[ub], Bsg], writes=[BaT])
                            drain(2)
                        if firstw:
                            drain(6 * ((at + nti - 1) // 2 + 1) - n_drained[0])
                        for ti in range(nti):
                            gtile = t0 // 128 + ti
                            pb = 4 + 2 * (at % 2)
                            for nb in range(2):
                                for fc in range(nch):
                                    A.op(PE, lambda e, nb=nb, fc=fc, aT=aT, ti=ti, pb=pb, wdq=wdq, nch=nch: e.matmul(
                                        ps[:, pb + nb, :], lhsT=aT[:, fc, ti * 128:(ti + 1) * 128], rhs=wdq[:, fc, nb * 512:(nb + 1) * 512],
                                        start=(fc == 0), stop=(fc == nch - 1)), reads=[BaT, Bwdq], writes=[Bps[pb + nb]])
                            av = acc[:, at, :].rearrange("p (a c) -> p a c", a=2)
                            if moe:
                                cw_ = comb[:, gtile, ex_:ex_ + 1]
                                if firstw:
                                    A.op(DVE, lambda e, av=av, pb=pb, cw_=cw_: e.tensor_scalar(
                                        out=av, in0=ps[:, pb:pb + 2, :], scalar1=cw_, scalar2=None, op0=ALU.mult),
                                        reads=[Bps[pb], Bps[pb + 1], Bcomb], writes=[Bacc[at]])
                                else:
                                    A.op(DVE, lambda e, av=av, pb=pb, cw_=cw_: e.scalar_tensor_tensor(
                                        out=av, in0=ps[:, pb:pb + 2, :], scalar=cw_, in1=av, op0=ALU.mult, op1=ALU.add),
                                        reads=[Bps[pb], Bps[pb + 1], Bcomb, Bacc[at]], writes=[Bacc[at]])
                            else:
                                if firstw:
                                    A.op(ACT, lambda e, av=av, pb=pb: e.copy(out=av, in_=ps[:, pb:pb + 2, :]),
                                         reads=[Bps[pb], Bps[pb + 1]], writes=[Bacc[at]])
                                else:
                                    A.op(DVE, lambda e, av=av, pb=pb: e.tensor_tensor(out=av, in0=ps[:, pb:pb + 2, :], in1=av, op=ALU.add),
                                         reads=[Bps[pb], Bps[pb + 1], Bacc[at]], writes=[Bacc[at]])
                            at += 1
                drain(len(pending))
                pending.extend(finalize_steps(sbl))
                n_drained[0] = 0
            drain(len(pending))
            A.barrier()


    for l in range(n_layers):
        layer(l)

    A.barrier()
    A.emit()
    glob.close()
    return nc


_NC_CACHE = {}


def _in_maps(inputs):
    cst = _consts()
    maps = []
    shared = {}
    for k in INPUT_SHAPES:
        if k in ("x", "c", "ctx"):
            continue
        shared[k] = np.ascontiguousarray(np.asarray(inputs[k], dtype=np.float32))
    for b in range(8):
        m = dict(shared)
        m["x"] = np.ascontiguousarray(np.asarray(inputs["x"][b], dtype=np.float32))
        m["c"] = np.ascontiguousarray(np.asarray(inputs["c"][b], dtype=np.float32))
        m["ctx"] = np.ascontiguousarray(np.asarray(inputs["ctx"][b], dtype=np.float32))
        m.update(cst)
        maps.append(m)
    return maps


def kernel(**inputs):
    if "nc" not in _NC_CACHE:
        _NC_CACHE["nc"] = build()
    nc = _NC_CACHE["nc"]
    res = run_bass_kernel_spmd(nc, _in_maps(inputs), core_ids=list(range(8)))
    return np.stack([np.asarray(r["out"], dtype=np.float32) for r in res.results], axis=0)
```

```python
import math
from contextlib import ExitStack
import numpy as np
import ml_dtypes
import concourse.bass as bass
import concourse.mybir as mybir
from concourse.bass_utils import run_bass_kernel_spmd

F32 = mybir.dt.float32
BF16 = mybir.dt.bfloat16
AF = mybir.ActivationFunctionType
ALU = mybir.AluOpType
AX = mybir.AxisListType
NPBF = ml_dtypes.bfloat16

PE, ACT, DVE, POOL, SP = "pe", "act", "dve", "pool", "sp"
ENGINES = (PE, ACT, DVE, POOL, SP)

D = 1024
S = 4096
CTX = 256
TOK = S + CTX
NT = TOK // 128
DEPTH = 4
D_IN = 1792
D_FF = 2816
D_FFE = 3584
NEXP = 8
ALPHA = (2.0 * DEPTH) ** 0.25
LN_EPS = 1e-5
ADA_EPS = 1e-6
QK_EPS = 1e-6
BLOCKS = [(i * 512, 4) for i in range(8)] + [(4096, 2)]
MAGIC = 12582912.0
TWO_PI = 2.0 * math.pi


class Buf:
    __slots__ = ("name", "w", "r")

    def __init__(self, name=""):
        self.name = name
        self.w = None
        self.r = {}


class AutoSync:
    def __init__(self, nc, ring_sizes=None):
        self.nc = nc
        self.q = {e: [] for e in ENGINES}
        self.cnt = {}
        self.sems = {}
        self.seen = {e: {} for e in ENGINES}
        for e in (PE, ACT, DVE, POOL):
            self._mksem(e)
        ring_sizes = ring_sizes or {SP: 40, POOL: 24, ACT: 8}
        self.ring = {}
        self.ring_pos = {}
        for qn, n in ring_sizes.items():
            keys = []
            for i in range(n):
                k = f"dma_{qn}_{i}"
                self._mksem(k)
                keys.append(k)
            self.ring[qn] = keys
            self.ring_pos[qn] = 0
        self.n_instr = 0

    def _mksem(self, key):
        self.sems[key] = self.nc.alloc_semaphore(f"s_{key}")
        self.cnt[key] = 0

    def _collect(self, eng, reads, writes, skip_self):
        need = {}

        def add(d):
            if d is None:
                return
            k, v = d
            if skip_self and k == eng:
                return
            if need.get(k, 0) < v:
                need[k] = v
        for b in reads:
            add(b.w)
        for b in writes:
            add(b.w)
            for k, v in b.r.items():
                add((k, v))
        seen = self.seen[eng]
        for k, v in need.items():
            if seen.get(k, 0) < v:
                seen[k] = v
                self.q[eng].append(("wait", k, v))

    def _mark(self, me, reads, writes):
        k, v = me
        for b in reads:
            if b.r.get(k, 0) < v:
                b.r[k] = v
        for b in writes:
            b.w = me
            b.r = {}

    def op(self, eng, fn, reads=(), writes=()):
        self._collect(eng, reads, writes, skip_self=(eng == PE))
        self.cnt[eng] += 1
        me = (eng, self.cnt[eng])
        self.q[eng].append(("op", fn, eng, 1))
        self._mark(me, reads, writes)
        self.n_instr += 1

    def dma(self, qn, fn, reads=(), writes=()):
        ring = self.ring[qn]
        key = ring[self.ring_pos[qn]]
        self.ring_pos[qn] = (self.ring_pos[qn] + 1) % len(ring)
        prev = self.cnt[key]
        if prev and self.seen[qn].get(key, 0) < prev:
            self.seen[qn][key] = prev
            self.q[qn].append(("wait", key, prev))
        self._collect(qn, reads, writes, skip_self=False)
        self.cnt[key] += 16
        me = (key, self.cnt[key])
        self.q[qn].append(("op", fn, key, 16))
        self._mark(me, reads, writes)
        self.n_instr += 1

    def barrier(self):
        for e in ENGINES:
            for k, v in self.cnt.items():
                if v and self.seen[e].get(k, 0) < v and not (k == e and e == PE):
                    self.seen[e][k] = v
                    self.q[e].append(("wait", k, v))

    def emit(self):
        nc = self.nc
        sems = self.sems

        def run(q):
            def body(e):
                for it in q:
                    if it[0] == "wait":
                        e.wait_ge(sems[it[1]], it[2])
                    else:
                        it[1](e).then_inc(sems[it[2]], it[3])
            return body

        with nc.Block() as block:
            block.tensor(run(self.q[PE]))
            block.scalar(run(self.q[ACT]))
            block.vector(run(self.q[DVE]))
            block.gpsimd(run(self.q[POOL]))
            block.sync(run(self.q[SP]))


_CONST = None


def _trig_tables(L):
    N = 2 * L
    nt = L // 128
    f = np.arange(L, dtype=np.float64) + 0.5
    s = np.arange(L, dtype=np.float64)
    k = (np.outer(2 * np.arange(L, dtype=np.int64) + 1, np.arange(L, dtype=np.int64))) % (2 * N)
    ang = k.astype(np.float64) * (math.pi / N)
    c = np.cos(ang)
    sn = -np.sin(ang)
    del k, ang
    fwd = np.empty((nt, 128, 2, nt, 128), dtype=NPBF)
    inv = np.empty((nt, 128, 2, nt, 128), dtype=NPBF)
    for i, m in enumerate((c, sn)):
        m4 = m.reshape(nt, 128, nt, 128)
        fwd[:, :, i] = m4.transpose(0, 3, 2, 1).astype(NPBF)
        mi = (m * (2.0 / N)).reshape(nt, 128, nt, 128)
        inv[:, :, i] = mi.transpose(2, 1, 0, 3).astype(NPBF)
    return (np.ascontiguousarray(fwd.reshape(nt, 128, 2 * nt * 128)),
            np.ascontiguousarray(inv.reshape(nt, 128, 2 * nt * 128)))


def _hy_feats(L):
    t = np.linspace(0.0, 1.0, L, dtype=np.float32)[:, None]
    omega = (2.0 * math.pi * np.arange(L, dtype=np.float32)[:, None] / L).astype(np.float32)
    bands = np.linspace(1e-4, 16 - 1, 16, dtype=np.float32)[None, :]
    feats = np.concatenate([t, np.cos(omega * bands), -np.sin(omega * bands)], axis=-1).astype(np.float32)
    max_decay = math.log(1e-2) / 0.3
    min_decay = math.log(1e-2) / 1.5
    deltas = np.linspace(min_decay, max_decay, 256, dtype=np.float32)
    decay = np.exp(-t * np.abs(deltas)[None, :]).astype(np.float32)
    nt = L // 128
    decay_t = np.ascontiguousarray(decay.reshape(nt, 128, 256).transpose(1, 0, 2))
    return np.ascontiguousarray(feats.T), decay_t


def _pool_band():
    out = np.zeros((128, 20, 128), dtype=np.float64)
    L = 1024
    for g, win in enumerate((2, 4, 8, 16)):
        A = np.zeros((L, L))
        for t in range(L):
            lo = max(t - win // 2, 0)
            hi = min(t + win - win // 2, L)
            A[t, lo:hi] = 1.0 / (hi - lo)
            A[t, t] -= 1.0
        last = L - 128
        out[:, g * 5 + 0, :] = A[0:128, 0:128].T
        out[:, g * 5 + 1, :] = A[256:384, 256:384].T
        out[:, g * 5 + 2, :] = A[last:, last:].T
        out[:, g * 5 + 3, :] = A[256:384, 128:256].T
        out[:, g * 5 + 4, :] = A[256:384, 384:512].T
    return out.astype(NPBF)


def _rope_tables():
    rows = S // 64
    row = np.repeat(np.arange(rows), 64).astype(np.float32)
    col = np.tile(np.arange(64), rows).astype(np.float32)
    inv = (10000.0 ** (-np.arange(0, 32, 2, dtype=np.float32) / 32)).astype(np.float32)
    ang = np.concatenate([row[:, None] * inv, col[:, None] * inv], axis=-1)
    cos = np.cos(ang).astype(np.float32).reshape(S, 2, 1, 16)
    sin = np.sin(ang).astype(np.float32).reshape(S, 2, 1, 16)
    cosF = np.broadcast_to(cos, (S, 2, 2, 16)).reshape(S, 1, 64)
    sinF = np.concatenate([-sin, sin], axis=2).reshape(S, 1, 64)
    cosF = np.ascontiguousarray(np.broadcast_to(cosF, (S, 10, 64)).reshape(S, 640))
    sinF = np.ascontiguousarray(np.broadcast_to(sinF, (S, 10, 64)).reshape(S, 640))
    return cosF, sinF


def _consts():
    global _CONST
    if _CONST is None:
        fwd, inv = _trig_tables(S)
        fwdc, invc = _trig_tables(CTX)
        featsT, decay_t = _hy_feats(S)
        featsTc, decay_tc = _hy_feats(CTX)
        cosF, sinF = _rope_tables()
        _CONST = dict(
            k_fwd=fwd, k_inv=inv, k_fwdc=fwdc, k_invc=invc,
            k_feats=featsT, k_decay=decay_t, k_featsc=featsTc, k_decayc=decay_tc,
            k_cosF=cosF, k_sinF=sinF, k_band=_pool_band(),
            k_identb=np.eye(128).astype(NPBF), k_identf=np.eye(128, dtype=np.float32),
        )
    return _CONST


INPUT_SHAPES = dict(
    x=[S, D], c=[D], ctx=[CTX, D], c_ctx=[D], w_mod=[DEPTH, D, 6 * D], b_mod=[DEPTH, 6 * D],
    w_in=[DEPTH, D, D_IN], q_gain=[DEPTH, 64], k_gain=[DEPTH, 64], hy_conv_w=[DEPTH, 3, 768],
    hy_conv_b=[DEPTH, 768], hy_f_w1=[DEPTH, 33, 64], hy_f_b1=[DEPTH, 64], hy_f_freq=[DEPTH, 64],
    hy_f_w2=[DEPTH, 64, 64], hy_f_b2=[DEPTH, 64], hy_f_w3=[DEPTH, 64, 1024], hy_d=[DEPTH, 2, 256],
    pool_w=[DEPTH, 4, 64, 64], pool_scale=[DEPTH, 256], w_out=[DEPTH, D, D], ln1_g=[DEPTH, D],
    ln1_b=[DEPTH, D], ln2_g=[DEPTH, D], ln2_b=[DEPTH, D], ffn_w_gate=[2, D, D_FF], ffn_w_up=[2, D, D_FF],
    ffn_w_down=[2, D_FF, D], router_w=[2, D, NEXP], moe_w_gate=[2, NEXP, D, D_FFE],
    moe_w_up=[2, NEXP, D, D_FFE], moe_w_down=[2, NEXP, D_FFE, D],
)


def build(n_layers=DEPTH, dbg=False):
    nc = bass.Bass("TRN2", target_bir_lowering=False)
    A = AutoSync(nc)
    cst = _consts()
    I = {}
    for k, shp in INPUT_SHAPES.items():
        I[k] = nc.dram_tensor(k, shp, F32, kind="ExternalInput").ap()
    for k, v in cst.items():
        I[k] = nc.dram_tensor(k, list(v.shape), BF16 if v.dtype == NPBF else F32, kind="ExternalInput").ap()
    out_d = nc.dram_tensor("out", [S, D], F32, kind="ExternalOutput").ap()
    DBG = {}
    if dbg:
        DBG["cat"] = nc.dram_tensor("dbg_cat", [8, 128, TOK], BF16, kind="ExternalOutput").ap()
        DBG["x1"] = nc.dram_tensor("dbg_x1", [TOK, D], F32, kind="ExternalOutput").ap()
        DBG["x2"] = nc.dram_tensor("dbg_x2", [TOK, D], F32, kind="ExternalOutput").ap()
        DBG["mod"] = nc.dram_tensor("dbg_mod", [128, DEPTH * 96], F32, kind="ExternalOutput").ap()
        DBG["uT"] = nc.dram_tensor("dbg_uT", [8, 128, TOK], BF16, kind="ExternalOutput").ap()
        DBG["hy"] = nc.dram_tensor("dbg_hy", [6, 128, TOK], F32, kind="ExternalOutput").ap()

    xres = nc.dram_tensor("xres", [TOK, D], F32).ap()
    uT_d = nc.dram_tensor("uT_d", [8, 128, TOK], BF16).ap()
    hyraw = nc.dram_tensor("hyraw", [6, 128, TOK], F32).ap()
    catT = nc.dram_tensor("catT", [8, 128, TOK], BF16).ap()
    gate_d = nc.dram_tensor("gate_d", [DEPTH, 4, D], F32).ap()
    invn_d = nc.dram_tensor("invn_d", [2, 512], F32).ap()
    Bxres = [Buf(f"xres{i}") for i in range(NT)]
    BuTd = [Buf(f"uTd{i}") for i in range(9)]
    Bhyraw = Buf("hyraw")
    Bcat = [Buf(f"cat{i}") for i in range(9)]
    Bgate = Buf("gate_d")
    Binvn = Buf("invn")
    Bout = Buf("out")
    Bdbg = Buf("dbg")

    ps = nc.alloc_psum_tensor("ps", [128, 8, 512], F32)
    Bps = [Buf(f"ps{i}") for i in range(8)]

    def psb(bank):
        return ps[:, bank, :].bitcast(BF16)

    glob = ExitStack()
    _uid = [0]
    _orig_sbuf = nc.sbuf_tensor

    def _sbuf_unique(name, shape, dt, **kw):
        _uid[0] += 1
        return _orig_sbuf(f"{name}_{_uid[0]}", shape, dt, **kw)

    def GT(name, shape, dt):
        return glob.enter_context(_sbuf_unique(name, shape, dt)), Buf(name)

    identb, Bidb = GT("identb", [128, 128], BF16)
    identf, Bidf = GT("identf", [128, 128], F32)
    modT, BmodT = GT("modT", [128, DEPTH, 48, 2], F32)
    ones_b, Bones = GT("ones_b", [128, 1], BF16)
    A.dma(SP, lambda e: e.dma_start(out=identb[:], in_=I["k_identb"]), writes=[Bidb])
    A.dma(SP, lambda e: e.dma_start(out=identf[:], in_=I["k_identf"]), writes=[Bidf])
    A.op(DVE, lambda e: e.memset(ones_b[:], 1.0), writes=[Bones])

    def load_cols(T, dst, Bdst, vec, n):
        tmp, Btmp = T("lc_tmp", [n, 128], F32)
        A.dma(SP, lambda e: e.dma_start(out=tmp[:], in_=vec.rearrange("(n p) -> n p", p=128)), writes=[Btmp])
        A.op(PE, lambda e: e.transpose(out=ps[:, 7, 0:n], in_=tmp[:], identity=identf[0:n, 0:n]),
             reads=[Btmp, Bidf], writes=[Bps[7]])
        A.op(DVE, lambda e: e.tensor_copy(out=dst, in_=ps[:, 7, 0:n]), reads=[Bps[7]], writes=[Bdst])

    def ln_stats(T, xt, Bx, eps, tag):
        st, Bst = T(f"st_{tag}", [128, 2, 6], F32)
        mv, Bmv = T(f"mv_{tag}", [128, 2], F32)
        rs, Brs = T(f"rs_{tag}", [128, 1], F32)
        return (st, Bst, mv, Bmv, rs, Brs)

    def ln_compute(stt, xt, Bx, eps):
        st, Bst, mv, Bmv, rs, Brs = stt
        for h in range(2):
            A.op(DVE, lambda e, h=h: e.bn_stats(out=st[:, h, :], in_=xt[:, h * 512:(h + 1) * 512]),
                 reads=[Bx], writes=[Bst])
        A.op(DVE, lambda e: e.bn_aggr(out=mv[:], in_=st[:].rearrange("p a b -> p (a b)")), reads=[Bst], writes=[Bmv])
        A.op(ACT, lambda e: e.activation(out=rs[:], in_=mv[:, 1:2], func=AF.Sqrt, bias=float(eps), scale=1.0),
             reads=[Bmv], writes=[Brs])
        A.op(DVE, lambda e: e.reciprocal(out=rs[:], in_=rs[:]), reads=[Brs], writes=[Brs])

    def modulate_block(uh_tiles, nti, l, j, base, uT, BuT):
        for ti, (uh, Buh) in enumerate(uh_tiles):
            for k in range(8):
                A.op(PE, lambda e, k=k, ti=ti, uh=uh: e.transpose(
                    out=psb(k // 2)[:, (k % 2) * 512 + ti * 128:(k % 2) * 512 + (ti + 1) * 128],
                    in_=uh[:, k * 128:(k + 1) * 128], identity=identb[:]),
                    reads=[Buh, Bidb], writes=[Bps[k // 2]])
        n = nti * 128
        for k in range(8):
            eng = ACT if k % 2 == 0 else DVE
            src = psb(k // 2)[:, (k % 2) * 512:(k % 2) * 512 + n]
            sc = modT[:, l, base + 8 + k, j:j + 1]
            sh = modT[:, l, base + k, j:j + 1]
            if eng == ACT:
                A.op(ACT, lambda e, k=k, src=src, sc=sc, sh=sh: e.activation(
                    out=uT[:, k, 0:n], in_=src, func=AF.Identity, scale=sc, bias=sh),
                    reads=[Bps[k // 2], BmodT], writes=[BuT])
            else:
                A.op(DVE, lambda e, k=k, src=src, sc=sc, sh=sh: e.tensor_scalar(
                    out=uT[:, k, 0:n], in0=src, scalar1=sc, scalar2=sh, op0=ALU.mult, op1=ALU.add),
                    reads=[Bps[k // 2], BmodT], writes=[BuT])

    with ExitStack() as stk:
        def T(name, shape, dt):
            return stk.enter_context(_sbuf_unique(name, shape, dt)), Buf(name)
        cT, BcT = T("cT", [128, 8, 2], F32)
        sT, BsT = T("sT", [128, 8, 2], BF16)
        load_cols(T, cT[:, :, 0], BcT, I["c"], 8)
        load_cols(T, cT[:, :, 1], BcT, I["c_ctx"], 8)
        A.op(ACT, lambda e: e.activation(out=sT[:], in_=cT[:], func=AF.Silu), reads=[BcT], writes=[BsT])
        wm = [T(f"wm{i}", [128, 8, 512], BF16) for i in range(3)]
        bT, BbT = T("bT", [128, 48], F32)
        for l in range(n_layers):
            load_cols(T, bT[:], BbT, I["b_mod"][l], 48)
            for nb in range(12):
                w, Bw = wm[nb % 3]
                A.dma(POOL, lambda e, w=w, l=l, nb=nb: e.dma_start(
                    out=w[:], in_=I["w_mod"][l, :, nb * 512:(nb + 1) * 512].rearrange("(k p) n -> p k n", p=128)),
                    writes=[Bw])
                for jj in range(4):
                    nch = nb * 4 + jj
                    for k in range(8):
                        A.op(PE, lambda e, w=w, k=k, jj=jj, nch=nch: e.matmul(
                            ps[:, 6, nch * 2:nch * 2 + 2], lhsT=w[:, k, jj * 128:(jj + 1) * 128], rhs=sT[:, k, :],
                            start=(k == 0), stop=(k == 7)), reads=[Bw, BsT], writes=[Bps[6]])
            for j in range(2):
                A.op(DVE, lambda e, l=l, j=j: e.tensor_tensor(
                    out=modT[:, l, :, j], in0=ps[:, 6, 0:96].rearrange("p (n t) -> p n t", t=2)[:, :, j],
                    in1=bT[:], op=ALU.add), reads=[Bps[6], BbT], writes=[BmodT])
            for base in (8, 32):
                A.op(DVE, lambda e, l=l, base=base: e.tensor_scalar(
                    out=modT[:, l, base:base + 8, :], in0=modT[:, l, base:base + 8, :], scalar1=1.0, scalar2=None,
                    op0=ALU.add), reads=[BmodT], writes=[BmodT])
            for base in (16, 40):
                A.op(DVE, lambda e, l=l, base=base: e.tensor_scalar(
                    out=modT[:, l, base:base + 8, :], in0=modT[:, l, base:base + 8, :], scalar1=1.0 / ALPHA,
                    scalar2=None, op0=ALU.mult), reads=[BmodT], writes=[BmodT])
            for gi, base in enumerate((16, 40)):
                for j in range(2):
                    A.dma(SP, lambda e, l=l, gi=gi, base=base, j=j: e.dma_start(
                        out=gate_d[l, gi * 2 + j].rearrange("(k p) -> p k", p=128),
                        in_=modT[:, l, base:base + 8, j], allow_slow_non_contiguous=True),
                        reads=[BmodT], writes=[Bgate])
        if dbg:
            A.dma(SP, lambda e: e.dma_start(out=DBG["mod"], in_=modT[:].rearrange("p l n t -> p (l n t)")),
                  reads=[BmodT], writes=[Bdbg])
        A.barrier()

    def layer(l):
        last = (l == DEPTH - 1)
        moe = (l % 2 == 1)
        jf = l // 2
        nblk = 8 if last else 9
        xsrc = (lambda t0, n: (I["x"][t0:t0 + n, :] if t0 < S else I["ctx"][t0 - S:t0 - S + n, :])) if l == 0 \
            else (lambda t0, n: xres[t0:t0 + n, :])

        with ExitStack() as stk:
            def T(name, shape, dt):
                return stk.enter_context(_sbuf_unique(name, shape, dt)), Buf(name)
            qT, BqT = T("qT", [128, 4, TOK], BF16)
            kT, BkT = T("kT", [128, 4, TOK], BF16)
            vp, Bvp = T("vx", [128, NT, 2, 128], BF16)
            pl_tm, Bpl = T("pl_tm", [128, NT, 256], BF16)
            A.op(DVE, lambda e: e.memset(vp[:], 1.0), writes=[Bvp])
            with ExitStack() as stk1:
                def T1(name, shape, dt):
                    return stk1.enter_context(_sbuf_unique(name, shape, dt)), Buf(name)
                wtm, Bwtm = T1("wtm", [128, 8, 1024], BF16)
                whm, Bwhm = T1("whm", [128, 8, 768], BF16)
                wv = I["w_in"][l].rearrange("(k p) n -> p k n", p=128)
                A.dma(POOL, lambda e: e.dma_start(out=wtm[:, :, 0:768], in_=wv[:, :, 0:768]), writes=[Bwtm])
                A.dma(POOL, lambda e: e.dma_start(out=wtm[:, :, 768:1024], in_=wv[:, :, 1536:1792]), writes=[Bwtm])
                A.dma(POOL, lambda e: e.dma_start(out=whm[:], in_=wv[:, :, 768:1536]), writes=[Bwhm])
                gain, Bgain = T1("gain", [128, 640], F32)
                A.dma(SP, lambda e: e.dma_start(out=gain[:, 0:64], in_=I["q_gain"][l].partition_broadcast(128)), writes=[Bgain])
                A.dma(SP, lambda e: e.dma_start(out=gain[:, 512:576], in_=I["k_gain"][l].partition_broadcast(128)), writes=[Bgain])
                A.op(DVE, lambda e: e.tensor_scalar(out=gain[:, 0:64], in0=gain[:, 0:64], scalar1=0.125, scalar2=None,
                                                    op0=ALU.mult), reads=[Bgain], writes=[Bgain])
                for h in range(1, 8):
                    A.op(DVE, lambda e, h=h: e.tensor_copy(out=gain[:, h * 64:(h + 1) * 64], in_=gain[:, 0:64]),
                         reads=[Bgain], writes=[Bgain])
                A.op(DVE, lambda e: e.tensor_copy(out=gain[:, 576:640], in_=gain[:, 512:576]), reads=[Bgain], writes=[Bgain])
                xt_r = [T1(f"xt{i}", [128, D], F32) for i in range(3)]
                uh_r = [T1(f"uh{i}", [128, D], BF16) for i in range(6)]
                uT_r = [T1(f"uTb{i}", [128, 8, 512], BF16) for i in range(2)]
                stt = [ln_stats(T1, None, None, None, f"a{i}") for i in range(2)]
                qk_r = [T1(f"qk{i}", [128, 640], F32) for i in range(2)]
                sq, Bsq = T1("sq", [128, 640], F32)
                ssq, Bssq = T1("ssq", [128, 10], F32)
                cosr = [T1(f"cos{i}", [128, 640], F32) for i in range(2)]
                sinr = [T1(f"sin{i}", [128, 640], F32) for i in range(2)]
                t1, Bt1 = T1("t1", [128, 640], F32)
                t2, Bt2 = T1("t2", [128, 640], F32)
                qkr_r = [T1(f"qkr{i}", [128, 1024], BF16) for i in range(2)]
                for qkr_, Bqkr_ in qkr_r:
                    A.op(POOL, lambda e, qkr_=qkr_: e.memset(qkr_[:, 512:1024], 0.0), writes=[Bqkr_])
                hys_r = [T1(f"hys{i}", [128, 512], F32) for i in range(2)]
                tile_ctr = 0
                blk_g0 = []
                _g = 0
                for (_t0, _n) in BLOCKS:
                    blk_g0.append(_g)
                    _g += _n
                uhs_blk = [[] for _ in BLOCKS]

                def ln_part(bj, tj):
                    t0_, _ = BLOCKS[bj]
                    g = blk_g0[bj] + tj
                    xt, Bx = xt_r[g % 3]
                    uh, Buh = uh_r[g % 6]
                    A.dma(SP, lambda e: e.dma_start(out=xt[:], in_=xsrc(t0_ + tj * 128, 128)),
                          reads=[Bxres[t0_ // 128 + tj]], writes=[Bx])
                    s_ = stt[g % 2]
                    ln_compute(s_, xt, Bx, ADA_EPS)
                    A.op(DVE, lambda e: e.tensor_scalar(
                        out=uh[:], in0=xt[:], scalar1=s_[2][:, 0:1], scalar2=s_[4][:], op0=ALU.subtract, op1=ALU.mult),
                        reads=[Bx, s_[3], s_[5]], writes=[Buh])
                    uhs_blk[bj].append((uh, Buh))

                for tj in range(BLOCKS[0][1]):
                    ln_part(0, tj)
                for bi, (t0, nti) in enumerate(BLOCKS):
                    is_ctx = (t0 >= S)
                    jm = 1 if is_ctx else 0
                    n = nti * 128
                    uT, BuT = uT_r[bi % 2]
                    nti_next = BLOCKS[bi + 1][1] if bi + 1 < len(BLOCKS) else 0
                    modulate_block(uhs_blk[bi], nti, l, jm, 0, uT, BuT)
                    if dbg:
                        A.dma(SP, lambda e, uT=uT, t0=t0, n=n: e.dma_start(
                            out=DBG["uT"][:, :, t0:t0 + n].rearrange("k p t -> p k t"), in_=uT[:, :, 0:n]),
                            reads=[BuT], writes=[Bdbg])
                    if not (last and is_ctx):
                        for hc in range(6):
                            bank = 6 + (hc % 2)
                            for k in range(8):
                                A.op(PE, lambda e, hc=hc, k=k, bank=bank, uT=uT, n=n: e.matmul(
                                    ps[:, bank, 0:n], lhsT=whm[:, k, hc * 128:(hc + 1) * 128], rhs=uT[:, k, 0:n],
                                    start=(k == 0), stop=(k == 7)), reads=[Bwhm, BuT], writes=[Bps[bank]])
                            hs, Bhs = hys_r[hc % 2]
                            A.op(ACT, lambda e, hs=hs, bank=bank, n=n: e.copy(out=hs[:, 0:n], in_=ps[:, bank, 0:n]),
                                 reads=[Bps[bank]], writes=[Bhs])
                            A.dma(SP, lambda e, hs=hs, hc=hc, t0=t0, n=n: e.dma_start(
                                out=hyraw[hc, :, t0:t0 + n], in_=hs[:, 0:n]), reads=[Bhs], writes=[Bhyraw])
                    for ti in range(nti):
                        g = tile_ctr + ti
                        tok = t0 + ti * 128
                        for nb in range(2):
                            bank = 4 + nb
                            for k in range(8):
                                A.op(PE, lambda e, nb=nb, k=k, bank=bank, uT=uT, ti=ti: e.matmul(
                                    ps[:, bank, :], lhsT=uT[:, k, ti * 128:(ti + 1) * 128],
                                    rhs=wtm[:, k, nb * 512:(nb + 1) * 512], start=(k == 0), stop=(k == 7)),
                                    reads=[Bwtm, BuT], writes=[Bps[bank]])
                        qk, Bqk = qk_r[g % 2]
                        A.op(ACT, lambda e, qk=qk: e.copy(out=qk[:, 0:512], in_=ps[:, 4, :]), reads=[Bps[4]], writes=[Bqk])
                        A.op(ACT, lambda e, qk=qk: e.copy(out=qk[:, 512:640], in_=ps[:, 5, 0:128]), reads=[Bps[5]], writes=[Bqk])
                        A.op(ACT, lambda e, g=g: e.copy(out=vp[:, g, :, 0:64],
                                                        in_=ps[:, 5, 128:256].rearrange("p (a b) -> p a b", b=64)),
                             reads=[Bps[5]], writes=[Bvp])
                        A.op(ACT, lambda e, g=g: e.copy(out=pl_tm[:, g, :], in_=ps[:, 5, 256:512]),
                             reads=[Bps[5]], writes=[Bpl])
                        A.op(DVE, lambda e, qk=qk: e.tensor_tensor(out=sq[:], in0=qk[:], in1=qk[:], op=ALU.mult),
                             reads=[Bqk], writes=[Bsq])
                        A.op(DVE, lambda e: e.tensor_reduce(out=ssq[:], in_=sq[:].rearrange("p (h d) -> p h d", d=64),
                                                            axis=AX.X, op=ALU.add), reads=[Bsq], writes=[Bssq])
                        A.op(ACT, lambda e: e.activation(out=ssq[:], in_=ssq[:], func=AF.Sqrt, bias=QK_EPS, scale=1.0 / 64),
                             reads=[Bssq], writes=[Bssq])
                        A.op(DVE, lambda e: e.reciprocal(out=ssq[:], in_=ssq[:]), reads=[Bssq], writes=[Bssq])
                        A.op(DVE, lambda e, qk=qk: e.tensor_tensor(
                            out=qk[:].rearrange("p (h d) -> p h d", d=64), in0=qk[:].rearrange("p (h d) -> p h d", d=64),
                            in1=ssq[:].unsqueeze(2).to_broadcast([128, 10, 64]), op=ALU.mult), reads=[Bqk, Bssq], writes=[Bqk])
                        qkr, Bqkr = qkr_r[g % 2]
                        kd4 = qkr[:, 512:1024].rearrange("p (a b c d) -> p a b c d", a=2, b=2, c=2)
                        if is_ctx:
                            A.op(DVE, lambda e, qk=qk, qkr=qkr: e.tensor_tensor(
                                out=qkr[:, 0:512], in0=qk[:, 0:512], in1=gain[:, 0:512], op=ALU.mult),
                                reads=[Bqk, Bgain], writes=[Bqkr])
                            for b2 in range(2):
                                A.op(DVE, lambda e, qk=qk, kd4=kd4, b2=b2: e.tensor_tensor(
                                    out=kd4[:, :, b2, b2, :], in0=qk[:, 512:640].rearrange("p (a d) -> p a d", d=64),
                                    in1=gain[:, 512:640].rearrange("p (a d) -> p a d", d=64), op=ALU.mult),
                                    reads=[Bqk, Bgain], writes=[Bqkr])
                        else:
                            cs_, Bcs = cosr[g % 2]
                            sn_, Bsn = sinr[g % 2]
                            A.dma(SP, lambda e, cs_=cs_, tok=tok: e.dma_start(out=cs_[:], in_=I["k_cosF"][tok:tok + 128, :]), writes=[Bcs])
                            A.dma(SP, lambda e, sn_=sn_, tok=tok: e.dma_start(out=sn_[:], in_=I["k_sinF"][tok:tok + 128, :]), writes=[Bsn])
                            A.op(DVE, lambda e, qk=qk: e.tensor_tensor(out=qk[:], in0=qk[:], in1=gain[:], op=ALU.mult),
                                 reads=[Bqk, Bgain], writes=[Bqk])
                            A.op(DVE, lambda e, qk=qk, cs_=cs_: e.tensor_tensor(out=t1[:], in0=qk[:], in1=cs_[:], op=ALU.mult),
                                 reads=[Bqk, Bcs], writes=[Bt1])
                            qv = qk[:].rearrange("p (h s d) -> p h s d", s=2, d=16)
                            sv = sn_[:].rearrange("p (h s d) -> p h s d", s=2, d=16)
                            tv = t2[:].rearrange("p (h s d) -> p h s d", s=2, d=16)
                            for s2 in range(2):
                                A.op(POOL, lambda e, s2=s2, qv=qv, sv=sv, tv=tv: e.tensor_tensor(
                                    out=tv[:, :, s2, :], in0=qv[:, :, 1 - s2, :], in1=sv[:, :, s2, :], op=ALU.mult),
                                    reads=[Bqk, Bsn], writes=[Bt2])
                            A.op(DVE, lambda e, qkr=qkr: e.tensor_tensor(out=qkr[:, 0:512], in0=t1[:, 0:512], in1=t2[:, 0:512], op=ALU.add),
                                 reads=[Bt1, Bt2], writes=[Bqkr])
                            for b2 in range(2):
                                A.op(DVE, lambda e, kd4=kd4, b2=b2: e.tensor_tensor(
                                    out=kd4[:, :, b2, b2, :], in0=t1[:, 512:640].rearrange("p (a d) -> p a d", d=64),
                                    in1=t2[:, 512:640].rearrange("p (a d) -> p a d", d=64), op=ALU.add),
                                    reads=[Bt1, Bt2], writes=[Bqkr])
                        for c6 in range(8):
                            A.op(PE, lambda e, c6=c6, qkr=qkr: e.transpose(
                                out=psb(6)[:, c6 * 128:(c6 + 1) * 128], in_=qkr[:, c6 * 128:(c6 + 1) * 128], identity=identb[:]),
                                reads=[Bqkr, Bidb], writes=[Bps[6]])
                        A.op(DVE, lambda e, tok=tok: e.tensor_copy(
                            out=qT[:, :, tok:tok + 128], in_=psb(6)[:, 0:512].rearrange("p (a t) -> p a t", t=128)),
                            reads=[Bps[6]], writes=[BqT])
                        A.op(DVE, lambda e, tok=tok: e.tensor_copy(
                            out=kT[:, :, tok:tok + 128], in_=psb(6)[:, 512:1024].rearrange("p (a t) -> p a t", t=128)),
                            reads=[Bps[6]], writes=[BkT])
                        if ti < nti_next:
                            ln_part(bi + 1, ti)
                    for tj in range(nti, nti_next):
                        ln_part(bi + 1, tj)
                    tile_ctr += nti
                A.barrier()

            if True:
                with ExitStack() as stk2:
                    def T2(name, shape, dt):
                        return stk2.enter_context(_sbuf_unique(name, shape, dt)), Buf(name)
                    band, Bband = T2("band", [128, 20, 128], BF16)
                    pw, Bpw = T2("pw", [128, 2, 64], BF16)
                    psc, Bpsc = T2("psc", [128, 2], F32)
                    A.dma(SP, lambda e: e.dma_start(out=band[:], in_=I["k_band"]), writes=[Bband])
                    A.dma(POOL, lambda e: e.dma_start(out=pw[:], in_=I["pool_w"][l].rearrange("(a b) c d -> (b c) a d", b=2)), writes=[Bpw])
                    load_cols(T2, psc[:], Bpsc, I["pool_scale"][l], 2)
                    yp_r = [T2(f"yp{i}", [128, 2, 128], BF16) for i in range(2)]
                    po_r = [T2(f"po{i}", [128, 2, 512], BF16) for i in range(2)]
                    seqs = [(0, 32)] if last else [(0, 32), (32, 2)]
                    for (g0, ng) in seqs:
                        for tb in range(0, ng, 4):
                            nb4 = min(4, ng - tb)
                            po, Bpo = po_r[(tb // 4) % 2]
                            for tq in range(nb4):
                                tt = tb + tq
                                g = g0 + tt
                                yp, Byp = yp_r[g % 2]
                                for grp in range(4):
                                    srcs = []
                                    if tt > 0:
                                        srcs.append((g - 1, 3))
                                    srcs.append((g, 0 if tt == 0 else (2 if tt == ng - 1 else 1)))
                                    if tt < ng - 1:
                                        srcs.append((g + 1, 4))
                                    h2 = (grp % 2) * 64
                                    for si, (sg, kind) in enumerate(srcs):
                                        A.op(PE, lambda e, grp=grp, sg=sg, kind=kind, si=si, ns=len(srcs), h2=h2: e.matmul(
                                            ps[h2:h2 + 64, 6, (grp // 2) * 128:(grp // 2 + 1) * 128],
                                            lhsT=pl_tm[:, sg, grp * 64:(grp + 1) * 64], rhs=band[:, grp * 5 + kind, :],
                                            start=(si == 0), stop=(si == ns - 1), skip_group_check=True),
                                            reads=[Bpl, Bband], writes=[Bps[6]])
                                A.op(ACT, lambda e, yp=yp: e.copy(out=yp[:], in_=ps[:, 6, 0:256].rearrange("p (a t) -> p a t", t=128)),
                                     reads=[Bps[6]], writes=[Byp])
                                for grp in range(4):
                                    h2 = (grp % 2) * 64
                                    A.op(PE, lambda e, grp=grp, h2=h2, yp=yp: e.matmul(
                                        ps[h2:h2 + 64, 7, (grp // 2) * 128:(grp // 2 + 1) * 128],
                                        lhsT=pw[h2:h2 + 64, grp // 2, :], rhs=yp[h2:h2 + 64, grp // 2, :],
                                        start=True, stop=True, skip_group_check=True), reads=[Bpw, Byp], writes=[Bps[7]])
                                for a in range(2):
                                    A.op(ACT, lambda e, a=a, po=po, tq=tq: e.activation(
                                        out=po[:, a, tq * 128:(tq + 1) * 128], in_=ps[:, 7, a * 128:(a + 1) * 128],
                                        func=AF.Copy, scale=psc[:, a:a + 1]), reads=[Bps[7], Bpsc], writes=[Bpo])
                            tok0 = (g0 + tb) * 128
                            bi = min(tok0 // 512, 8)
                            A.dma(SP, lambda e, po=po, tok0=tok0, nb4=nb4: e.dma_start(
                                out=catT[6:8, :, tok0:tok0 + nb4 * 128].rearrange("k p t -> p k t"), in_=po[:, :, 0:nb4 * 128]),
                                reads=[Bpo], writes=[Bcat[bi]])
                    A.barrier()

            with ExitStack() as stk3:
                def T3(name, shape, dt):
                    return stk3.enter_context(_sbuf_unique(name, shape, dt)), Buf(name)
                ex_r = [T3(f"ex{i}", [128, 2, 512], BF16) for i in range(4)]
                rc_r = [T3(f"rc{i}", [64, 512], F32) for i in range(2)]
                ca_r = [T3(f"ca{i}", [128, 4, 512], BF16) for i in range(2)]
                SB = (0, 2, 6)
                qblocks = [(i * 512, 4, list(range(NT))) for i in range(8)]
                if not last:
                    qblocks.append((S, 2, [32, 33]))
                units = []
                for qi, (q0, nq, kts) in enumerate(qblocks):
                    for h in range(8):
                        pairs = [kts[i:i + 2] for i in range(0, len(kts), 2)]
                        for pi, pk in enumerate(pairs):
                            units.append((qi, q0, nq, h, pi, len(pairs), pk))

                def emit_qk(ui):
                    qi, q0, nq, h, pi, npairs, pk = units[ui]
                    nqt = nq * 128
                    kv, hf, pr = h // 4, (h % 2) * 64, h // 2
                    sb0 = SB[ui % 3]
                    for j2, kt in enumerate(pk):
                        A.op(PE, lambda e, kt=kt, j2=j2, sb0=sb0, h=h, kv=kv, pr=pr, q0=q0, nqt=nqt: e.matmul(
                            ps[:, sb0 + j2, 0:nqt], lhsT=kT[:, kv * 2 + (h % 2), kt * 128:(kt + 1) * 128],
                            rhs=qT[:, pr, q0:q0 + nqt], start=True, stop=True),
                            reads=[BkT, BqT], writes=[Bps[sb0 + j2]])

                LOOK = 2
                for ui in range(min(LOOK, len(units))):
                    emit_qk(ui)
                for ui, (qi, q0, nq, h, pi, npairs, pk) in enumerate(units):
                    nqt = nq * 128
                    kv, hf, pr = h // 4, (h % 2) * 64, h // 2
                    obank = 4 + (h % 2)
                    sb0 = SB[ui % 3]
                    npk = len(pk)
                    ex, Bex = ex_r[ui % 4]
                    A.op(ACT, lambda e, ex=ex, sb0=sb0, npk=npk, nqt=nqt: e.activation(
                        out=ex[:, 0:npk, 0:nqt], in_=ps[:, sb0:sb0 + npk, 0:nqt], func=AF.Exp),
                        reads=[Bps[sb0 + i] for i in range(npk)], writes=[Bex])
                    if ui + LOOK < len(units):
                        emit_qk(ui + LOOK)
                    for j2, kt in enumerate(pk):
                        first = (pi == 0 and j2 == 0)
                        lastmm = (pi == npairs - 1 and j2 == npk - 1)
                        A.op(PE, lambda e, ex=ex, j2=j2, kt=kt, kv=kv, obank=obank, first=first, lastmm=lastmm, nqt=nqt: e.matmul(
                            ps[:, obank, 0:nqt], lhsT=vp[:, kt, kv, :], rhs=ex[:, j2, 0:nqt],
                            start=first, stop=lastmm), reads=[Bex, Bvp], writes=[Bps[obank]])
                    if pi == npairs - 1:
                        ca, Bca = ca_r[qi % 2]
                        rc, Brc = rc_r[h % 2]
                        A.op(DVE, lambda e, rc=rc, obank=obank, nqt=nqt: e.reciprocal(
                            out=rc[0:64, 0:nqt], in_=ps[64:128, obank, 0:nqt]), reads=[Bps[obank]], writes=[Brc])
                        A.op(DVE, lambda e, rc=rc, ca=ca, obank=obank, nqt=nqt, hf=hf, pr=pr: e.tensor_tensor(
                            out=ca[hf:hf + 64, pr, 0:nqt], in0=ps[0:64, obank, 0:nqt], in1=rc[0:64, 0:nqt], op=ALU.mult),
                            reads=[Bps[obank], Brc], writes=[Bca])
                        if h == 7:
                            bi = min(q0 // 512, 8)
                            A.dma(SP, lambda e, ca=ca, q0=q0, nqt=nqt: e.dma_start(
                                out=catT[0:4, :, q0:q0 + nqt].rearrange("k p t -> p k t"), in_=ca[:, :, 0:nqt]),
                                reads=[Bca], writes=[Bcat[bi]])
                A.barrier()

        def hyena(seq_t0, L, fwd_d, inv_d, feats_d, decay_d):
            nt = L // 128
            nfb = max(L // 512, 1)
            fbw = min(L, 512)
            with ExitStack() as stk:
                def T(name, shape, dt):
                    return stk.enter_context(_sbuf_unique(name, shape, dt)), Buf(name)
                Ksp, BK = T("Ksp", [128, nt, 2, 512], BF16)
                slab_r = [T(f"slab{i}", [128, 2, nt, 128], BF16) for i in range(2)]
                slab_ctr = [0]

                def load_slab(src_d, idx):
                    sl, Bsl = slab_r[slab_ctr[0] % 2]
                    slab_ctr[0] += 1
                    A.dma(SP, lambda e, sl=sl, idx=idx: e.dma_start(
                        out=sl[:].rearrange("p a b c -> p (a b c)"), in_=src_d[idx]), writes=[Bsl])
                    return sl, Bsl

                with ExitStack() as stkf:
                    def TF(name, shape, dt):
                        return stkf.enter_context(_sbuf_unique(name, shape, dt)), Buf(name)
                    Ptm, BP = TF("Ptm", [128, nt, 512], BF16)
                    Qtm, BQ = TF("Qtm", [128, nt, 512], BF16)
                    fe_r = [TF(f"feats{i}", [33, 512], F32) for i in range(2)]
                    dc_r = [TF(f"decay{i}", [128, 256], F32) for i in range(2)]
                    w1, Bw1 = TF("w1", [33, 64], F32)
                    w2, Bw2 = TF("w2", [64, 64], F32)
                    w3, Bw3 = TF("w3", [64, 1024], F32)
                    fb, Bfb = TF("fb", [64, 3], F32)
                    h1, Bh1 = TF("h1", [64, 512], F32)
                    h2, Bh2 = TF("h2", [64, 512], F32)
                    arg, Barg = TF("arg", [64, 512], F32)
                    rr, Brr = TF("rr", [64, 512], F32)
                    A.dma(SP, lambda e: e.dma_start(out=w1[:], in_=I["hy_f_w1"][l]), writes=[Bw1])
                    A.dma(SP, lambda e: e.dma_start(out=w2[:], in_=I["hy_f_w2"][l]), writes=[Bw2])
                    A.dma(SP, lambda e: e.dma_start(out=w3[:], in_=I["hy_f_w3"][l]), writes=[Bw3])
                    for ci, nm in enumerate(("hy_f_b1", "hy_f_freq", "hy_f_b2")):
                        A.dma(SP, lambda e, ci=ci, nm=nm: e.dma_start(
                            out=fb[:, ci:ci + 1], in_=I[nm][l].rearrange("(p o) -> p o", o=1)), writes=[Bfb])

                    def sin_layer(wt, Bwt, kdim, src, Bsrc, bcol, dst, Bdst):
                        A.op(PE, lambda e: e.matmul(ps[0:64, 0, 0:fbw], lhsT=wt[0:kdim, :], rhs=src[0:kdim, 0:fbw],
                                                    start=True, stop=True), reads=[Bwt, Bsrc], writes=[Bps[0]])
                        A.op(DVE, lambda e: e.tensor_scalar(out=arg[:, 0:fbw], in0=ps[0:64, 0, 0:fbw], scalar1=fb[:, bcol:bcol + 1],
                                                            scalar2=fb[:, 1:2], op0=ALU.add, op1=ALU.mult),
                             reads=[Bps[0], Bfb], writes=[Barg])
                        A.op(DVE, lambda e: e.tensor_scalar(out=rr[:, 0:fbw], in0=arg[:, 0:fbw], scalar1=1.0 / TWO_PI, scalar2=MAGIC,
                                                            op0=ALU.mult, op1=ALU.add), reads=[Barg], writes=[Brr])
                        A.op(DVE, lambda e: e.tensor_scalar(out=rr[:, 0:fbw], in0=rr[:, 0:fbw], scalar1=-MAGIC, scalar2=-TWO_PI,
                                                            op0=ALU.add, op1=ALU.mult), reads=[Brr], writes=[Brr])
                        A.op(DVE, lambda e: e.tensor_tensor(out=arg[:, 0:fbw], in0=arg[:, 0:fbw], in1=rr[:, 0:fbw], op=ALU.add),
                             reads=[Barg, Brr], writes=[Barg])
                        A.op(DVE, lambda e: e.tensor_scalar(out=arg[:, 0:fbw], in0=arg[:, 0:fbw], scalar1=math.pi, scalar2=-math.pi,
                                                            op0=ALU.min, op1=ALU.max), reads=[Barg], writes=[Barg])
                        A.op(ACT, lambda e: e.activation(out=dst[:, 0:fbw], in_=arg[:, 0:fbw], func=AF.Sin),
                             reads=[Barg], writes=[Bdst])
                    hd_r = [TF(f"hd{i}", [128, 2, 2, 256], F32) for i in range(2)]
                    ab_r = [TF(f"ab{i}", [128, 1024], BF16) for i in range(2)]
                    for fbk in range(nfb):
                        feats, Bfe = fe_r[fbk % 2]
                        A.dma(SP, lambda e, feats=feats, fbk=fbk: e.dma_start(out=feats[:, 0:fbw], in_=feats_d[:, fbk * fbw:(fbk + 1) * fbw]),
                              writes=[Bfe])
                        sin_layer(w1, Bw1, 33, feats, Bfe, 0, h1, Bh1)
                        sin_layer(w2, Bw2, 64, h1, Bh1, 2, h2, Bh2)
                        for jj in range(fbw // 128):
                            j = fbk * (fbw // 128) + jj
                            hd, Bhd = hd_r[j % 2]
                            ab, Bab = ab_r[j % 2]
                            decay, Bdec = dc_r[j % 2]
                            A.dma(SP, lambda e, decay=decay, j=j: e.dma_start(out=decay[:], in_=decay_d[:, j, :]), writes=[Bdec])
                            for o in range(2):
                                A.op(PE, lambda e, o=o, jj=jj: e.matmul(ps[:, 1 + o, :], lhsT=h2[:, jj * 128:(jj + 1) * 128],
                                                                        rhs=w3[:, o * 512:(o + 1) * 512], start=True, stop=True),
                                     reads=[Bh2, Bw3], writes=[Bps[1 + o]])
                                for dr in range(2):
                                    A.op(DVE, lambda e, o=o, dr=dr, hd=hd, decay=decay: e.tensor_tensor(
                                        out=hd[:, o, dr, :], in0=ps[:, 1 + o, dr * 256:(dr + 1) * 256], in1=decay[:], op=ALU.mult),
                                        reads=[Bps[1 + o], Bdec], writes=[Bhd])
                            A.op(DVE, lambda e, hd=hd, ab=ab: e.scalar_tensor_tensor(
                                out=ab[:], in0=hd[:].rearrange("p a b c -> p (a b c)"), scalar=-1.0,
                                in1=hd[:].rearrange("p a b c -> p (a b c)"), op0=ALU.mult, op1=ALU.max),
                                reads=[Bhd], writes=[Bab])
                            for o in range(2):
                                A.op(PE, lambda e, o=o, j=j, ab=ab: e.matmul(ps[0:1, 3 + o, :], lhsT=ones_b[:, 0:1], rhs=ab[:, o * 512:(o + 1) * 512],
                                                                             start=(j == 0), stop=(j == nt - 1)),
                                     reads=[Bones, Bab], writes=[Bps[3 + o]])
                            A.op(POOL, lambda e, hd=hd, j=j: e.tensor_tensor(
                                out=Ptm[:, j, :].rearrange("p (o c) -> p o c", o=2), in0=hd[:, :, 0, :], in1=hd[:, :, 1, :], op=ALU.add),
                                reads=[Bhd], writes=[BP])
                            A.op(POOL, lambda e, hd=hd, j=j: e.tensor_tensor(
                                out=Qtm[:, j, :].rearrange("p (o c) -> p o c", o=2), in0=hd[:, :, 0, :], in1=hd[:, :, 1, :], op=ALU.subtract),
                                reads=[Bhd], writes=[BQ])
                            if j == 0:
                                for dst, Bd in ((Ptm, BP), (Qtm, BQ)):
                                    A.op(POOL, lambda e, hd=hd, dst=dst: e.tensor_copy(
                                        out=dst[0:1, 0, :].rearrange("p (o c) -> p o c", o=2), in_=hd[0:1, :, 0, :]),
                                        reads=[Bhd], writes=[Bd])
                    nrm, Bnrm = TF("nrm", [1, 2, 256], F32)
                    invn, Binv = TF("invn_bc", [128, 512], F32)
                    dbc, Bdbc = TF("d_bc", [128, 512], F32)
                    for o in range(2):
                        A.op(DVE, lambda e, o=o: e.tensor_copy(out=nrm[:, o, :], in_=ps[0:1, 3 + o, 0:256]), reads=[Bps[3 + o]], writes=[Bnrm])
                        A.op(DVE, lambda e, o=o: e.tensor_tensor(out=nrm[:, o, :], in0=nrm[:, o, :], in1=ps[0:1, 3 + o, 256:512], op=ALU.add),
                             reads=[Bps[3 + o], Bnrm], writes=[Bnrm])
                    A.op(DVE, lambda e: e.reciprocal(out=nrm[:], in_=nrm[:]), reads=[Bnrm], writes=[Bnrm])
                    isl = 0 if L == S else 1
                    A.dma(SP, lambda e: e.dma_start(out=invn_d[isl:isl + 1, :], in_=nrm[:].rearrange("p a b -> p (a b)")),
                          reads=[Bnrm], writes=[Binvn])
                    A.dma(SP, lambda e: e.dma_start(out=invn[:], in_=invn_d[isl].partition_broadcast(128)), reads=[Binvn], writes=[Binv])
                    A.dma(SP, lambda e: e.dma_start(out=dbc[:], in_=I["hy_d"][l].rearrange("a b -> (a b)").partition_broadcast(128)),
                          writes=[Bdbc])
                    for ft in range(nt):
                        sl, Bsl = load_slab(fwd_d, ft)
                        sbk = 4 + (ft % 2) * 2
                        for cs, (src, Bsrc) in enumerate(((Ptm, BP), (Qtm, BQ))):
                            bank = sbk + cs
                            for st_ in range(nt):
                                A.op(PE, lambda e, cs=cs, st_=st_, sl=sl, src=src, bank=bank: e.matmul(
                                    ps[:, bank, :], lhsT=sl[:, cs, st_, :], rhs=src[:, st_, :], start=(st_ == 0), stop=(st_ == nt - 1)),
                                    reads=[Bsl, Bsrc], writes=[Bps[bank]])
                        A.op(DVE, lambda e, ft=ft, sbk=sbk: e.tensor_tensor(out=Ksp[:, ft, 1, :], in0=ps[:, sbk + 1, :], in1=invn[:], op=ALU.mult),
                             reads=[Bps[sbk + 1], Binv], writes=[BK])
                        krt, Bkrt = hd_r[ft % 2]
                        krv = krt[:].rearrange("p a b c -> p (a b c)")[:, 0:512]
                        A.op(DVE, lambda e, krv=krv, sbk=sbk: e.tensor_tensor(out=krv, in0=ps[:, sbk, :], in1=invn[:], op=ALU.mult),
                             reads=[Bps[sbk], Binv], writes=[Bkrt])
                        A.op(POOL, lambda e, krv=krv, ft=ft: e.tensor_tensor(out=Ksp[:, ft, 0, :], in0=krv, in1=dbc[:], op=ALU.add),
                             reads=[Bkrt, Bdbc], writes=[BK])
                    A.barrier()

                tm3, Btm3 = T("tm3", [128, nt, 768], BF16)
                with ExitStack() as stks:
                    def TS(name, shape, dt):
                        return stks.enter_context(_sbuf_unique(name, shape, dt)), Buf(name)
                    cw, Bcw = TS("cw", [128, 18], F32)
                    cb, Bcb = TS("cb", [128, 6], F32)
                    load_cols(TS, cw[:], Bcw, I["hy_conv_w"][l].rearrange("a b -> (a b)"), 18)
                    load_cols(TS, cb[:], Bcb, I["hy_conv_b"][l], 6)
                    W = min(L, 1024)
                    zin_r = [TS(f"zin{i}", [128, W + 2], F32) for i in range(2)]
                    zt_r = [TS(f"zt{i}", [128, W], F32) for i in range(2)]
                    zc_r = [TS(f"zc{i}", [128, W], BF16) for i in range(2)]
                    ctr = 0
                    for hc in range(6):
                        for b0 in range(0, L, W):
                            zin, Bzin = zin_r[ctr % 2]
                            zt, Bzt = zt_r[ctr % 2]
                            zc, Bzc = zc_r[ctr % 2]
                            ctr += 1
                            lo = b0 - 1
                            hi = b0 + W + 1
                            clo = max(lo, 0)
                            chi = min(hi, L)
                            if lo < 0:
                                A.op(DVE, lambda e, zin=zin: e.memset(zin[:, 0:1], 0.0), writes=[Bzin])
                            if hi > L:
                                A.op(DVE, lambda e, zin=zin: e.memset(zin[:, W + 1:W + 2], 0.0), writes=[Bzin])
                            A.dma(SP, lambda e, zin=zin, hc=hc, clo=clo, chi=chi, lo=lo: e.dma_start(
                                out=zin[:, clo - lo:chi - lo], in_=hyraw[hc, :, seq_t0 + clo:seq_t0 + chi]),
                                reads=[Bhyraw], writes=[Bzin])
                            A.op(DVE, lambda e, zin=zin, zt=zt, hc=hc: e.tensor_scalar(
                                out=zt[:], in0=zin[:, 1:W + 1], scalar1=cw[:, 6 + hc:7 + hc], scalar2=cb[:, hc:hc + 1],
                                op0=ALU.mult, op1=ALU.add), reads=[Bzin, Bcw, Bcb], writes=[Bzt])
                            A.op(DVE, lambda e, zin=zin, zt=zt, hc=hc: e.scalar_tensor_tensor(
                                out=zt[:], in0=zin[:, 0:W], scalar=cw[:, hc:hc + 1], in1=zt[:], op0=ALU.mult, op1=ALU.add),
                                reads=[Bzin, Bcw, Bzt], writes=[Bzt])
                            A.op(DVE, lambda e, zin=zin, zt=zt, zc=zc, hc=hc: e.scalar_tensor_tensor(
                                out=zc[:], in0=zin[:, 2:W + 2], scalar=cw[:, 12 + hc:13 + hc], in1=zt[:], op0=ALU.mult, op1=ALU.add),
                                reads=[Bzin, Bcw, Bzt], writes=[Bzc])
                            for t4 in range(0, W // 128, 4):
                                n4 = min(4, W // 128 - t4)
                                bank = (t4 // 4) % 2
                                for q in range(n4):
                                    A.op(PE, lambda e, zc=zc, t4=t4, q=q, bank=bank: e.transpose(
                                        out=psb(bank)[:, q * 128:(q + 1) * 128], in_=zc[:, (t4 + q) * 128:(t4 + q + 1) * 128],
                                        identity=identb[:]), reads=[Bzc, Bidb], writes=[Bps[bank]])
                                tile0 = b0 // 128 + t4
                                A.op(ACT, lambda e, tile0=tile0, n4=n4, bank=bank, hc=hc: e.copy(
                                    out=tm3[:, tile0:tile0 + n4, hc * 128:(hc + 1) * 128],
                                    in_=psb(bank)[:, 0:n4 * 128].rearrange("p (a c) -> p a c", c=128)),
                                    reads=[Bps[bank]], writes=[Btm3])
                    A.barrier()

                with ExitStack() as stkc:
                    def TC(name, shape, dt):
                        return stkc.enter_context(_sbuf_unique(name, shape, dt)), Buf(name)
                    Ysb, BY = TC("Ysb", [128, nt, 2, 256], BF16)
                    z1, Bz1 = TC("z1", [128, nt, 256], BF16)
                    tA = [TC(f"tA{i}", [128, 256], F32) for i in range(4)]
                    yo_r = [TC(f"yo{i}", [128, 4, 256], BF16) for i in range(2)]
                    cy_r = [TC(f"cy{i}", [128, 2, 512], BF16) for i in range(2)]
                    for o in range(2):
                        for ft in range(nt):
                            sl, Bsl = load_slab(fwd_d, ft)
                            fb0 = (ft % 2) * 2
                            for cs in range(2):
                                bank = fb0 + cs
                                for st_ in range(nt):
                                    rhs = tm3[:, st_, 0:256] if o == 0 else z1[:, st_, :]
                                    A.op(PE, lambda e, cs=cs, st_=st_, sl=sl, rhs=rhs, bank=bank: e.matmul(
                                        ps[:, bank, 0:256], lhsT=sl[:, cs, st_, :], rhs=rhs, start=(st_ == 0), stop=(st_ == nt - 1)),
                                        reads=[Bsl, Btm3 if o == 0 else Bz1], writes=[Bps[bank]])
                            Kr = Ksp[:, ft, 0, o * 256:(o + 1) * 256]
                            Ki = Ksp[:, ft, 1, o * 256:(o + 1) * 256]
                            for i4, (zb, kk) in enumerate(((fb0, Kr), (fb0 + 1, Ki), (fb0, Ki), (fb0 + 1, Kr))):
                                A.op(DVE, lambda e, i4=i4, zb=zb, kk=kk: e.tensor_tensor(out=tA[i4][0][:], in0=ps[:, zb, 0:256], in1=kk, op=ALU.mult),
                                     reads=[Bps[zb], BK], writes=[tA[i4][1]])
                            A.op(POOL, lambda e, ft=ft: e.tensor_tensor(out=Ysb[:, ft, 0, :], in0=tA[0][0][:], in1=tA[1][0][:], op=ALU.subtract),
                                 reads=[tA[0][1], tA[1][1]], writes=[BY])
                            A.op(POOL, lambda e, ft=ft: e.tensor_tensor(out=Ysb[:, ft, 1, :], in0=tA[2][0][:], in1=tA[3][0][:], op=ALU.add),
                                 reads=[tA[2][1], tA[3][1]], writes=[BY])
                        for tt in range(nt):
                            sl, Bsl = load_slab(inv_d, tt)
                            bank = 4 + tt % 2
                            n_mm = 2 * nt
                            i_mm = 0
                            for cs in range(2):
                                for ft in range(nt):
                                    A.op(PE, lambda e, cs=cs, ft=ft, sl=sl, bank=bank, i_mm=i_mm: e.matmul(
                                        ps[:, bank, 0:256], lhsT=sl[:, cs, ft, :], rhs=Ysb[:, ft, cs, :],
                                        start=(i_mm == 0), stop=(i_mm == n_mm - 1)), reads=[Bsl, BY], writes=[Bps[bank]])
                                    i_mm += 1
                            if o == 0:
                                A.op(DVE, lambda e, tt=tt, bank=bank: e.tensor_tensor(
                                    out=z1[:, tt, :], in0=ps[:, bank, 0:256], in1=tm3[:, tt, 256:512], op=ALU.mult),
                                    reads=[Bps[bank], Btm3], writes=[Bz1])
                            else:
                                yo, Byo = yo_r[(tt // 4) % 2]
                                A.op(DVE, lambda e, tt=tt, bank=bank, yo=yo: e.tensor_tensor(
                                    out=yo[:, tt % 4, :], in0=ps[:, bank, 0:256], in1=tm3[:, tt, 512:768], op=ALU.mult),
                                    reads=[Bps[bank], Btm3], writes=[Byo])
                                if tt % 4 == 3 or tt == nt - 1:
                                    n4 = tt % 4 + 1
                                    cy, Bcy = cy_r[(tt // 4) % 2]
                                    for q in range(n4):
                                        for a in range(2):
                                            A.op(PE, lambda e, q=q, a=a, yo=yo: e.transpose(
                                                out=psb(6 + a)[:, q * 128:(q + 1) * 128], in_=yo[:, q, a * 128:(a + 1) * 128],
                                                identity=identb[:]), reads=[Byo, Bidb], writes=[Bps[6 + a]])
                                    for a in range(2):
                                        A.op(ACT, lambda e, a=a, cy=cy, n4=n4: e.copy(out=cy[:, a, 0:n4 * 128], in_=psb(6 + a)[:, 0:n4 * 128]),
                                             reads=[Bps[6 + a]], writes=[Bcy])
                                    tok0 = seq_t0 + (tt - n4 + 1) * 128
                                    bi = min(tok0 // 512, 8)
                                    A.dma(SP, lambda e, cy=cy, tok0=tok0, n4=n4: e.dma_start(
                                        out=catT[4:6, :, tok0:tok0 + n4 * 128].rearrange("k p t -> p k t"), in_=cy[:, :, 0:n4 * 128]),
                                        reads=[Bcy], writes=[Bcat[bi]])
                    A.barrier()

        hyena(0, S, I["k_fwd"], I["k_inv"], I["k_feats"], I["k_decay"])
        if not last:
            hyena(S, CTX, I["k_fwdc"], I["k_invc"], I["k_featsc"], I["k_decayc"])
        if dbg:
            A.dma(SP, lambda e: e.dma_start(out=DBG["cat"], in_=catT), reads=Bcat, writes=[Bdbg])
            A.dma(SP, lambda e: e.dma_start(out=DBG["hy"], in_=hyraw), reads=[Bhyraw], writes=[Bdbg])
            A.barrier()

        def load_bc(T, name, src_vec):
            t, Bt = T(name, [128, D], F32)
            A.dma(SP, lambda e: e.dma_start(out=t[:], in_=src_vec.partition_broadcast(128)), reads=[Bgate], writes=[Bt])
            return t, Bt

        def deepnorm_tile(T_, stt_, psbanks, gate_bc, Bgate_bc, xin, Bxin, g_bc, Bg, b_bc, Bb, yt, Byt, xo, Bxo):
            b0 = psbanks
            A.op(DVE, lambda e: e.tensor_tensor(out=yt[:].rearrange("p (a c) -> p a c", a=2), in0=ps[:, b0:b0 + 2, :],
                                                in1=gate_bc[:].rearrange("p (a c) -> p a c", a=2), op=ALU.mult),
                 reads=[Bps[b0], Bps[b0 + 1], Bgate_bc], writes=[Byt])
            A.op(POOL, lambda e: e.tensor_tensor(out=yt[:], in0=yt[:], in1=xin[:], op=ALU.add), reads=[Byt, Bxin], writes=[Byt])
            ln_compute(stt_, yt, Byt, LN_EPS / (ALPHA * ALPHA))
            A.op(DVE, lambda e: e.tensor_scalar(out=yt[:], in0=yt[:], scalar1=stt_[2][:, 0:1], scalar2=stt_[4][:],
                                                op0=ALU.subtract, op1=ALU.mult), reads=[Byt, stt_[3], stt_[5]], writes=[Byt])
            A.op(POOL, lambda e: e.tensor_tensor(out=yt[:], in0=yt[:], in1=g_bc[:], op=ALU.mult), reads=[Byt, Bg], writes=[Byt])
            A.op(POOL, lambda e: e.tensor_tensor(out=xo[:], in0=yt[:], in1=b_bc[:], op=ALU.add), reads=[Byt, Bb], writes=[Bxo])

        with ExitStack() as stk:
            def T(name, shape, dt):
                return stk.enter_context(_sbuf_unique(name, shape, dt)), Buf(name)
            wo, Bwo = T("wo", [128, 8, D], BF16)
            A.dma(POOL, lambda e: e.dma_start(out=wo[:], in_=I["w_out"][l].rearrange("(k p) n -> p k n", p=128)), writes=[Bwo])
            g1l = load_bc(T, "g1l", gate_d[l, 0])
            g1c = load_bc(T, "g1c", gate_d[l, 1])
            lg = load_bc(T, "lg", I["ln1_g"][l])
            lb = load_bc(T, "lb", I["ln1_b"][l])
            if moe:
                rw, Brw = T("rw", [128, 8, NEXP], BF16)
                A.dma(POOL, lambda e: e.dma_start(out=rw[:], in_=I["router_w"][jf].rearrange("(k p) n -> p k n", p=128)), writes=[Brw])
            cat_r = [T(f"catb{i}", [128, 8, 512], BF16) for i in range(2)]
            xin_r = [T(f"xin{i}", [128, D], F32) for i in range(4)]
            yt_r = [T(f"yt{i}", [128, D], F32) for i in range(4)]
            x1_r = [T(f"x1_{i}", [128, D], F32) for i in range(4)]
            nmr_r = [T(f"nmr5_{i}", [128, 2], F32) for i in range(4)]
            uh_r = [T(f"uh5_{i}", [128, D], BF16) for i in range(8)]
            uT_r = [T(f"uT5_{i}", [128, 8, 512], BF16) for i in range(2)]
            stt = [ln_stats(T, None, None, None, f"p5{i}") for i in range(8)]
            p5_g0 = []
            _g = 0
            for (_t0, _n) in BLOCKS:
                p5_g0.append(_g)
                _g += _n

            def p5_loads(bj):
                t0_, nti_ = BLOCKS[bj]
                n_ = nti_ * 128
                cbj, Bcbj = cat_r[bj % 2]
                A.dma(SP, lambda e: e.dma_start(
                    out=cbj[:, :, 0:n_], in_=catT[:, :, t0_:t0_ + n_].rearrange("k p t -> p k t")), reads=[Bcat[bj]], writes=[Bcbj])
                for tj in range(nti_):
                    xin_, Bxin_ = xin_r[(p5_g0[bj] + tj) % 4]
                    tok_ = t0_ + tj * 128
                    A.dma(SP, lambda e, xin_=xin_, tok_=tok_: e.dma_start(out=xin_[:], in_=xsrc(tok_, 128)),
                          reads=[Bxres[tok_ // 128]], writes=[Bxin_])

            tile_ctr = 0
            for bi, (t0, nti) in enumerate(BLOCKS[:nblk]):
                is_ctx = t0 >= S
                jm = 1 if is_ctx else 0
                n = nti * 128
                cb_, Bcb_ = cat_r[bi % 2]
                if bi == 0:
                    p5_loads(0)
                uT, BuT = uT_r[bi % 2]
                gt = g1c if is_ctx else g1l
                for ti in range(nti):
                    g = tile_ctr + ti
                    tok = t0 + ti * 128
                    xin, Bxin = xin_r[g % 4]
                    yt, Byt = yt_r[g % 4]
                    pb = 4 + 2 * (ti % 2)
                    for nb in range(2):
                        for k in range(8):
                            A.op(PE, lambda e, nb=nb, k=k, cb_=cb_, ti=ti, pb=pb: e.matmul(
                                ps[:, pb + nb, :], lhsT=cb_[:, k, ti * 128:(ti + 1) * 128], rhs=wo[:, k, nb * 512:(nb + 1) * 512],
                                start=(k == 0), stop=(k == 7)), reads=[Bcb_, Bwo], writes=[Bps[pb + nb]])
                    A.op(DVE, lambda e, yt=yt, pb=pb, gt=gt: e.tensor_tensor(
                        out=yt[:].rearrange("p (a c) -> p a c", a=2), in0=ps[:, pb:pb + 2, :],
                        in1=gt[0][:].rearrange("p (a c) -> p a c", a=2), op=ALU.mult),
                        reads=[Bps[pb], Bps[pb + 1], gt[1]], writes=[Byt])
                    A.op(POOL, lambda e, yt=yt, xin=xin: e.tensor_tensor(out=yt[:], in0=yt[:], in1=xin[:], op=ALU.add),
                         reads=[Byt, Bxin], writes=[Byt])
                if bi + 1 < nblk:
                    p5_loads(bi + 1)
                for ti in range(nti):
                    g = tile_ctr + ti
                    yt, Byt = yt_r[g % 4]
                    s_ = stt[g % 4]
                    nmr, Bnmr = nmr_r[g % 4]
                    ln_compute(s_, yt, Byt, LN_EPS / (ALPHA * ALPHA))
                    A.op(DVE, lambda e, nmr=nmr, s_=s_: e.tensor_scalar(
                        out=nmr[:, 0:1], in0=s_[2][:, 0:1], scalar1=s_[4][:], scalar2=-1.0, op0=ALU.mult, op1=ALU.mult),
                        reads=[s_[3], s_[5]], writes=[Bnmr])
                    A.op(ACT, lambda e, yt=yt, s_=s_, nmr=nmr: e.activation(
                        out=yt[:], in_=yt[:], func=AF.Identity, scale=s_[4][:], bias=nmr[:, 0:1]),
                        reads=[Byt, s_[5], Bnmr], writes=[Byt])
                for ti in range(nti):
                    g = tile_ctr + ti
                    tok = t0 + ti * 128
                    yt, Byt = yt_r[g % 4]
                    x1, Bx1 = x1_r[g % 4]
                    A.op(DVE, lambda e, yt=yt: e.tensor_tensor(out=yt[:], in0=yt[:], in1=lg[0][:], op=ALU.mult), reads=[Byt, lg[1]], writes=[Byt])
                    A.op(POOL, lambda e, yt=yt, x1=x1: e.tensor_tensor(out=x1[:], in0=yt[:], in1=lb[0][:], op=ALU.add), reads=[Byt, lb[1]], writes=[Bx1])
                    A.dma(SP, lambda e, x1=x1, tok=tok: e.dma_start(out=xres[tok:tok + 128, :], in_=x1[:]), reads=[Bx1], writes=[Bxres[tok // 128]])
                    if dbg:
                        A.dma(SP, lambda e, x1=x1, tok=tok: e.dma_start(out=DBG["x1"][tok:tok + 128, :], in_=x1[:]), reads=[Bx1], writes=[Bdbg])
                uhs = []
                for ti in range(nti):
                    g = tile_ctr + ti
                    x1, Bx1 = x1_r[g % 4]
                    uh, Buh = uh_r[g % 8]
                    s_ = stt[4 + g % 4]
                    nmr, Bnmr = nmr_r[g % 4]
                    ln_compute(s_, x1, Bx1, ADA_EPS)
                    A.op(DVE, lambda e, nmr=nmr, s_=s_: e.tensor_scalar(
                        out=nmr[:, 1:2], in0=s_[2][:, 0:1], scalar1=s_[4][:], scalar2=-1.0, op0=ALU.mult, op1=ALU.mult),
                        reads=[s_[3], s_[5]], writes=[Bnmr])
                    A.op(ACT, lambda e, x1=x1, uh=uh, s_=s_, nmr=nmr: e.activation(
                        out=uh[:], in_=x1[:], func=AF.Identity, scale=s_[4][:], bias=nmr[:, 1:2]),
                        reads=[Bx1, s_[5], Bnmr], writes=[Buh])
                    uhs.append((uh, Buh))
                modulate_block(uhs, nti, l, jm, 24, uT, BuT)
                A.dma(SP, lambda e, uT=uT, t0=t0, n=n: e.dma_start(
                    out=uT_d[:, :, t0:t0 + n].rearrange("k p t -> p k t"), in_=uT[:, :, 0:n]), reads=[BuT], writes=[BuTd[bi]])
                tile_ctr += nti
            A.barrier()

        with ExitStack() as stk:
            def T(name, shape, dt):
                return stk.enter_context(_sbuf_unique(name, shape, dt)), Buf(name)
            NQ = 7
            if moe:
                pieces = [(ex_, q * 7, 7) for ex_ in range(NEXP) for q in range(4)]
                wsrc = lambda ex_: (I["moe_w_gate"][jf, ex_], I["moe_w_up"][jf, ex_], I["moe_w_down"][jf, ex_])
            else:
                pieces = [(0, 0, 6), (0, 6, 6), (0, 12, 5), (0, 17, 5)]
                wsrc = lambda ex_: (I["ffn_w_gate"][jf], I["ffn_w_up"][jf], I["ffn_w_down"][jf])
            comb, Bcomb = T("comb", [128, NT, NEXP], F32)
            if moe:
                with ExitStack() as stkr:
                    def TR(name, shape, dt):
                        return stkr.enter_context(_sbuf_unique(name, shape, dt)), Buf(name)
                    rw, Brw = TR("rw", [128, 8, NEXP], BF16)
                    A.dma(POOL, lambda e: e.dma_start(out=rw[:], in_=I["router_w"][jf].rearrange("(k p) n -> p k n", p=128)), writes=[Brw])
                    uT_r = [TR(f"uTr{i}", [128, 8, 512], BF16) for i in range(2)]
                    lg_r = [TR(f"lgt{i}", [128, 8], F32) for i in range(2)]
                    m8_r = [TR(f"m8{i}", [128, 8], F32) for i in range(2)]
                    gg_r = [TR(f"gg{i}", [128, 4], F32) for i in range(2)]
                    eq_r = [TR(f"eq{i}", [128, 8], F32) for i in range(2)]
                    tile_ctr = 0
                    for bi, (t0, nti) in enumerate(BLOCKS[:nblk]):
                        n = nti * 128
                        uT, BuT = uT_r[bi % 2]
                        A.dma(SP, lambda e, uT=uT, t0=t0, n=n: e.dma_start(
                            out=uT[:, :, 0:n], in_=uT_d[:, :, t0:t0 + n].rearrange("k p t -> p k t")), reads=[BuTd[bi]], writes=[BuT])
                        for ti in range(nti):
                            g = tile_ctr + ti
                            bank = g % 2
                            for k in range(8):
                                A.op(PE, lambda e, k=k, uT=uT, ti=ti, bank=bank: e.matmul(
                                    ps[:, bank, 0:NEXP], lhsT=uT[:, k, ti * 128:(ti + 1) * 128], rhs=rw[:, k, :],
                                    start=(k == 0), stop=(k == 7)), reads=[BuT, Brw], writes=[Bps[bank]])
                            lgt, Blg = lg_r[g % 2]
                            m8, Bm8 = m8_r[g % 2]
                            gg, Bgg = gg_r[g % 2]
                            eq, Beq = eq_r[g % 2]
                            A.op(DVE, lambda e, lgt=lgt, bank=bank: e.tensor_copy(out=lgt[:], in_=ps[:, bank, 0:NEXP]), reads=[Bps[bank]], writes=[Blg])
                            A.op(DVE, lambda e, lgt=lgt, m8=m8: e.max(out=m8[:], in_=lgt[:]), reads=[Blg], writes=[Bm8])
                            A.op(DVE, lambda e, m8=m8, gg=gg: e.tensor_tensor(out=gg[:, 0:1], in0=m8[:, 1:2], in1=m8[:, 0:1], op=ALU.subtract),
                                 reads=[Bm8], writes=[Bgg])
                            A.op(ACT, lambda e, gg=gg: e.activation(out=gg[:, 1:2], in_=gg[:, 0:1], func=AF.Exp), reads=[Bgg], writes=[Bgg])
                            A.op(DVE, lambda e, gg=gg: e.tensor_scalar(out=gg[:, 2:3], in0=gg[:, 1:2], scalar1=1.0, scalar2=None, op0=ALU.add),
                                 reads=[Bgg], writes=[Bgg])
                            A.op(DVE, lambda e, gg=gg: e.reciprocal(out=gg[:, 2:3], in_=gg[:, 2:3]), reads=[Bgg], writes=[Bgg])
                            A.op(DVE, lambda e, gg=gg: e.tensor_tensor(out=gg[:, 3:4], in0=gg[:, 1:2], in1=gg[:, 2:3], op=ALU.mult),
                                 reads=[Bgg], writes=[Bgg])
                            A.op(DVE, lambda e, lgt=lgt, m8=m8, gg=gg, eq=eq: e.tensor_scalar(
                                out=eq[:], in0=lgt[:], scalar1=m8[:, 0:1], scalar2=gg[:, 2:3], op0=ALU.is_equal, op1=ALU.mult),
                                reads=[Blg, Bm8, Bgg], writes=[Beq])
                            tg = t0 // 128 + ti
                            A.op(DVE, lambda e, lgt=lgt, m8=m8, gg=gg, tg=tg: e.tensor_scalar(
                                out=comb[:, tg, :], in0=lgt[:], scalar1=m8[:, 1:2], scalar2=gg[:, 3:4], op0=ALU.is_equal, op1=ALU.mult),
                                reads=[Blg, Bm8, Bgg], writes=[Bcomb])
                            A.op(DVE, lambda e, eq=eq, tg=tg: e.tensor_tensor(out=comb[:, tg, :], in0=comb[:, tg, :], in1=eq[:], op=ALU.add),
                                 reads=[Beq, Bcomb], writes=[Bcomb])
                        tile_ctr += nti
                    A.barrier()
            if last:
                sblocks = [[0, 1, 2], [3, 4, 5], [6, 7]]
            else:
                sblocks = [[0, 1, 2], [3, 4, 5], [6, 7, 8]]
            acc, _ = T("acc", [128, 12, D], F32)
            Bacc = [Buf(f"acc{i}") for i in range(12)]
            w_r = [(T(f"wgq{i}", [128, 8, NQ * 128], BF16), T(f"wuq{i}", [128, 8, NQ * 128], BF16), T(f"wdq{i}", [128, NQ, D], BF16)) for i in range(2)]
            uT_r = [T(f"uT7_{i}", [128, 8, 512], BF16) for i in range(2)]
            aT_r = [T(f"aT7_{i}", [128, NQ, 512], BF16) for i in range(2)]
            sg_r = [T(f"sg7_{i}", [128, 512], BF16) for i in range(2)]
            g2l = load_bc(T, "g2l7", gate_d[l, 2])
            g2c = load_bc(T, "g2c7", gate_d[l, 3])
            lg2 = load_bc(T, "lg27", I["ln2_g"][l])
            lb2 = load_bc(T, "lb27", I["ln2_b"][l])
            xin_r = [T(f"xin7_{i}", [128, D], F32) for i in range(2)]
            yt_r = [T(f"yt7_{i}", [128, D], F32) for i in range(2)]
            xo_r = [T(f"xo7_{i}", [128, D], F32) for i in range(2)]
            nmr_r = [T(f"nmr7_{i}", [128, 1], F32) for i in range(2)]
            stt = [ln_stats(T, None, None, None, f"p7{i}") for i in range(2)]

            def finalize_steps(sbl):
                tiles = []
                at = 0
                for bi in sbl:
                    t0, nti = BLOCKS[bi]
                    for ti in range(nti):
                        tiles.append((at, t0 + ti * 128, g2c if t0 >= S else g2l))
                        at += 1
                steps = []

                def s1(at, tok, g2):
                    xin, Bxin = xin_r[at % 2]
                    yt, Byt = yt_r[at % 2]
                    A.dma(SP, lambda e: e.dma_start(out=xin[:], in_=xres[tok:tok + 128, :]), reads=[Bxres[tok // 128]], writes=[Bxin])
                    A.op(DVE, lambda e: e.tensor_tensor(out=yt[:], in0=acc[:, at, :], in1=g2[0][:], op=ALU.mult),
                         reads=[Bacc[at], g2[1]], writes=[Byt])
                    A.op(POOL, lambda e: e.tensor_tensor(out=yt[:], in0=yt[:], in1=xin[:], op=ALU.add), reads=[Byt, Bxin], writes=[Byt])

                def s2(at, tok, g2):
                    yt, Byt = yt_r[at % 2]
                    stt_ = stt[at % 2]
                    nmr, Bnmr = nmr_r[at % 2]
                    ln_compute(stt_, yt, Byt, LN_EPS / (ALPHA * ALPHA))
                    A.op(DVE, lambda e: e.tensor_scalar(out=nmr[:], in0=stt_[2][:, 0:1], scalar1=stt_[4][:], scalar2=-1.0,
                                                        op0=ALU.mult, op1=ALU.mult), reads=[stt_[3], stt_[5]], writes=[Bnmr])
                    A.op(ACT, lambda e: e.activation(out=yt[:], in_=yt[:], func=AF.Identity, scale=stt_[4][:], bias=nmr[:]),
                         reads=[Byt, stt_[5], Bnmr], writes=[Byt])

                def s3(at, tok, g2):
                    yt, Byt = yt_r[at % 2]
                    xo, Bxo = xo_r[at % 2]
                    A.op(DVE, lambda e: e.tensor_tensor(out=yt[:], in0=yt[:], in1=lg2[0][:], op=ALU.mult), reads=[Byt, lg2[1]], writes=[Byt])
                    A.op(POOL, lambda e: e.tensor_tensor(out=xo[:], in0=yt[:], in1=lb2[0][:], op=ALU.add), reads=[Byt, lb2[1]], writes=[Bxo])
                    if last:
                        A.dma(SP, lambda e: e.dma_start(out=out_d[tok:tok + 128, :], in_=xo[:]), reads=[Bxo], writes=[Bout])
                    else:
                        A.dma(SP, lambda e: e.dma_start(out=xres[tok:tok + 128, :], in_=xo[:]), reads=[Bxo], writes=[Bxres[tok // 128]])
                    if dbg:
                        A.dma(SP, lambda e: e.dma_start(out=DBG["x2"][tok:tok + 128, :], in_=xo[:]), reads=[Bxo], writes=[Bdbg])

                for p0 in range(0, len(tiles), 2):
                    pair = tiles[p0:p0 + 2]
                    for stage in (s1, s2, s3):
                        for tl in pair:
                            steps.append(lambda stage=stage, tl=tl: stage(*tl))
                    while len(steps) % 6:
                        steps.append(lambda: None)
                return steps

            pending = []
            n_drained = [0]

            def drain(n):
                while n > 0 and pending:
                    pending.pop(0)()
                    n_drained[0] += 1
                    n -= 1

            wctr = 0
            bctr = 0
            for sbl in sblocks:
                for pi_, (ex_, ch0, nch) in enumerate(pieces):
                    (wgq, Bwgq), (wuq, Bwuq), (wdq, Bwdq) = w_r[wctr % 2]
                    wctr += 1
                    c0 = ch0 * 128
                    c1 = c0 + nch * 128
                    sg_, su_, sd_ = wsrc(ex_)
                    A.dma(POOL, lambda e, wgq=wgq, sg_=sg_, c0=c0, c1=c1, nch=nch: e.dma_start(
                        out=wgq[:, :, 0:nch * 128], in_=sg_[:, c0:c1].rearrange("(k p) n -> p k n", p=128)), writes=[Bwgq])
                    A.dma(POOL, lambda e, wuq=wuq, su_=su_, c0=c0, c1=c1, nch=nch: e.dma_start(
                        out=wuq[:, :, 0:nch * 128], in_=su_[:, c0:c1].rearrange("(k p) n -> p k n", p=128)), writes=[Bwuq])
                    A.dma(POOL, lambda e, wdq=wdq, sd_=sd_, c0=c0, c1=c1, nch=nch: e.dma_start(
                        out=wdq[:, 0:nch, :], in_=sd_[c0:c1, :].rearrange("(k p) n -> p k n", p=128)), writes=[Bwdq])
                    firstw = (pi_ == 0)
                    at = 0
                    for bi in sbl:
                        t0, nti = BLOCKS[bi]
                        n = nti * 128
                        uT, BuT = uT_r[bctr % 2]
                        aT, BaT = aT_r[bctr % 2]
                        bctr += 1
                        A.dma(SP, lambda e, uT=uT, t0=t0, n=n: e.dma_start(
                            out=uT[:, :, 0:n], in_=uT_d[:, :, t0:t0 + n].rearrange("k p t -> p k t")), reads=[BuTd[bi]], writes=[BuT])
                        for fc in range(nch):
                            gb, ub = (fc % 2) * 2, (fc % 2) * 2 + 1
                            for (wt_, Bwt_, bank) in ((wgq, Bwgq, gb), (wuq, Bwuq, ub)):
                                for k in range(8):
                                    A.op(PE, lambda e, wt_=wt_, k=k, fc=fc, bank=bank, uT=uT, n=n: e.matmul(
                                        ps[:, bank, 0:n], lhsT=wt_[:, k, fc * 128:(fc + 1) * 128], rhs=uT[:, k, 0:n],
                                        start=(k == 0), stop=(k == 7)), reads=[Bwt_, BuT], writes=[Bps[bank]])
                            sg, Bsg = sg_r[fc % 2]
                            A.op(ACT, lambda e, sg=sg, gb=gb, n=n: e.activation(out=sg[:, 0:n], in_=ps[:, gb, 0:n], func=AF.Silu),
                                 reads=[Bps[gb]], writes=[Bsg])
                            A.op(DVE, lambda e, sg=sg, ub=ub, aT=aT, fc=fc, n=n: e.tensor_tensor(
                                out=aT[:, fc, 0:n], in0=ps[:, ub, 0:n], in1=sg[:, 0:n], op=ALU.mult),
                                reads=[Bps[ub], Bsg], writes=[BaT])
                            drain(2)
                        if firstw:
                            drain(6 * ((at + nti - 1) // 2 + 1) - n_drained[0])
                        for ti in range(nti):
                            gtile = t0 // 128 + ti
                            pb = 4 + 2 * (at % 2)
                            for nb in range(2):
                                for fc in range(nch):
                                    A.op(PE, lambda e, nb=nb, fc=fc, aT=aT, ti=ti, pb=pb, wdq=wdq, nch=nch: e.matmul(
                                        ps[:, pb + nb, :], lhsT=aT[:, fc, ti * 128:(ti + 1) * 128], rhs=wdq[:, fc, nb * 512:(nb + 1) * 512],
                                        start=(fc == 0), stop=(fc == nch - 1)), reads=[BaT, Bwdq], writes=[Bps[pb + nb]])
                            av = acc[:, at, :].rearrange("p (a c) -> p a c", a=2)
                            if moe:
                                cw_ = comb[:, gtile, ex_:ex_ + 1]
                                if firstw:
                                    A.op(DVE, lambda e, av=av, pb=pb, cw_=cw_: e.tensor_scalar(
                                        out=av, in0=ps[:, pb:pb + 2, :], scalar1=cw_, scalar2=None, op0=ALU.mult),
                                        reads=[Bps[pb], Bps[pb + 1], Bcomb], writes=[Bacc[at]])
                                else:
                                    A.op(DVE, lambda e, av=av, pb=pb, cw_=cw_: e.scalar_tensor_tensor(
                                        out=av, in0=ps[:, pb:pb + 2, :], scalar=cw_, in1=av, op0=ALU.mult, op1=ALU.add),
                                        reads=[Bps[pb], Bps[pb + 1], Bcomb, Bacc[at]], writes=[Bacc[at]])
                            else:
                                if firstw:
                                    A.op(ACT, lambda e, av=av, pb=pb: e.copy(out=av, in_=ps[:, pb:pb + 2, :]),
                                         reads=[Bps[pb], Bps[pb + 1]], writes=[Bacc[at]])
                                else:
                                    A.op(DVE, lambda e, av=av, pb=pb: e.tensor_tensor(out=av, in0=ps[:, pb:pb + 2, :], in1=av, op=ALU.add),
                                         reads=[Bps[pb], Bps[pb + 1], Bacc[at]], writes=[Bacc[at]])
                            at += 1
                drain(len(pending))
                pending.extend(finalize_steps(sbl))
                n_drained[0] = 0
            drain(len(pending))
            A.barrier()


    for l in range(n_layers):
        layer(l)

    A.barrier()
    A.emit()
    glob.close()
    return nc


_NC_CACHE = {}


def _in_maps(inputs):
    cst = _consts()
    maps = []
    shared = {}
    for k in INPUT_SHAPES:
        if k in ("x", "c", "ctx"):
            continue
        shared[k] = np.ascontiguousarray(np.asarray(inputs[k], dtype=np.float32))
    for b in range(8):
        m = dict(shared)
        m["x"] = np.ascontiguousarray(np.asarray(inputs["x"][b], dtype=np.float32))
        m["c"] = np.ascontiguousarray(np.asarray(inputs["c"][b], dtype=np.float32))
        m["ctx"] = np.ascontiguousarray(np.asarray(inputs["ctx"][b], dtype=np.float32))
        m.update(cst)
        maps.append(m)
    return maps


def kernel(**inputs):
    if "nc" not in _NC_CACHE:
        _NC_CACHE["nc"] = build()
    nc = _NC_CACHE["nc"]
    res = run_bass_kernel_spmd(nc, _in_maps(inputs), core_ids=list(range(8)))
    return np.stack([np.asarray(r["out"], dtype=np.float32) for r in res.results], axis=0)
```

```python
import math
from contextlib import ExitStack
import numpy as np
import ml_dtypes
import concourse.bass as bass
import concourse.mybir as mybir
from concourse.bass_utils import run_bass_kernel_spmd

F32 = mybir.dt.float32
BF16 = mybir.dt.bfloat16
AF = mybir.ActivationFunctionType
ALU = mybir.AluOpType
AX = mybir.AxisListType
NPBF = ml_dtypes.bfloat16

PE, ACT, DVE, POOL, SP = "pe", "act", "dve", "pool", "sp"
ENGINES = (PE, ACT, DVE, POOL, SP)

D = 1024
S = 4096
CTX = 256
TOK = S + CTX
NT = TOK // 128
DEPTH = 4
D_IN = 1792
D_FF = 2816
D_FFE = 3584
NEXP = 8
ALPHA = (2.0 * DEPTH) ** 0.25
LN_EPS = 1e-5
ADA_EPS = 1e-6
QK_EPS = 1e-6
BLOCKS = [(i * 512, 4) for i in range(8)] + [(4096, 2)]
MAGIC = 12582912.0
TWO_PI = 2.0 * math.pi


class Buf:
    __slots__ = ("name", "w", "r")

    def __init__(self, name=""):
        self.name = name
        self.w = None
        self.r = {}


class AutoSync:
    def __init__(self, nc, ring_sizes=None):
        self.nc = nc
        self.q = {e: [] for e in ENGINES}
        self.cnt = {}
        self.sems = {}
        self.seen = {e: {} for e in ENGINES}
        for e in (PE, ACT, DVE, POOL):
            self._mksem(e)
        ring_sizes = ring_sizes or {SP: 40, POOL: 24, ACT: 8}
        self.ring = {}
        self.ring_pos = {}
        for qn, n in ring_sizes.items():
            keys = []
            for i in range(n):
                k = f"dma_{qn}_{i}"
                self._mksem(k)
                keys.append(k)
            self.ring[qn] = keys
            self.ring_pos[qn] = 0
        self.n_instr = 0

    def _mksem(self, key):
        self.sems[key] = self.nc.alloc_semaphore(f"s_{key}")
        self.cnt[key] = 0

    def _collect(self, eng, reads, writes, skip_self):
        need = {}

        def add(d):
            if d is None:
                return
            k, v = d
            if skip_self and k == eng:
                return
            if need.get(k, 0) < v:
                need[k] = v
        for b in reads:
            add(b.w)
        for b in writes:
            add(b.w)
            for k, v in b.r.items():
                add((k, v))
        seen = self.seen[eng]
        for k, v in need.items():
            if seen.get(k, 0) < v:
                seen[k] = v
                self.q[eng].append(("wait", k, v))

    def _mark(self, me, reads, writes):
        k, v = me
        for b in reads:
            if b.r.get(k, 0) < v:
                b.r[k] = v
        for b in writes:
            b.w = me
            b.r = {}

    def op(self, eng, fn, reads=(), writes=()):
        self._collect(eng, reads, writes, skip_self=(eng == PE))
        self.cnt[eng] += 1
        me = (eng, self.cnt[eng])
        self.q[eng].append(("op", fn, eng, 1))
        self._mark(me, reads, writes)
        self.n_instr += 1

    def dma(self, qn, fn, reads=(), writes=()):
        ring = self.ring[qn]
        key = ring[self.ring_pos[qn]]
        self.ring_pos[qn] = (self.ring_pos[qn] + 1) % len(ring)
        prev = self.cnt[key]
        if prev and self.seen[qn].get(key, 0) < prev:
            self.seen[qn][key] = prev
            self.q[qn].append(("wait", key, prev))
        self._collect(qn, reads, writes, skip_self=False)
        self.cnt[key] += 16
        me = (key, self.cnt[key])
        self.q[qn].append(("op", fn, key, 16))
        self._mark(me, reads, writes)
        self.n_instr += 1

    def barrier(self):
        for e in ENGINES:
            for k, v in self.cnt.items():
                if v and self.seen[e].get(k, 0) < v and not (k == e and e == PE):
                    self.seen[e][k] = v
                    self.q[e].append(("wait", k, v))

    def emit(self):
        nc = self.nc
        sems = self.sems

        def run(q):
            def body(e):
                for it in q:
                    if it[0] == "wait":
                        e.wait_ge(sems[it[1]], it[2])
                    else:
                        it[1](e).then_inc(sems[it[2]], it[3])
            return body

        with nc.Block() as block:
            block.tensor(run(self.q[PE]))
            block.scalar(run(self.q[ACT]))
            block.vector(run(self.q[DVE]))
            block.gpsimd(run(self.q[POOL]))
            block.sync(run(self.q[SP]))


_CONST = None


def _trig_tables(L):
    N = 2 * L
    nt = L // 128
    f = np.arange(L, dtype=np.float64) + 0.5
    s = np.arange(L, dtype=np.float64)
    k = (np.outer(2 * np.arange(L, dtype=np.int64) + 1, np.arange(L, dtype=np.int64))) % (2 * N)
    ang = k.astype(np.float64) * (math.pi / N)
    c = np.cos(ang)
    sn = -np.sin(ang)
    del k, ang
    fwd = np.empty((nt, 128, 2, nt, 128), dtype=NPBF)
    inv = np.empty((nt, 128, 2, nt, 128), dtype=NPBF)
    for i, m in enumerate((c, sn)):
        m4 = m.reshape(nt, 128, nt, 128)
        fwd[:, :, i] = m4.transpose(0, 3, 2, 1).astype(NPBF)
        mi = (m * (2.0 / N)).reshape(nt, 128, nt, 128)
        inv[:, :, i] = mi.transpose(2, 1, 0, 3).astype(NPBF)
    return (np.ascontiguousarray(fwd.reshape(nt, 128, 2 * nt * 128)),
            np.ascontiguousarray(inv.reshape(nt, 128, 2 * nt * 128)))


def _hy_feats(L):
    t = np.linspace(0.0, 1.0, L, dtype=np.float32)[:, None]
    omega = (2.0 * math.pi * np.arange(L, dtype=np.float32)[:, None] / L).astype(np.float32)
    bands = np.linspace(1e-4, 16 - 1, 16, dtype=np.float32)[None, :]
    feats = np.concatenate([t, np.cos(omega * bands), -np.sin(omega * bands)], axis=-1).astype(np.float32)
    max_decay = math.log(1e-2) / 0.3
    min_decay = math.log(1e-2) / 1.5
    deltas = np.linspace(min_decay, max_decay, 256, dtype=np.float32)
    decay = np.exp(-t * np.abs(deltas)[None, :]).astype(np.float32)
    nt = L // 128
    decay_t = np.ascontiguousarray(decay.reshape(nt, 128, 256).transpose(1, 0, 2))
    return np.ascontiguousarray(feats.T), decay_t


def _pool_band():
    out = np.zeros((128, 20, 128), dtype=np.float64)
    L = 1024
    for g, win in enumerate((2, 4, 8, 16)):
        A = np.zeros((L, L))
        for t in range(L):
            lo = max(t - win // 2, 0)
            hi = min(t + win - win // 2, L)
            A[t, lo:hi] = 1.0 / (hi - lo)
            A[t, t] -= 1.0
        last = L - 128
        out[:, g * 5 + 0, :] = A[0:128, 0:128].T
        out[:, g * 5 + 1, :] = A[256:384, 256:384].T
        out[:, g * 5 + 2, :] = A[last:, last:].T
        out[:, g * 5 + 3, :] = A[256:384, 128:256].T
        out[:, g * 5 + 4, :] = A[256:384, 384:512].T
    return out.astype(NPBF)


def _rope_tables():
    rows = S // 64
    row = np.repeat(np.arange(rows), 64).astype(np.float32)
    col = np.tile(np.arange(64), rows).astype(np.float32)
    inv = (10000.0 ** (-np.arange(0, 32, 2, dtype=np.float32) / 32)).astype(np.float32)
    ang = np.concatenate([row[:, None] * inv, col[:, None] * inv], axis=-1)
    cos = np.cos(ang).astype(np.float32).reshape(S, 2, 1, 16)
    sin = np.sin(ang).astype(np.float32).reshape(S, 2, 1, 16)
    cosF = np.broadcast_to(cos, (S, 2, 2, 16)).reshape(S, 1, 64)
    sinF = np.concatenate([-sin, sin], axis=2).reshape(S, 1, 64)
    cosF = np.ascontiguousarray(np.broadcast_to(cosF, (S, 10, 64)).reshape(S, 640))
    sinF = np.ascontiguousarray(np.broadcast_to(sinF, (S, 10, 64)).reshape(S, 640))
    return cosF, sinF


def _consts():
    global _CONST
    if _CONST is None:
        fwd, inv = _trig_tables(S)
        fwdc, invc = _trig_tables(CTX)
        featsT, decay_t = _hy_feats(S)
        featsTc, decay_tc = _hy_feats(CTX)
        cosF, sinF = _rope_tables()
        _CONST = dict(
            k_fwd=fwd, k_inv=inv, k_fwdc=fwdc, k_invc=invc,
            k_feats=featsT, k_decay=decay_t, k_featsc=featsTc, k_decayc=decay_tc,
            k_cosF=cosF, k_sinF=sinF, k_band=_pool_band(),
            k_identb=np.eye(128).astype(NPBF), k_identf=np.eye(128, dtype=np.float32),
        )
    return _CONST


INPUT_SHAPES = dict(
    x=[S, D], c=[D], ctx=[CTX, D], c_ctx=[D], w_mod=[DEPTH, D, 6 * D], b_mod=[DEPTH, 6 * D],
    w_in=[DEPTH, D, D_IN], q_gain=[DEPTH, 64], k_gain=[DEPTH, 64], hy_conv_w=[DEPTH, 3, 768],
    hy_conv_b=[DEPTH, 768], hy_f_w1=[DEPTH, 33, 64], hy_f_b1=[DEPTH, 64], hy_f_freq=[DEPTH, 64],
    hy_f_w2=[DEPTH, 64, 64], hy_f_b2=[DEPTH, 64], hy_f_w3=[DEPTH, 64, 1024], hy_d=[DEPTH, 2, 256],
    pool_w=[DEPTH, 4, 64, 64], pool_scale=[DEPTH, 256], w_out=[DEPTH, D, D], ln1_g=[DEPTH, D],
    ln1_b=[DEPTH, D], ln2_g=[DEPTH, D], ln2_b=[DEPTH, D], ffn_w_gate=[2, D, D_FF], ffn_w_up=[2, D, D_FF],
    ffn_w_down=[2, D_FF, D], router_w=[2, D, NEXP], moe_w_gate=[2, NEXP, D, D_FFE],
    moe_w_up=[2, NEXP, D, D_FFE], moe_w_down=[2, NEXP, D_FFE, D],
)


def build(n_layers=DEPTH, dbg=False):
    nc = bass.Bass("TRN2", target_bir_lowering=False)
    A = AutoSync(nc)
    cst = _consts()
    I = {}
    for k, shp in INPUT_SHAPES.items():
        I[k] = nc.dram_tensor(k, shp, F32, kind="ExternalInput").ap()
    for k, v in cst.items():
        I[k] = nc.dram_tensor(k, list(v.shape), BF16 if v.dtype == NPBF else F32, kind="ExternalInput").ap()
    out_d = nc.dram_tensor("out", [S, D], F32, kind="ExternalOutput").ap()
    DBG = {}
    if dbg:
        DBG["cat"] = nc.dram_tensor("dbg_cat", [8, 128, TOK], BF16, kind="ExternalOutput").ap()
        DBG["x1"] = nc.dram_tensor("dbg_x1", [TOK, D], F32, kind="ExternalOutput").ap()
        DBG["x2"] = nc.dram_tensor("dbg_x2", [TOK, D], F32, kind="ExternalOutput").ap()
        DBG["mod"] = nc.dram_tensor("dbg_mod", [128, DEPTH * 96], F32, kind="ExternalOutput").ap()
        DBG["uT"] = nc.dram_tensor("dbg_uT", [8, 128, TOK], BF16, kind="ExternalOutput").ap()
        DBG["hy"] = nc.dram_tensor("dbg_hy", [6, 128, TOK], F32, kind="ExternalOutput").ap()

    xres = nc.dram_tensor("xres", [TOK, D], F32).ap()
    uT_d = nc.dram_tensor("uT_d", [8, 128, TOK], BF16).ap()
    hyraw = nc.dram_tensor("hyraw", [6, 128, TOK], F32).ap()
    catT = nc.dram_tensor("catT", [8, 128, TOK], BF16).ap()
    gate_d = nc.dram_tensor("gate_d", [DEPTH, 4, D], F32).ap()
    invn_d = nc.dram_tensor("invn_d", [2, 512], F32).ap()
    Bxres = [Buf(f"xres{i}") for i in range(NT)]
    BuTd = [Buf(f"uTd{i}") for i in range(9)]
    Bhyraw = Buf("hyraw")
    Bcat = [Buf(f"cat{i}") for i in range(9)]
    Bgate = Buf("gate_d")
    Binvn = Buf("invn")
    Bout = Buf("out")
    Bdbg = Buf("dbg")

    ps = nc.alloc_psum_tensor("ps", [128, 8, 512], F32)
    Bps = [Buf(f"ps{i}") for i in range(8)]

    def psb(bank):
        return ps[:, bank, :].bitcast(BF16)

    glob = ExitStack()
    _uid = [0]
    _orig_sbuf = nc.sbuf_tensor

    def _sbuf_unique(name, shape, dt, **kw):
        _uid[0] += 1
        return _orig_sbuf(f"{name}_{_uid[0]}", shape, dt, **kw)

    def GT(name, shape, dt):
        return glob.enter_context(_sbuf_unique(name, shape, dt)), Buf(name)

    identb, Bidb = GT("identb", [128, 128], BF16)
    identf, Bidf = GT("identf", [128, 128], F32)
    modT, BmodT = GT("modT", [128, DEPTH, 48, 2], F32)
    ones_b, Bones = GT("ones_b", [128, 1], BF16)
    A.dma(SP, lambda e: e.dma_start(out=identb[:], in_=I["k_identb"]), writes=[Bidb])
    A.dma(SP, lambda e: e.dma_start(out=identf[:], in_=I["k_identf"]), writes=[Bidf])
    A.op(DVE, lambda e: e.memset(ones_b[:], 1.0), writes=[Bones])

    def load_cols(T, dst, Bdst, vec, n):
        tmp, Btmp = T("lc_tmp", [n, 128], F32)
        A.dma(SP, lambda e: e.dma_start(out=tmp[:], in_=vec.rearrange("(n p) -> n p", p=128)), writes=[Btmp])
        A.op(PE, lambda e: e.transpose(out=ps[:, 7, 0:n], in_=tmp[:], identity=identf[0:n, 0:n]),
             reads=[Btmp, Bidf], writes=[Bps[7]])
        A.op(DVE, lambda e: e.tensor_copy(out=dst, in_=ps[:, 7, 0:n]), reads=[Bps[7]], writes=[Bdst])

    def ln_stats(T, xt, Bx, eps, tag):
        st, Bst = T(f"st_{tag}", [128, 2, 6], F32)
        mv, Bmv = T(f"mv_{tag}", [128, 2], F32)
        rs, Brs = T(f"rs_{tag}", [128, 1], F32)
        return (st, Bst, mv, Bmv, rs, Brs)

    def ln_compute(stt, xt, Bx, eps):
        st, Bst, mv, Bmv, rs, Brs = stt
        for h in range(2):
            A.op(DVE, lambda e, h=h: e.bn_stats(out=st[:, h, :], in_=xt[:, h * 512:(h + 1) * 512]),
                 reads=[Bx], writes=[Bst])
        A.op(DVE, lambda e: e.bn_aggr(out=mv[:], in_=st[:].rearrange("p a b -> p (a b)")), reads=[Bst], writes=[Bmv])
        A.op(ACT, lambda e: e.activation(out=rs[:], in_=mv[:, 1:2], func=AF.Sqrt, bias=float(eps), scale=1.0),
             reads=[Bmv], writes=[Brs])
        A.op(DVE, lambda e: e.reciprocal(out=rs[:], in_=rs[:]), reads=[Brs], writes=[Brs])

    def modulate_block(uh_tiles, nti, l, j, base, uT, BuT):
        for ti, (uh, Buh) in enumerate(uh_tiles):
            for k in range(8):
                A.op(PE, lambda e, k=k, ti=ti, uh=uh: e.transpose(
                    out=psb(k // 2)[:, (k % 2) * 512 + ti * 128:(k % 2) * 512 + (ti + 1) * 128],
                    in_=uh[:, k * 128:(k + 1) * 128], identity=identb[:]),
                    reads=[Buh, Bidb], writes=[Bps[k // 2]])
        n = nti * 128
        for k in range(8):
            eng = ACT if k % 2 == 0 else DVE
            src = psb(k // 2)[:, (k % 2) * 512:(k % 2) * 512 + n]
            sc = modT[:, l, base + 8 + k, j:j + 1]
            sh = modT[:, l, base + k, j:j + 1]
            if eng == ACT:
                A.op(ACT, lambda e, k=k, src=src, sc=sc, sh=sh: e.activation(
                    out=uT[:, k, 0:n], in_=src, func=AF.Identity, scale=sc, bias=sh),
                    reads=[Bps[k // 2], BmodT], writes=[BuT])
            else:
                A.op(DVE, lambda e, k=k, src=src, sc=sc, sh=sh: e.tensor_scalar(
                    out=uT[:, k, 0:n], in0=src, scalar1=sc, scalar2=sh, op0=ALU.mult, op1=ALU.add),
                    reads=[Bps[k // 2], BmodT], writes=[BuT])

    with ExitStack() as stk:
        def T(name, shape, dt):
            return stk.enter_context(_sbuf_unique(name, shape, dt)), Buf(name)
        cT, BcT = T("cT", [128, 8, 2], F32)
        sT, BsT = T("sT", [128, 8, 2], BF16)
        load_cols(T, cT[:, :, 0], BcT, I["c"], 8)
        load_cols(T, cT[:, :, 1], BcT, I["c_ctx"], 8)
        A.op(ACT, lambda e: e.activation(out=sT[:], in_=cT[:], func=AF.Silu), reads=[BcT], writes=[BsT])
        wm = [T(f"wm{i}", [128, 8, 512], BF16) for i in range(3)]
        bT, BbT = T("bT", [128, 48], F32)
        for l in range(n_layers):
            load_cols(T, bT[:], BbT, I["b_mod"][l], 48)
            for nb in range(12):
                w, Bw = wm[nb % 3]
                A.dma(POOL, lambda e, w=w, l=l, nb=nb: e.dma_start(
                    out=w[:], in_=I["w_mod"][l, :, nb * 512:(nb + 1) * 512].rearrange("(k p) n -> p k n", p=128)),
                    writes=[Bw])
                for jj in range(4):
                    nch = nb * 4 + jj
                    for k in range(8):
                        A.op(PE, lambda e, w=w, k=k, jj=jj, nch=nch: e.matmul(
                            ps[:, 6, nch * 2:nch * 2 + 2], lhsT=w[:, k, jj * 128:(jj + 1) * 128], rhs=sT[:, k, :],
                            start=(k == 0), stop=(k == 7)), reads=[Bw, BsT], writes=[Bps[6]])
            for j in range(2):
                A.op(DVE, lambda e, l=l, j=j: e.tensor_tensor(
                    out=modT[:, l, :, j], in0=ps[:, 6, 0:96].rearrange("p (n t) -> p n t", t=2)[:, :, j],
                    in1=bT[:], op=ALU.add), reads=[Bps[6], BbT], writes=[BmodT])
            for base in (8, 32):
                A.op(DVE, lambda e, l=l, base=base: e.tensor_scalar(
                    out=modT[:, l, base:base + 8, :], in0=modT[:, l, base:base + 8, :], scalar1=1.0, scalar2=None,
                    op0=ALU.add), reads=[BmodT], writes=[BmodT])
            for base in (16, 40):
                A.op(DVE, lambda e, l=l, base=base: e.tensor_scalar(
                    out=modT[:, l, base:base + 8, :], in0=modT[:, l, base:base + 8, :], scalar1=1.0 / ALPHA,
                    scalar2=None, op0=ALU.mult), reads=[BmodT], writes=[BmodT])
            for gi, base in enumerate((16, 40)):
                for j in range(2):
                    A.dma(SP, lambda e, l=l, gi=gi, base=base, j=j: e.dma_start(
                        out=gate_d[l, gi * 2 + j].rearrange("(k p) -> p k", p=128),
                        in_=modT[:, l, base:base + 8, j], allow_slow_non_contiguous=True),
                        reads=[BmodT], writes=[Bgate])
        if dbg:
            A.dma(SP, lambda e: e.dma_start(out=DBG["mod"], in_=modT[:].rearrange("p l n t -> p (l n t)")),
                  reads=[BmodT], writes=[Bdbg])
        A.barrier()

    def layer(l):
        last = (l == DEPTH - 1)
        moe = (l % 2 == 1)
        jf = l // 2
        nblk = 8 if last else 9
        xsrc = (lambda t0, n: (I["x"][t0:t0 + n, :] if t0 < S else I["ctx"][t0 - S:t0 - S + n, :])) if l == 0 \
            else (lambda t0, n: xres[t0:t0 + n, :])

        with ExitStack() as stk:
            def T(name, shape, dt):
                return stk.enter_context(_sbuf_unique(name, shape, dt)), Buf(name)
            qT, BqT = T("qT", [128, 4, TOK], BF16)
            kT, BkT = T("kT", [128, 4, TOK], BF16)
            vp, Bvp = T("vx", [128, NT, 2, 128], BF16)
            pl_tm, Bpl = T("pl_tm", [128, NT, 256], BF16)
            A.op(DVE, lambda e: e.memset(vp[:], 1.0), writes=[Bvp])
            with ExitStack() as stk1:
                def T1(name, shape, dt):
                    return stk1.enter_context(_sbuf_unique(name, shape, dt)), Buf(name)
                wtm, Bwtm = T1("wtm", [128, 8, 1024], BF16)
                whm, Bwhm = T1("whm", [128, 8, 768], BF16)
                wv = I["w_in"][l].rearrange("(k p) n -> p k n", p=128)
                A.dma(POOL, lambda e: e.dma_start(out=wtm[:, :, 0:768], in_=wv[:, :, 0:768]), writes=[Bwtm])
                A.dma(POOL, lambda e: e.dma_start(out=wtm[:, :, 768:1024], in_=wv[:, :, 1536:1792]), writes=[Bwtm])
                A.dma(POOL, lambda e: e.dma_start(out=whm[:], in_=wv[:, :, 768:1536]), writes=[Bwhm])
                gain, Bgain = T1("gain", [128, 640], F32)
                A.dma(SP, lambda e: e.dma_start(out=gain[:, 0:64], in_=I["q_gain"][l].partition_broadcast(128)), writes=[Bgain])
                A.dma(SP, lambda e: e.dma_start(out=gain[:, 512:576], in_=I["k_gain"][l].partition_broadcast(128)), writes=[Bgain])
                A.op(DVE, lambda e: e.tensor_scalar(out=gain[:, 0:64], in0=gain[:, 0:64], scalar1=0.125, scalar2=None,
                                                    op0=ALU.mult), reads=[Bgain], writes=[Bgain])
                for h in range(1, 8):
                    A.op(DVE, lambda e, h=h: e.tensor_copy(out=gain[:, h * 64:(h + 1) * 64], in_=gain[:, 0:64]),
                         reads=[Bgain], writes=[Bgain])
                A.op(DVE, lambda e: e.tensor_copy(out=gain[:, 576:640], in_=gain[:, 512:576]), reads=[Bgain], writes=[Bgain])
                xt_r = [T1(f"xt{i}", [128, D], F32) for i in range(3)]
                uh_r = [T1(f"uh{i}", [128, D], BF16) for i in range(6)]
                uT_r = [T1(f"uTb{i}", [128, 8, 512], BF16) for i in range(2)]
                stt = [ln_stats(T1, None, None, None, f"a{i}") for i in range(2)]
                qk_r = [T1(f"qk{i}", [128, 640], F32) for i in range(2)]
                sq, Bsq = T1("sq", [128, 640], F32)
                ssq, Bssq = T1("ssq", [128, 10], F32)
                cosr = [T1(f"cos{i}", [128, 640], F32) for i in range(2)]
                sinr = [T1(f"sin{i}", [128, 640], F32) for i in range(2)]
                t1, Bt1 = T1("t1", [128, 640], F32)
                t2, Bt2 = T1("t2", [128, 640], F32)
                qkr_r = [T1(f"qkr{i}", [128, 1024], BF16) for i in range(2)]
                for qkr_, Bqkr_ in qkr_r:
                    A.op(POOL, lambda e, qkr_=qkr_: e.memset(qkr_[:, 512:1024], 0.0), writes=[Bqkr_])
                hys_r = [T1(f"hys{i}", [128, 512], F32) for i in range(2)]
                tile_ctr = 0
                blk_g0 = []
                _g = 0
                for (_t0, _n) in BLOCKS:
                    blk_g0.append(_g)
                    _g += _n
                uhs_blk = [[] for _ in BLOCKS]

                def ln_part(bj, tj):
                    t0_, _ = BLOCKS[bj]
                    g = blk_g0[bj] + tj
                    xt, Bx = xt_r[g % 3]
                    uh, Buh = uh_r[g % 6]
                    A.dma(SP, lambda e: e.dma_start(out=xt[:], in_=xsrc(t0_ + tj * 128, 128)),
                          reads=[Bxres[t0_ // 128 + tj]], writes=[Bx])
                    s_ = stt[g % 2]
                    ln_compute(s_, xt, Bx, ADA_EPS)
                    A.op(DVE, lambda e: e.tensor_scalar(
                        out=uh[:], in0=xt[:], scalar1=s_[2][:, 0:1], scalar2=s_[4][:], op0=ALU.subtract, op1=ALU.mult),
                        reads=[Bx, s_[3], s_[5]], writes=[Buh])
                    uhs_blk[bj].append((uh, Buh))

                for tj in range(BLOCKS[0][1]):
                    ln_part(0, tj)
                deferred_back = [None]
                for bi, (t0, nti) in enumerate(BLOCKS):
                    is_ctx = (t0 >= S)
                    jm = 1 if is_ctx else 0
                    n = nti * 128
                    uT, BuT = uT_r[bi % 2]
                    nti_next = BLOCKS[bi + 1][1] if bi + 1 < len(BLOCKS) else 0
                    modulate_block(uhs_blk[bi], nti, l, jm, 0, uT, BuT)
                    if dbg:
                        A.dma(SP, lambda e, uT=uT, t0=t0, n=n: e.dma_start(
                            out=DBG["uT"][:, :, t0:t0 + n].rearrange("k p t -> p k t"), in_=uT[:, :, 0:n]),
                            reads=[BuT], writes=[Bdbg])
                    if not (last and is_ctx):
                        for hc in range(6):
                            bank = 6 + (hc % 2)
                            for k in range(8):
                                A.op(PE, lambda e, hc=hc, k=k, bank=bank, uT=uT, n=n: e.matmul(
                                    ps[:, bank, 0:n], lhsT=whm[:, k, hc * 128:(hc + 1) * 128], rhs=uT[:, k, 0:n],
                                    start=(k == 0), stop=(k == 7)), reads=[Bwhm, BuT], writes=[Bps[bank]])
                            hs, Bhs = hys_r[hc % 2]
                            A.op(ACT, lambda e, hs=hs, bank=bank, n=n: e.copy(out=hs[:, 0:n], in_=ps[:, bank, 0:n]),
                                 reads=[Bps[bank]], writes=[Bhs])
                            A.dma(SP, lambda e, hs=hs, hc=hc, t0=t0, n=n: e.dma_start(
                                out=hyraw[hc, :, t0:t0 + n], in_=hs[:, 0:n]), reads=[Bhs], writes=[Bhyraw])
                    for ti in range(nti):
                        g = tile_ctr + ti
                        tok = t0 + ti * 128
                        for nb in range(2):
                            bank = 4 + nb
                            for k in range(8):
                                A.op(PE, lambda e, nb=nb, k=k, bank=bank, uT=uT, ti=ti: e.matmul(
                                    ps[:, bank, :], lhsT=uT[:, k, ti * 128:(ti + 1) * 128],
                                    rhs=wtm[:, k, nb * 512:(nb + 1) * 512], start=(k == 0), stop=(k == 7)),
                                    reads=[Bwtm, BuT], writes=[Bps[bank]])
                        qk, Bqk = qk_r[g % 2]
                        A.op(ACT, lambda e, qk=qk: e.copy(out=qk[:, 0:512], in_=ps[:, 4, :]), reads=[Bps[4]], writes=[Bqk])
                        A.op(ACT, lambda e, qk=qk: e.copy(out=qk[:, 512:640], in_=ps[:, 5, 0:128]), reads=[Bps[5]], writes=[Bqk])
                        A.op(ACT, lambda e, g=g: e.copy(out=vp[:, g, :, 0:64],
                                                        in_=ps[:, 5, 128:256].rearrange("p (a b) -> p a b", b=64)),
                             reads=[Bps[5]], writes=[Bvp])
                        A.op(ACT, lambda e, g=g: e.copy(out=pl_tm[:, g, :], in_=ps[:, 5, 256:512]),
                             reads=[Bps[5]], writes=[Bpl])
                        A.op(DVE, lambda e, qk=qk: e.tensor_tensor(out=sq[:], in0=qk[:], in1=qk[:], op=ALU.mult),
                             reads=[Bqk], writes=[Bsq])
                        A.op(DVE, lambda e: e.tensor_reduce(out=ssq[:], in_=sq[:].rearrange("p (h d) -> p h d", d=64),
                                                            axis=AX.X, op=ALU.add), reads=[Bsq], writes=[Bssq])
                        A.op(ACT, lambda e: e.activation(out=ssq[:], in_=ssq[:], func=AF.Sqrt, bias=QK_EPS, scale=1.0 / 64),
                             reads=[Bssq], writes=[Bssq])
                        A.op(DVE, lambda e: e.reciprocal(out=ssq[:], in_=ssq[:]), reads=[Bssq], writes=[Bssq])
                        A.op(DVE, lambda e, qk=qk: e.tensor_tensor(
                            out=qk[:].rearrange("p (h d) -> p h d", d=64), in0=qk[:].rearrange("p (h d) -> p h d", d=64),
                            in1=ssq[:].unsqueeze(2).to_broadcast([128, 10, 64]), op=ALU.mult), reads=[Bqk, Bssq], writes=[Bqk])
                        qkr, Bqkr = qkr_r[g % 2]
                        kd4 = qkr[:, 512:1024].rearrange("p (a b c d) -> p a b c d", a=2, b=2, c=2)
                        if is_ctx:
                            A.op(DVE, lambda e, qk=qk, qkr=qkr: e.tensor_tensor(
                                out=qkr[:, 0:512], in0=qk[:, 0:512], in1=gain[:, 0:512], op=ALU.mult),
                                reads=[Bqk, Bgain], writes=[Bqkr])
                            for b2 in range(2):
                                A.op(DVE, lambda e, qk=qk, kd4=kd4, b2=b2: e.tensor_tensor(
                                    out=kd4[:, :, b2, b2, :], in0=qk[:, 512:640].rearrange("p (a d) -> p a d", d=64),
                                    in1=gain[:, 512:640].rearrange("p (a d) -> p a d", d=64), op=ALU.mult),
                                    reads=[Bqk, Bgain], writes=[Bqkr])
                        else:
                            cs_, Bcs = cosr[g % 2]
                            sn_, Bsn = sinr[g % 2]
                            A.dma(SP, lambda e, cs_=cs_, tok=tok: e.dma_start(out=cs_[:], in_=I["k_cosF"][tok:tok + 128, :]), writes=[Bcs])
                            A.dma(SP, lambda e, sn_=sn_, tok=tok: e.dma_start(out=sn_[:], in_=I["k_sinF"][tok:tok + 128, :]), writes=[Bsn])
                            A.op(DVE, lambda e, qk=qk: e.tensor_tensor(out=qk[:], in0=qk[:], in1=gain[:], op=ALU.mult),
                                 reads=[Bqk, Bgain], writes=[Bqk])
                            A.op(DVE, lambda e, qk=qk, cs_=cs_: e.tensor_tensor(out=t1[:], in0=qk[:], in1=cs_[:], op=ALU.mult),
                                 reads=[Bqk, Bcs], writes=[Bt1])
                            qv = qk[:].rearrange("p (h s d) -> p h s d", s=2, d=16)
                            sv = sn_[:].rearrange("p (h s d) -> p h s d", s=2, d=16)
                            tv = t2[:].rearrange("p (h s d) -> p h s d", s=2, d=16)
                            for s2 in range(2):
                                A.op(POOL, lambda e, s2=s2, qv=qv, sv=sv, tv=tv: e.tensor_tensor(
                                    out=tv[:, :, s2, :], in0=qv[:, :, 1 - s2, :], in1=sv[:, :, s2, :], op=ALU.mult),
                                    reads=[Bqk, Bsn], writes=[Bt2])
                            A.op(DVE, lambda e, qkr=qkr: e.tensor_tensor(out=qkr[:, 0:512], in0=t1[:, 0:512], in1=t2[:, 0:512], op=ALU.add),
                                 reads=[Bt1, Bt2], writes=[Bqkr])
                            for b2 in range(2):
                                A.op(DVE, lambda e, kd4=kd4, b2=b2: e.tensor_tensor(
                                    out=kd4[:, :, b2, b2, :], in0=t1[:, 512:640].rearrange("p (a d) -> p a d", d=64),
                                    in1=t2[:, 512:640].rearrange("p (a d) -> p a d", d=64), op=ALU.add),
                                    reads=[Bt1, Bt2], writes=[Bqkr])
                        def back_part(qkr=qkr, Bqkr=Bqkr, tok=tok):
                            for c6 in range(8):
                                A.op(PE, lambda e, c6=c6: e.transpose(
                                    out=psb(6)[:, c6 * 128:(c6 + 1) * 128], in_=qkr[:, c6 * 128:(c6 + 1) * 128], identity=identb[:]),
                                    reads=[Bqkr, Bidb], writes=[Bps[6]])
                            A.op(DVE, lambda e: e.tensor_copy(
                                out=qT[:, :, tok:tok + 128], in_=psb(6)[:, 0:512].rearrange("p (a t) -> p a t", t=128)),
                                reads=[Bps[6]], writes=[BqT])
                            A.op(DVE, lambda e: e.tensor_copy(
                                out=kT[:, :, tok:tok + 128], in_=psb(6)[:, 512:1024].rearrange("p (a t) -> p a t", t=128)),
                                reads=[Bps[6]], writes=[BkT])
                        if deferred_back[0] is not None:
                            deferred_back[0]()
                        deferred_back[0] = back_part
                        if ti < nti_next:
                            ln_part(bi + 1, ti)
                    if deferred_back[0] is not None:
                        deferred_back[0]()
                        deferred_back[0] = None
                    for tj in range(nti, nti_next):
                        ln_part(bi + 1, tj)
                    tile_ctr += nti
                A.barrier()

            if True:
                with ExitStack() as stk2:
                    def T2(name, shape, dt):
                        return stk2.enter_context(_sbuf_unique(name, shape, dt)), Buf(name)
                    band, Bband = T2("band", [128, 20, 128], BF16)
                    pw, Bpw = T2("pw", [128, 2, 64], BF16)
                    psc, Bpsc = T2("psc", [128, 2], F32)
                    A.dma(SP, lambda e: e.dma_start(out=band[:], in_=I["k_band"]), writes=[Bband])
                    A.dma(POOL, lambda e: e.dma_start(out=pw[:], in_=I["pool_w"][l].rearrange("(a b) c d -> (b c) a d", b=2)), writes=[Bpw])
                    load_cols(T2, psc[:], Bpsc, I["pool_scale"][l], 2)
                    yp_r = [T2(f"yp{i}", [128, 2, 128], BF16) for i in range(2)]
                    po_r = [T2(f"po{i}", [128, 2, 512], BF16) for i in range(2)]
                    seqs = [(0, 32)] if last else [(0, 32), (32, 2)]
                    for (g0, ng) in seqs:
                        for tb in range(0, ng, 4):
                            nb4 = min(4, ng - tb)
                            po, Bpo = po_r[(tb // 4) % 2]
                            for tq in range(nb4):
                                tt = tb + tq
                                g = g0 + tt
                                yp, Byp = yp_r[g % 2]
                                for grp in range(4):
                                    srcs = []
                                    if tt > 0:
                                        srcs.append((g - 1, 3))
                                    srcs.append((g, 0 if tt == 0 else (2 if tt == ng - 1 else 1)))
                                    if tt < ng - 1:
                                        srcs.append((g + 1, 4))
                                    h2 = (grp % 2) * 64
                                    for si, (sg, kind) in enumerate(srcs):
                                        A.op(PE, lambda e, grp=grp, sg=sg, kind=kind, si=si, ns=len(srcs), h2=h2: e.matmul(
                                            ps[h2:h2 + 64, 6, (grp // 2) * 128:(grp // 2 + 1) * 128],
                                            lhsT=pl_tm[:, sg, grp * 64:(grp + 1) * 64], rhs=band[:, grp * 5 + kind, :],
                                            start=(si == 0), stop=(si == ns - 1), skip_group_check=True),
                                            reads=[Bpl, Bband], writes=[Bps[6]])
                                A.op(ACT, lambda e, yp=yp: e.copy(out=yp[:], in_=ps[:, 6, 0:256].rearrange("p (a t) -> p a t", t=128)),
                                     reads=[Bps[6]], writes=[Byp])
                                for grp in range(4):
                                    h2 = (grp % 2) * 64
                                    A.op(PE, lambda e, grp=grp, h2=h2, yp=yp: e.matmul(
                                        ps[h2:h2 + 64, 7, (grp // 2) * 128:(grp // 2 + 1) * 128],
                                        lhsT=pw[h2:h2 + 64, grp // 2, :], rhs=yp[h2:h2 + 64, grp // 2, :],
                                        start=True, stop=True, skip_group_check=True), reads=[Bpw, Byp], writes=[Bps[7]])
                                for a in range(2):
                                    A.op(ACT, lambda e, a=a, po=po, tq=tq: e.activation(
                                        out=po[:, a, tq * 128:(tq + 1) * 128], in_=ps[:, 7, a * 128:(a + 1) * 128],
                                        func=AF.Copy, scale=psc[:, a:a + 1]), reads=[Bps[7], Bpsc], writes=[Bpo])
                            tok0 = (g0 + tb) * 128
                            bi = min(tok0 // 512, 8)
                            A.dma(SP, lambda e, po=po, tok0=tok0, nb4=nb4: e.dma_start(
                                out=catT[6:8, :, tok0:tok0 + nb4 * 128].rearrange("k p t -> p k t"), in_=po[:, :, 0:nb4 * 128]),
                                reads=[Bpo], writes=[Bcat[bi]])
                    A.barrier()

            with ExitStack() as stk3:
                def T3(name, shape, dt):
                    return stk3.enter_context(_sbuf_unique(name, shape, dt)), Buf(name)
                ex_r = [T3(f"ex{i}", [128, 2, 512], BF16) for i in range(4)]
                rc_r = [T3(f"rc{i}", [64, 512], F32) for i in range(2)]
                ca_r = [T3(f"ca{i}", [128, 4, 512], BF16) for i in range(2)]
                SB = (0, 2, 6)
                qblocks = [(i * 512, 4, list(range(NT))) for i in range(8)]
                if not last:
                    qblocks.append((S, 2, [32, 33]))
                units = []
                for qi, (q0, nq, kts) in enumerate(qblocks):
                    for h in range(8):
                        pairs = [kts[i:i + 2] for i in range(0, len(kts), 2)]
                        for pi, pk in enumerate(pairs):
                            units.append((qi, q0, nq, h, pi, len(pairs), pk))

                def emit_qk(ui):
                    qi, q0, nq, h, pi, npairs, pk = units[ui]
                    nqt = nq * 128
                    kv, hf, pr = h // 4, (h % 2) * 64, h // 2
                    sb0 = SB[ui % 3]
                    for j2, kt in enumerate(pk):
                        A.op(PE, lambda e, kt=kt, j2=j2, sb0=sb0, h=h, kv=kv, pr=pr, q0=q0, nqt=nqt: e.matmul(
                            ps[:, sb0 + j2, 0:nqt], lhsT=kT[:, kv * 2 + (h % 2), kt * 128:(kt + 1) * 128],
                            rhs=qT[:, pr, q0:q0 + nqt], start=True, stop=True),
                            reads=[BkT, BqT], writes=[Bps[sb0 + j2]])

                LOOK = 2
                for ui in range(min(LOOK, len(units))):
                    emit_qk(ui)
                for ui, (qi, q0, nq, h, pi, npairs, pk) in enumerate(units):
                    nqt = nq * 128
                    kv, hf, pr = h // 4, (h % 2) * 64, h // 2
                    obank = 4 + (h % 2)
                    sb0 = SB[ui % 3]
                    npk = len(pk)
                    ex, Bex = ex_r[ui % 4]
                    A.op(ACT, lambda e, ex=ex, sb0=sb0, npk=npk, nqt=nqt: e.activation(
                        out=ex[:, 0:npk, 0:nqt], in_=ps[:, sb0:sb0 + npk, 0:nqt], func=AF.Exp),
                        reads=[Bps[sb0 + i] for i in range(npk)], writes=[Bex])
                    if ui + LOOK < len(units):
                        emit_qk(ui + LOOK)
                    for j2, kt in enumerate(pk):
                        first = (pi == 0 and j2 == 0)
                        lastmm = (pi == npairs - 1 and j2 == npk - 1)
                        A.op(PE, lambda e, ex=ex, j2=j2, kt=kt, kv=kv, obank=obank, first=first, lastmm=lastmm, nqt=nqt: e.matmul(
                            ps[:, obank, 0:nqt], lhsT=vp[:, kt, kv, :], rhs=ex[:, j2, 0:nqt],
                            start=first, stop=lastmm), reads=[Bex, Bvp], writes=[Bps[obank]])
                    if pi == npairs - 1:
                        ca, Bca = ca_r[qi % 2]
                        rc, Brc = rc_r[h % 2]
                        A.op(DVE, lambda e, rc=rc, obank=obank, nqt=nqt: e.reciprocal(
                            out=rc[0:64, 0:nqt], in_=ps[64:128, obank, 0:nqt]), reads=[Bps[obank]], writes=[Brc])
                        A.op(DVE, lambda e, rc=rc, ca=ca, obank=obank, nqt=nqt, hf=hf, pr=pr: e.tensor_tensor(
                            out=ca[hf:hf + 64, pr, 0:nqt], in0=ps[0:64, obank, 0:nqt], in1=rc[0:64, 0:nqt], op=ALU.mult),
                            reads=[Bps[obank], Brc], writes=[Bca])
                        if h == 7:
                            bi = min(q0 // 512, 8)
                            A.dma(SP, lambda e, ca=ca, q0=q0, nqt=nqt: e.dma_start(
                                out=catT[0:4, :, q0:q0 + nqt].rearrange("k p t -> p k t"), in_=ca[:, :, 0:nqt]),
                                reads=[Bca], writes=[Bcat[bi]])
                A.barrier()

        def hyena(seq_t0, L, fwd_d, inv_d, feats_d, decay_d):
            nt = L // 128
            nfb = max(L // 512, 1)
            fbw = min(L, 512)
            with ExitStack() as stk:
                def T(name, shape, dt):
                    return stk.enter_context(_sbuf_unique(name, shape, dt)), Buf(name)
                Ksp, BK = T("Ksp", [128, nt, 2, 512], BF16)
                slab_r = [T(f"hslab{i}", [128, nt, 128], BF16) for i in range(4)]
                slab_ctr = [0]

                def load_half(src_d, idx, cs):
                    sl, Bsl = slab_r[slab_ctr[0] % 4]
                    qn = SP if slab_ctr[0] % 2 == 0 else ACT
                    slab_ctr[0] += 1
                    A.dma(qn, lambda e, sl=sl, idx=idx, cs=cs: e.dma_start(
                        out=sl[:].rearrange("p b c -> p (b c)"), in_=src_d[idx][:, cs * nt * 128:(cs + 1) * nt * 128]),
                        writes=[Bsl])
                    return sl, Bsl

                with ExitStack() as stkf:
                    def TF(name, shape, dt):
                        return stkf.enter_context(_sbuf_unique(name, shape, dt)), Buf(name)
                    Ptm, BP = TF("Ptm", [128, nt, 512], BF16)
                    Qtm, BQ = TF("Qtm", [128, nt, 512], BF16)
                    fe_r = [TF(f"feats{i}", [33, 512], F32) for i in range(2)]
                    dc_r = [TF(f"decay{i}", [128, 256], F32) for i in range(2)]
                    w1, Bw1 = TF("w1", [33, 64], F32)
                    w2, Bw2 = TF("w2", [64, 64], F32)
                    w3, Bw3 = TF("w3", [64, 1024], F32)
                    fb, Bfb = TF("fb", [64, 3], F32)
                    h1, Bh1 = TF("h1", [64, 512], F32)
                    h2, Bh2 = TF("h2", [64, 512], F32)
                    arg, Barg = TF("arg", [64, 512], F32)
                    rr, Brr = TF("rr", [64, 512], F32)
                    A.dma(SP, lambda e: e.dma_start(out=w1[:], in_=I["hy_f_w1"][l]), writes=[Bw1])
                    A.dma(SP, lambda e: e.dma_start(out=w2[:], in_=I["hy_f_w2"][l]), writes=[Bw2])
                    A.dma(SP, lambda e: e.dma_start(out=w3[:], in_=I["hy_f_w3"][l]), writes=[Bw3])
                    for ci, nm in enumerate(("hy_f_b1", "hy_f_freq", "hy_f_b2")):
                        A.dma(SP, lambda e, ci=ci, nm=nm: e.dma_start(
                            out=fb[:, ci:ci + 1], in_=I[nm][l].rearrange("(p o) -> p o", o=1)), writes=[Bfb])

                    def sin_layer(wt, Bwt, kdim, src, Bsrc, bcol, dst, Bdst):
                        A.op(PE, lambda e: e.matmul(ps[0:64, 0, 0:fbw], lhsT=wt[0:kdim, :], rhs=src[0:kdim, 0:fbw],
                                                    start=True, stop=True), reads=[Bwt, Bsrc], writes=[Bps[0]])
                        A.op(DVE, lambda e: e.tensor_scalar(out=arg[:, 0:fbw], in0=ps[0:64, 0, 0:fbw], scalar1=fb[:, bcol:bcol + 1],
                                                            scalar2=fb[:, 1:2], op0=ALU.add, op1=ALU.mult),
                             reads=[Bps[0], Bfb], writes=[Barg])
                        A.op(DVE, lambda e: e.tensor_scalar(out=rr[:, 0:fbw], in0=arg[:, 0:fbw], scalar1=1.0 / TWO_PI, scalar2=MAGIC,
                                                            op0=ALU.mult, op1=ALU.add), reads=[Barg], writes=[Brr])
                        A.op(DVE, lambda e: e.tensor_scalar(out=rr[:, 0:fbw], in0=rr[:, 0:fbw], scalar1=-MAGIC, scalar2=-TWO_PI,
                                                            op0=ALU.add, op1=ALU.mult), reads=[Brr], writes=[Brr])
                        A.op(DVE, lambda e: e.tensor_tensor(out=arg[:, 0:fbw], in0=arg[:, 0:fbw], in1=rr[:, 0:fbw], op=ALU.add),
                             reads=[Barg, Brr], writes=[Barg])
                        A.op(DVE, lambda e: e.tensor_scalar(out=arg[:, 0:fbw], in0=arg[:, 0:fbw], scalar1=math.pi, scalar2=-math.pi,
                                                            op0=ALU.min, op1=ALU.max), reads=[Barg], writes=[Barg])
                        A.op(ACT, lambda e: e.activation(out=dst[:, 0:fbw], in_=arg[:, 0:fbw], func=AF.Sin),
                             reads=[Barg], writes=[Bdst])
                    hd_r = [TF(f"hd{i}", [128, 2, 2, 256], F32) for i in range(2)]
                    ab_r = [TF(f"ab{i}", [128, 1024], BF16) for i in range(2)]
                    for fbk in range(nfb):
                        feats, Bfe = fe_r[fbk % 2]
                        A.dma(SP, lambda e, feats=feats, fbk=fbk: e.dma_start(out=feats[:, 0:fbw], in_=feats_d[:, fbk * fbw:(fbk + 1) * fbw]),
                              writes=[Bfe])
                        sin_layer(w1, Bw1, 33, feats, Bfe, 0, h1, Bh1)
                        sin_layer(w2, Bw2, 64, h1, Bh1, 2, h2, Bh2)
                        for jj in range(fbw // 128):
                            j = fbk * (fbw // 128) + jj
                            hd, Bhd = hd_r[j % 2]
                            ab, Bab = ab_r[j % 2]
                            decay, Bdec = dc_r[j % 2]
                            A.dma(SP, lambda e, decay=decay, j=j: e.dma_start(out=decay[:], in_=decay_d[:, j, :]), writes=[Bdec])
                            for o in range(2):
                                A.op(PE, lambda e, o=o, jj=jj: e.matmul(ps[:, 1 + o, :], lhsT=h2[:, jj * 128:(jj + 1) * 128],
                                                                        rhs=w3[:, o * 512:(o + 1) * 512], start=True, stop=True),
                                     reads=[Bh2, Bw3], writes=[Bps[1 + o]])
                                for dr in range(2):
                                    A.op(DVE, lambda e, o=o, dr=dr, hd=hd, decay=decay: e.tensor_tensor(
                                        out=hd[:, o, dr, :], in0=ps[:, 1 + o, dr * 256:(dr + 1) * 256], in1=decay[:], op=ALU.mult),
                                        reads=[Bps[1 + o], Bdec], writes=[Bhd])
                            A.op(DVE, lambda e, hd=hd, ab=ab: e.scalar_tensor_tensor(
                                out=ab[:], in0=hd[:].rearrange("p a b c -> p (a b c)"), scalar=-1.0,
                                in1=hd[:].rearrange("p a b c -> p (a b c)"), op0=ALU.mult, op1=ALU.max),
                                reads=[Bhd], writes=[Bab])
                            for o in range(2):
                                A.op(PE, lambda e, o=o, j=j, ab=ab: e.matmul(ps[0:1, 3 + o, :], lhsT=ones_b[:, 0:1], rhs=ab[:, o * 512:(o + 1) * 512],
                                                                             start=(j == 0), stop=(j == nt - 1)),
                                     reads=[Bones, Bab], writes=[Bps[3 + o]])
                            A.op(POOL, lambda e, hd=hd, j=j: e.tensor_tensor(
                                out=Ptm[:, j, :].rearrange("p (o c) -> p o c", o=2), in0=hd[:, :, 0, :], in1=hd[:, :, 1, :], op=ALU.add),
                                reads=[Bhd], writes=[BP])
                            A.op(POOL, lambda e, hd=hd, j=j: e.tensor_tensor(
                                out=Qtm[:, j, :].rearrange("p (o c) -> p o c", o=2), in0=hd[:, :, 0, :], in1=hd[:, :, 1, :], op=ALU.subtract),
                                reads=[Bhd], writes=[BQ])
                            if j == 0:
                                for dst, Bd in ((Ptm, BP), (Qtm, BQ)):
                                    A.op(POOL, lambda e, hd=hd, dst=dst: e.tensor_copy(
                                        out=dst[0:1, 0, :].rearrange("p (o c) -> p o c", o=2), in_=hd[0:1, :, 0, :]),
                                        reads=[Bhd], writes=[Bd])
                    nrm, Bnrm = TF("nrm", [1, 2, 256], F32)
                    invn, Binv = TF("invn_bc", [128, 512], F32)
                    dbc, Bdbc = TF("d_bc", [128, 512], F32)
                    for o in range(2):
                        A.op(DVE, lambda e, o=o: e.tensor_copy(out=nrm[:, o, :], in_=ps[0:1, 3 + o, 0:256]), reads=[Bps[3 + o]], writes=[Bnrm])
                        A.op(DVE, lambda e, o=o: e.tensor_tensor(out=nrm[:, o, :], in0=nrm[:, o, :], in1=ps[0:1, 3 + o, 256:512], op=ALU.add),
                             reads=[Bps[3 + o], Bnrm], writes=[Bnrm])
                    A.op(DVE, lambda e: e.reciprocal(out=nrm[:], in_=nrm[:]), reads=[Bnrm], writes=[Bnrm])
                    isl = 0 if L == S else 1
                    A.dma(SP, lambda e: e.dma_start(out=invn_d[isl:isl + 1, :], in_=nrm[:].rearrange("p a b -> p (a b)")),
                          reads=[Bnrm], writes=[Binvn])
                    A.dma(SP, lambda e: e.dma_start(out=invn[:], in_=invn_d[isl].partition_broadcast(128)), reads=[Binvn], writes=[Binv])
                    A.dma(SP, lambda e: e.dma_start(out=dbc[:], in_=I["hy_d"][l].rearrange("a b -> (a b)").partition_broadcast(128)),
                          writes=[Bdbc])
                    for ft in range(nt):
                        sbk = 4 + (ft % 2) * 2
                        for cs, (src, Bsrc) in enumerate(((Ptm, BP), (Qtm, BQ))):
                            sl, Bsl = load_half(fwd_d, ft, cs)
                            bank = sbk + cs
                            for st_ in range(nt):
                                A.op(PE, lambda e, cs=cs, st_=st_, sl=sl, src=src, bank=bank: e.matmul(
                                    ps[:, bank, :], lhsT=sl[:, st_, :], rhs=src[:, st_, :], start=(st_ == 0), stop=(st_ == nt - 1)),
                                    reads=[Bsl, Bsrc], writes=[Bps[bank]])
                        A.op(DVE, lambda e, ft=ft, sbk=sbk: e.tensor_tensor(out=Ksp[:, ft, 1, :], in0=ps[:, sbk + 1, :], in1=invn[:], op=ALU.mult),
                             reads=[Bps[sbk + 1], Binv], writes=[BK])
                        krt, Bkrt = hd_r[ft % 2]
                        krv = krt[:].rearrange("p a b c -> p (a b c)")[:, 0:512]
                        A.op(DVE, lambda e, krv=krv, sbk=sbk: e.tensor_tensor(out=krv, in0=ps[:, sbk, :], in1=invn[:], op=ALU.mult),
                             reads=[Bps[sbk], Binv], writes=[Bkrt])
                        A.op(POOL, lambda e, krv=krv, ft=ft: e.tensor_tensor(out=Ksp[:, ft, 0, :], in0=krv, in1=dbc[:], op=ALU.add),
                             reads=[Bkrt, Bdbc], writes=[BK])
                    A.barrier()

                tm3, Btm3 = T("tm3", [128, nt, 768], BF16)
                with ExitStack() as stks:
                    def TS(name, shape, dt):
                        return stks.enter_context(_sbuf_unique(name, shape, dt)), Buf(name)
                    cw, Bcw = TS("cw", [128, 18], F32)
                    cb, Bcb = TS("cb", [128, 6], F32)
                    load_cols(TS, cw[:], Bcw, I["hy_conv_w"][l].rearrange("a b -> (a b)"), 18)
                    load_cols(TS, cb[:], Bcb, I["hy_conv_b"][l], 6)
                    W = min(L, 1024)
                    zin_r = [TS(f"zin{i}", [128, W + 2], F32) for i in range(2)]
                    zt_r = [TS(f"zt{i}", [128, W], F32) for i in range(2)]
                    zc_r = [TS(f"zc{i}", [128, W], BF16) for i in range(2)]
                    ctr = 0
                    for hc in range(6):
                        for b0 in range(0, L, W):
                            zin, Bzin = zin_r[ctr % 2]
                            zt, Bzt = zt_r[ctr % 2]
                            zc, Bzc = zc_r[ctr % 2]
                            ctr += 1
                            lo = b0 - 1
                            hi = b0 + W + 1
                            clo = max(lo, 0)
                            chi = min(hi, L)
                            if lo < 0:
                                A.op(DVE, lambda e, zin=zin: e.memset(zin[:, 0:1], 0.0), writes=[Bzin])
                            if hi > L:
                                A.op(DVE, lambda e, zin=zin: e.memset(zin[:, W + 1:W + 2], 0.0), writes=[Bzin])
                            A.dma(SP, lambda e, zin=zin, hc=hc, clo=clo, chi=chi, lo=lo: e.dma_start(
                                out=zin[:, clo - lo:chi - lo], in_=hyraw[hc, :, seq_t0 + clo:seq_t0 + chi]),
                                reads=[Bhyraw], writes=[Bzin])
                            A.op(DVE, lambda e, zin=zin, zt=zt, hc=hc: e.tensor_scalar(
                                out=zt[:], in0=zin[:, 1:W + 1], scalar1=cw[:, 6 + hc:7 + hc], scalar2=cb[:, hc:hc + 1],
                                op0=ALU.mult, op1=ALU.add), reads=[Bzin, Bcw, Bcb], writes=[Bzt])
                            A.op(DVE, lambda e, zin=zin, zt=zt, hc=hc: e.scalar_tensor_tensor(
                                out=zt[:], in0=zin[:, 0:W], scalar=cw[:, hc:hc + 1], in1=zt[:], op0=ALU.mult, op1=ALU.add),
                                reads=[Bzin, Bcw, Bzt], writes=[Bzt])
                            A.op(DVE, lambda e, zin=zin, zt=zt, zc=zc, hc=hc: e.scalar_tensor_tensor(
                                out=zc[:], in0=zin[:, 2:W + 2], scalar=cw[:, 12 + hc:13 + hc], in1=zt[:], op0=ALU.mult, op1=ALU.add),
                                reads=[Bzin, Bcw, Bzt], writes=[Bzc])
                            for t4 in range(0, W // 128, 4):
                                n4 = min(4, W // 128 - t4)
                                bank = (t4 // 4) % 2
                                for q in range(n4):
                                    A.op(PE, lambda e, zc=zc, t4=t4, q=q, bank=bank: e.transpose(
                                        out=psb(bank)[:, q * 128:(q + 1) * 128], in_=zc[:, (t4 + q) * 128:(t4 + q + 1) * 128],
                                        identity=identb[:]), reads=[Bzc, Bidb], writes=[Bps[bank]])
                                tile0 = b0 // 128 + t4
                                A.op(ACT, lambda e, tile0=tile0, n4=n4, bank=bank, hc=hc: e.copy(
                                    out=tm3[:, tile0:tile0 + n4, hc * 128:(hc + 1) * 128],
                                    in_=psb(bank)[:, 0:n4 * 128].rearrange("p (a c) -> p a c", c=128)),
                                    reads=[Bps[bank]], writes=[Btm3])
                    A.barrier()

                with ExitStack() as stkc:
                    def TC(name, shape, dt):
                        return stkc.enter_context(_sbuf_unique(name, shape, dt)), Buf(name)
                    Ysb, BY = TC("Ysb", [128, nt, 2, 256], BF16)
                    z1, Bz1 = TC("z1", [128, nt, 256], BF16)
                    tA = [TC(f"tA{i}", [128, 256], F32) for i in range(4)]
                    yo_r = [TC(f"yo{i}", [128, 4, 256], BF16) for i in range(2)]
                    cy_r = [TC(f"cy{i}", [128, 2, 512], BF16) for i in range(2)]
                    for o in range(2):
                        for ft in range(nt):
                            fb0 = (ft % 2) * 2
                            for cs in range(2):
                                sl, Bsl = load_half(fwd_d, ft, cs)
                                bank = fb0 + cs
                                for st_ in range(nt):
                                    rhs = tm3[:, st_, 0:256] if o == 0 else z1[:, st_, :]
                                    A.op(PE, lambda e, cs=cs, st_=st_, sl=sl, rhs=rhs, bank=bank: e.matmul(
                                        ps[:, bank, 0:256], lhsT=sl[:, st_, :], rhs=rhs, start=(st_ == 0), stop=(st_ == nt - 1)),
                                        reads=[Bsl, Btm3 if o == 0 else Bz1], writes=[Bps[bank]])
                            Kr = Ksp[:, ft, 0, o * 256:(o + 1) * 256]
                            Ki = Ksp[:, ft, 1, o * 256:(o + 1) * 256]
                            for i4, (zb, kk) in enumerate(((fb0, Kr), (fb0 + 1, Ki), (fb0, Ki), (fb0 + 1, Kr))):
                                A.op(DVE, lambda e, i4=i4, zb=zb, kk=kk: e.tensor_tensor(out=tA[i4][0][:], in0=ps[:, zb, 0:256], in1=kk, op=ALU.mult),
                                     reads=[Bps[zb], BK], writes=[tA[i4][1]])
                            A.op(POOL, lambda e, ft=ft: e.tensor_tensor(out=Ysb[:, ft, 0, :], in0=tA[0][0][:], in1=tA[1][0][:], op=ALU.subtract),
                                 reads=[tA[0][1], tA[1][1]], writes=[BY])
                            A.op(POOL, lambda e, ft=ft: e.tensor_tensor(out=Ysb[:, ft, 1, :], in0=tA[2][0][:], in1=tA[3][0][:], op=ALU.add),
                                 reads=[tA[2][1], tA[3][1]], writes=[BY])
                        for tt in range(nt):
                            bank = 4 + tt % 2
                            n_mm = 2 * nt
                            i_mm = 0
                            for cs in range(2):
                                sl, Bsl = load_half(inv_d, tt, cs)
                                for ft in range(nt):
                                    A.op(PE, lambda e, cs=cs, ft=ft, sl=sl, bank=bank, i_mm=i_mm: e.matmul(
                                        ps[:, bank, 0:256], lhsT=sl[:, ft, :], rhs=Ysb[:, ft, cs, :],
                                        start=(i_mm == 0), stop=(i_mm == n_mm - 1)), reads=[Bsl, BY], writes=[Bps[bank]])
                                    i_mm += 1
                            if o == 0:
                                A.op(DVE, lambda e, tt=tt, bank=bank: e.tensor_tensor(
                                    out=z1[:, tt, :], in0=ps[:, bank, 0:256], in1=tm3[:, tt, 256:512], op=ALU.mult),
                                    reads=[Bps[bank], Btm3], writes=[Bz1])
                            else:
                                yo, Byo = yo_r[(tt // 4) % 2]
                                A.op(DVE, lambda e, tt=tt, bank=bank, yo=yo: e.tensor_tensor(
                                    out=yo[:, tt % 4, :], in0=ps[:, bank, 0:256], in1=tm3[:, tt, 512:768], op=ALU.mult),
                                    reads=[Bps[bank], Btm3], writes=[Byo])
                                if tt % 4 == 3 or tt == nt - 1:
                                    n4 = tt % 4 + 1
                                    cy, Bcy = cy_r[(tt // 4) % 2]
                                    for q in range(n4):
                                        for a in range(2):
                                            A.op(PE, lambda e, q=q, a=a, yo=yo: e.transpose(
                                                out=psb(6 + a)[:, q * 128:(q + 1) * 128], in_=yo[:, q, a * 128:(a + 1) * 128],
                                                identity=identb[:]), reads=[Byo, Bidb], writes=[Bps[6 + a]])
                                    for a in range(2):
                                        A.op(ACT, lambda e, a=a, cy=cy, n4=n4: e.copy(out=cy[:, a, 0:n4 * 128], in_=psb(6 + a)[:, 0:n4 * 128]),
                                             reads=[Bps[6 + a]], writes=[Bcy])
                                    tok0 = seq_t0 + (tt - n4 + 1) * 128
                                    bi = min(tok0 // 512, 8)
                                    A.dma(SP, lambda e, cy=cy, tok0=tok0, n4=n4: e.dma_start(
                                        out=catT[4:6, :, tok0:tok0 + n4 * 128].rearrange("k p t -> p k t"), in_=cy[:, :, 0:n4 * 128]),
                                        reads=[Bcy], writes=[Bcat[bi]])
                    A.barrier()

        hyena(0, S, I["k_fwd"], I["k_inv"], I["k_feats"], I["k_decay"])
        if not last:
            hyena(S, CTX, I["k_fwdc"], I["k_invc"], I["k_featsc"], I["k_decayc"])
        if dbg:
            A.dma(SP, lambda e: e.dma_start(out=DBG["cat"], in_=catT), reads=Bcat, writes=[Bdbg])
            A.dma(SP, lambda e: e.dma_start(out=DBG["hy"], in_=hyraw), reads=[Bhyraw], writes=[Bdbg])
            A.barrier()

        def load_bc(T, name, src_vec):
            t, Bt = T(name, [128, D], F32)
            A.dma(SP, lambda e: e.dma_start(out=t[:], in_=src_vec.partition_broadcast(128)), reads=[Bgate], writes=[Bt])
            return t, Bt

        def deepnorm_tile(T_, stt_, psbanks, gate_bc, Bgate_bc, xin, Bxin, g_bc, Bg, b_bc, Bb, yt, Byt, xo, Bxo):
            b0 = psbanks
            A.op(DVE, lambda e: e.tensor_tensor(out=yt[:].rearrange("p (a c) -> p a c", a=2), in0=ps[:, b0:b0 + 2, :],
                                                in1=gate_bc[:].rearrange("p (a c) -> p a c", a=2), op=ALU.mult),
                 reads=[Bps[b0], Bps[b0 + 1], Bgate_bc], writes=[Byt])
            A.op(POOL, lambda e: e.tensor_tensor(out=yt[:], in0=yt[:], in1=xin[:], op=ALU.add), reads=[Byt, Bxin], writes=[Byt])
            ln_compute(stt_, yt, Byt, LN_EPS / (ALPHA * ALPHA))
            A.op(DVE, lambda e: e.tensor_scalar(out=yt[:], in0=yt[:], scalar1=stt_[2][:, 0:1], scalar2=stt_[4][:],
                                                op0=ALU.subtract, op1=ALU.mult), reads=[Byt, stt_[3], stt_[5]], writes=[Byt])
            A.op(POOL, lambda e: e.tensor_tensor(out=yt[:], in0=yt[:], in1=g_bc[:], op=ALU.mult), reads=[Byt, Bg], writes=[Byt])
            A.op(POOL, lambda e: e.tensor_tensor(out=xo[:], in0=yt[:], in1=b_bc[:], op=ALU.add), reads=[Byt, Bb], writes=[Bxo])

        with ExitStack() as stk:
            def T(name, shape, dt):
                return stk.enter_context(_sbuf_unique(name, shape, dt)), Buf(name)
            wo, Bwo = T("wo", [128, 8, D], BF16)
            A.dma(POOL, lambda e: e.dma_start(out=wo[:], in_=I["w_out"][l].rearrange("(k p) n -> p k n", p=128)), writes=[Bwo])
            g1l = load_bc(T, "g1l", gate_d[l, 0])
            g1c = load_bc(T, "g1c", gate_d[l, 1])
            lg = load_bc(T, "lg", I["ln1_g"][l])
            lb = load_bc(T, "lb", I["ln1_b"][l])
            if moe:
                rw, Brw = T("rw", [128, 8, NEXP], BF16)
                A.dma(POOL, lambda e: e.dma_start(out=rw[:], in_=I["router_w"][jf].rearrange("(k p) n -> p k n", p=128)), writes=[Brw])
            cat_r = [T(f"catb{i}", [128, 8, 512], BF16) for i in range(2)]
            xin_r = [T(f"xin{i}", [128, D], F32) for i in range(4)]
            yt_r = [T(f"yt{i}", [128, D], F32) for i in range(4)]
            x1_r = [T(f"x1_{i}", [128, D], F32) for i in range(4)]
            nmr_r = [T(f"nmr5_{i}", [128, 2], F32) for i in range(4)]
            uh_r = [T(f"uh5_{i}", [128, D], BF16) for i in range(8)]
            uT_r = [T(f"uT5_{i}", [128, 8, 512], BF16) for i in range(2)]
            stt = [ln_stats(T, None, None, None, f"p5{i}") for i in range(8)]
            p5_g0 = []
            _g = 0
            for (_t0, _n) in BLOCKS:
                p5_g0.append(_g)
                _g += _n

            def p5_loads(bj):
                t0_, nti_ = BLOCKS[bj]
                n_ = nti_ * 128
                cbj, Bcbj = cat_r[bj % 2]
                A.dma(SP, lambda e: e.dma_start(
                    out=cbj[:, :, 0:n_], in_=catT[:, :, t0_:t0_ + n_].rearrange("k p t -> p k t")), reads=[Bcat[bj]], writes=[Bcbj])
                for tj in range(nti_):
                    xin_, Bxin_ = xin_r[(p5_g0[bj] + tj) % 4]
                    tok_ = t0_ + tj * 128
                    A.dma(SP, lambda e, xin_=xin_, tok_=tok_: e.dma_start(out=xin_[:], in_=xsrc(tok_, 128)),
                          reads=[Bxres[tok_ // 128]], writes=[Bxin_])

            tile_ctr = 0
            for bi, (t0, nti) in enumerate(BLOCKS[:nblk]):
                is_ctx = t0 >= S
                jm = 1 if is_ctx else 0
                n = nti * 128
                cb_, Bcb_ = cat_r[bi % 2]
                if bi == 0:
                    p5_loads(0)
                uT, BuT = uT_r[bi % 2]
                gt = g1c if is_ctx else g1l
                for ti in range(nti):
                    g = tile_ctr + ti
                    tok = t0 + ti * 128
                    xin, Bxin = xin_r[g % 4]
                    yt, Byt = yt_r[g % 4]
                    pb = 4 + 2 * (ti % 2)
                    for nb in range(2):
                        for k in range(8):
                            A.op(PE, lambda e, nb=nb, k=k, cb_=cb_, ti=ti, pb=pb: e.matmul(
                                ps[:, pb + nb, :], lhsT=cb_[:, k, ti * 128:(ti + 1) * 128], rhs=wo[:, k, nb * 512:(nb + 1) * 512],
                                start=(k == 0), stop=(k == 7)), reads=[Bcb_, Bwo], writes=[Bps[pb + nb]])
                    A.op(DVE, lambda e, yt=yt, pb=pb, gt=gt: e.tensor_tensor(
                        out=yt[:].rearrange("p (a c) -> p a c", a=2), in0=ps[:, pb:pb + 2, :],
                        in1=gt[0][:].rearrange("p (a c) -> p a c", a=2), op=ALU.mult),
                        reads=[Bps[pb], Bps[pb + 1], gt[1]], writes=[Byt])
                    A.op(POOL, lambda e, yt=yt, xin=xin: e.tensor_tensor(out=yt[:], in0=yt[:], in1=xin[:], op=ALU.add),
                         reads=[Byt, Bxin], writes=[Byt])
                if bi + 1 < nblk:
                    p5_loads(bi + 1)
                for ti in range(nti):
                    g = tile_ctr + ti
                    yt, Byt = yt_r[g % 4]
                    s_ = stt[g % 4]
                    nmr, Bnmr = nmr_r[g % 4]
                    ln_compute(s_, yt, Byt, LN_EPS / (ALPHA * ALPHA))
                    A.op(DVE, lambda e, nmr=nmr, s_=s_: e.tensor_scalar(
                        out=nmr[:, 0:1], in0=s_[2][:, 0:1], scalar1=s_[4][:], scalar2=-1.0, op0=ALU.mult, op1=ALU.mult),
                        reads=[s_[3], s_[5]], writes=[Bnmr])
                    A.op(ACT, lambda e, yt=yt, s_=s_, nmr=nmr: e.activation(
                        out=yt[:], in_=yt[:], func=AF.Identity, scale=s_[4][:], bias=nmr[:, 0:1]),
                        reads=[Byt, s_[5], Bnmr], writes=[Byt])
                for ti in range(nti):
                    g = tile_ctr + ti
                    tok = t0 + ti * 128
                    yt, Byt = yt_r[g % 4]
                    x1, Bx1 = x1_r[g % 4]
                    A.op(DVE, lambda e, yt=yt: e.tensor_tensor(out=yt[:], in0=yt[:], in1=lg[0][:], op=ALU.mult), reads=[Byt, lg[1]], writes=[Byt])
                    A.op(POOL, lambda e, yt=yt, x1=x1: e.tensor_tensor(out=x1[:], in0=yt[:], in1=lb[0][:], op=ALU.add), reads=[Byt, lb[1]], writes=[Bx1])
                    A.dma(SP, lambda e, x1=x1, tok=tok: e.dma_start(out=xres[tok:tok + 128, :], in_=x1[:]), reads=[Bx1], writes=[Bxres[tok // 128]])
                    if dbg:
                        A.dma(SP, lambda e, x1=x1, tok=tok: e.dma_start(out=DBG["x1"][tok:tok + 128, :], in_=x1[:]), reads=[Bx1], writes=[Bdbg])
                uhs = []
                for ti in range(nti):
                    g = tile_ctr + ti
                    x1, Bx1 = x1_r[g % 4]
                    uh, Buh = uh_r[g % 8]
                    s_ = stt[4 + g % 4]
                    nmr, Bnmr = nmr_r[g % 4]
                    ln_compute(s_, x1, Bx1, ADA_EPS)
                    A.op(DVE, lambda e, nmr=nmr, s_=s_: e.tensor_scalar(
                        out=nmr[:, 1:2], in0=s_[2][:, 0:1], scalar1=s_[4][:], scalar2=-1.0, op0=ALU.mult, op1=ALU.mult),
                        reads=[s_[3], s_[5]], writes=[Bnmr])
                    A.op(ACT, lambda e, x1=x1, uh=uh, s_=s_, nmr=nmr: e.activation(
                        out=uh[:], in_=x1[:], func=AF.Identity, scale=s_[4][:], bias=nmr[:, 1:2]),
                        reads=[Bx1, s_[5], Bnmr], writes=[Buh])
                    uhs.append((uh, Buh))
                modulate_block(uhs, nti, l, jm, 24, uT, BuT)
                A.dma(SP, lambda e, uT=uT, t0=t0, n=n: e.dma_start(
                    out=uT_d[:, :, t0:t0 + n].rearrange("k p t -> p k t"), in_=uT[:, :, 0:n]), reads=[BuT], writes=[BuTd[bi]])
                tile_ctr += nti
            A.barrier()

        with ExitStack() as stk:
            def T(name, shape, dt):
                return stk.enter_context(_sbuf_unique(name, shape, dt)), Buf(name)
            NQ = 7
            if moe:
                pieces = [(ex_, q * 7, 7) for ex_ in range(NEXP) for q in range(4)]
                wsrc = lambda ex_: (I["moe_w_gate"][jf, ex_], I["moe_w_up"][jf, ex_], I["moe_w_down"][jf, ex_])
            else:
                pieces = [(0, 0, 6), (0, 6, 6), (0, 12, 5), (0, 17, 5)]
                wsrc = lambda ex_: (I["ffn_w_gate"][jf], I["ffn_w_up"][jf], I["ffn_w_down"][jf])
            comb, Bcomb = T("comb", [128, NT, NEXP], F32)
            if moe:
                with ExitStack() as stkr:
                    def TR(name, shape, dt):
                        return stkr.enter_context(_sbuf_unique(name, shape, dt)), Buf(name)
                    rw, Brw = TR("rw", [128, 8, NEXP], BF16)
                    A.dma(POOL, lambda e: e.dma_start(out=rw[:], in_=I["router_w"][jf].rearrange("(k p) n -> p k n", p=128)), writes=[Brw])
                    uT_r = [TR(f"uTr{i}", [128, 8, 512], BF16) for i in range(2)]
                    lg_r = [TR(f"lgt{i}", [128, 8], F32) for i in range(2)]
                    m8_r = [TR(f"m8{i}", [128, 8], F32) for i in range(2)]
                    gg_r = [TR(f"gg{i}", [128, 4], F32) for i in range(2)]
                    eq_r = [TR(f"eq{i}", [128, 8], F32) for i in range(2)]
                    tile_ctr = 0
                    for bi, (t0, nti) in enumerate(BLOCKS[:nblk]):
                        n = nti * 128
                        uT, BuT = uT_r[bi % 2]
                        A.dma(SP, lambda e, uT=uT, t0=t0, n=n: e.dma_start(
                            out=uT[:, :, 0:n], in_=uT_d[:, :, t0:t0 + n].rearrange("k p t -> p k t")), reads=[BuTd[bi]], writes=[BuT])
                        for ti in range(nti):
                            g = tile_ctr + ti
                            bank = g % 2
                            for k in range(8):
                                A.op(PE, lambda e, k=k, uT=uT, ti=ti, bank=bank: e.matmul(
                                    ps[:, bank, 0:NEXP], lhsT=uT[:, k, ti * 128:(ti + 1) * 128], rhs=rw[:, k, :],
                                    start=(k == 0), stop=(k == 7)), reads=[BuT, Brw], writes=[Bps[bank]])
                            lgt, Blg = lg_r[g % 2]
                            m8, Bm8 = m8_r[g % 2]
                            gg, Bgg = gg_r[g % 2]
                            eq, Beq = eq_r[g % 2]
                            A.op(DVE, lambda e, lgt=lgt, bank=bank: e.tensor_copy(out=lgt[:], in_=ps[:, bank, 0:NEXP]), reads=[Bps[bank]], writes=[Blg])
                            A.op(DVE, lambda e, lgt=lgt, m8=m8: e.max(out=m8[:], in_=lgt[:]), reads=[Blg], writes=[Bm8])
                            A.op(DVE, lambda e, m8=m8, gg=gg: e.tensor_tensor(out=gg[:, 0:1], in0=m8[:, 1:2], in1=m8[:, 0:1], op=ALU.subtract),
                                 reads=[Bm8], writes=[Bgg])
                            A.op(ACT, lambda e, gg=gg: e.activation(out=gg[:, 1:2], in_=gg[:, 0:1], func=AF.Exp), reads=[Bgg], writes=[Bgg])
                            A.op(DVE, lambda e, gg=gg: e.tensor_scalar(out=gg[:, 2:3], in0=gg[:, 1:2], scalar1=1.0, scalar2=None, op0=ALU.add),
                                 reads=[Bgg], writes=[Bgg])
                            A.op(DVE, lambda e, gg=gg: e.reciprocal(out=gg[:, 2:3], in_=gg[:, 2:3]), reads=[Bgg], writes=[Bgg])
                            A.op(DVE, lambda e, gg=gg: e.tensor_tensor(out=gg[:, 3:4], in0=gg[:, 1:2], in1=gg[:, 2:3], op=ALU.mult),
                                 reads=[Bgg], writes=[Bgg])
                            A.op(DVE, lambda e, lgt=lgt, m8=m8, gg=gg, eq=eq: e.tensor_scalar(
                                out=eq[:], in0=lgt[:], scalar1=m8[:, 0:1], scalar2=gg[:, 2:3], op0=ALU.is_equal, op1=ALU.mult),
                                reads=[Blg, Bm8, Bgg], writes=[Beq])
                            tg = t0 // 128 + ti
                            A.op(DVE, lambda e, lgt=lgt, m8=m8, gg=gg, tg=tg: e.tensor_scalar(
                                out=comb[:, tg, :], in0=lgt[:], scalar1=m8[:, 1:2], scalar2=gg[:, 3:4], op0=ALU.is_equal, op1=ALU.mult),
                                reads=[Blg, Bm8, Bgg], writes=[Bcomb])
                            A.op(DVE, lambda e, eq=eq, tg=tg: e.tensor_tensor(out=comb[:, tg, :], in0=comb[:, tg, :], in1=eq[:], op=ALU.add),
                                 reads=[Beq, Bcomb], writes=[Bcomb])
                        tile_ctr += nti
                    A.barrier()
            if last:
                sblocks = [[0, 1, 2], [3, 4, 5], [6, 7]]
            else:
                sblocks = [[0, 1, 2], [3, 4, 5], [6, 7, 8]]
            acc, _ = T("acc", [128, 12, D], F32)
            Bacc = [Buf(f"acc{i}") for i in range(12)]
            w_r = [(T(f"wgq{i}", [128, 8, NQ * 128], BF16), T(f"wuq{i}", [128, 8, NQ * 128], BF16), T(f"wdq{i}", [128, NQ, D], BF16)) for i in range(2)]
            uT_r = [T(f"uT7_{i}", [128, 8, 512], BF16) for i in range(2)]
            aT_r = [T(f"aT7_{i}", [128, NQ, 512], BF16) for i in range(2)]
            sg_r = [T(f"sg7_{i}", [128, 512], BF16) for i in range(2)]
            g2l = load_bc(T, "g2l7", gate_d[l, 2])
            g2c = load_bc(T, "g2c7", gate_d[l, 3])
            lg2 = load_bc(T, "lg27", I["ln2_g"][l])
            lb2 = load_bc(T, "lb27", I["ln2_b"][l])
            xin_r = [T(f"xin7_{i}", [128, D], F32) for i in range(2)]
            yt_r = [T(f"yt7_{i}", [128, D], F32) for i in range(2)]
            xo_r = [T(f"xo7_{i}", [128, D], F32) for i in range(2)]
            nmr_r = [T(f"nmr7_{i}", [128, 1], F32) for i in range(2)]
            stt = [ln_stats(T, None, None, None, f"p7{i}") for i in range(2)]

            def finalize_steps(sbl):
                tiles = []
                at = 0
                for bi in sbl:
                    t0, nti = BLOCKS[bi]
                    for ti in range(nti):
                        tiles.append((at, t0 + ti * 128, g2c if t0 >= S else g2l))
                        at += 1
                steps = []

                def s1(at, tok, g2):
                    xin, Bxin = xin_r[at % 2]
                    yt, Byt = yt_r[at % 2]
                    A.dma(SP, lambda e: e.dma_start(out=xin[:], in_=xres[tok:tok + 128, :]), reads=[Bxres[tok // 128]], writes=[Bxin])
                    A.op(DVE, lambda e: e.tensor_tensor(out=yt[:], in0=acc[:, at, :], in1=g2[0][:], op=ALU.mult),
                         reads=[Bacc[at], g2[1]], writes=[Byt])
                    A.op(POOL, lambda e: e.tensor_tensor(out=yt[:], in0=yt[:], in1=xin[:], op=ALU.add), reads=[Byt, Bxin], writes=[Byt])

                def s2(at, tok, g2):
                    yt, Byt = yt_r[at % 2]
                    stt_ = stt[at % 2]
                    nmr, Bnmr = nmr_r[at % 2]
                    ln_compute(stt_, yt, Byt, LN_EPS / (ALPHA * ALPHA))
                    A.op(DVE, lambda e: e.tensor_scalar(out=nmr[:], in0=stt_[2][:, 0:1], scalar1=stt_[4][:], scalar2=-1.0,
                                                        op0=ALU.mult, op1=ALU.mult), reads=[stt_[3], stt_[5]], writes=[Bnmr])
                    A.op(ACT, lambda e: e.activation(out=yt[:], in_=yt[:], func=AF.Identity, scale=stt_[4][:], bias=nmr[:]),
                         reads=[Byt, stt_[5], Bnmr], writes=[Byt])

                def s3(at, tok, g2):
                    yt, Byt = yt_r[at % 2]
                    xo, Bxo = xo_r[at % 2]
                    A.op(DVE, lambda e: e.tensor_tensor(out=yt[:], in0=yt[:], in1=lg2[0][:], op=ALU.mult), reads=[Byt, lg2[1]], writes=[Byt])
                    A.op(POOL, lambda e: e.tensor_tensor(out=xo[:], in0=yt[:], in1=lb2[0][:], op=ALU.add), reads=[Byt, lb2[1]], writes=[Bxo])
                    if last:
                        A.dma(SP, lambda e: e.dma_start(out=out_d[tok:tok + 128, :], in_=xo[:]), reads=[Bxo], writes=[Bout])
                    else:
                        A.dma(SP, lambda e: e.dma_start(out=xres[tok:tok + 128, :], in_=xo[:]), reads=[Bxo], writes=[Bxres[tok // 128]])
                    if dbg:
                        A.dma(SP, lambda e: e.dma_start(out=DBG["x2"][tok:tok + 128, :], in_=xo[:]), reads=[Bxo], writes=[Bdbg])

                for p0 in range(0, len(tiles), 2):
                    pair = tiles[p0:p0 + 2]
                    for stage in (s1, s2, s3):
                        for tl in pair:
                            steps.append(lambda stage=stage, tl=tl: stage(*tl))
                    while len(steps) % 6:
                        steps.append(lambda: None)
                return steps

            pending = []
            n_drained = [0]

            def drain(n):
                while n > 0 and pending:
                    pending.pop(0)()
                    n_drained[0] += 1
                    n -= 1

            wctr = 0
            bctr = 0
            for sbl in sblocks:
                for pi_, (ex_, ch0, nch) in enumerate(pieces):
                    (wgq, Bwgq), (wuq, Bwuq), (wdq, Bwdq) = w_r[wctr % 2]
                    wctr += 1
                    c0 = ch0 * 128
                    c1 = c0 + nch * 128
                    sg_, su_, sd_ = wsrc(ex_)
                    A.dma(POOL, lambda e, wgq=wgq, sg_=sg_, c0=c0, c1=c1, nch=nch: e.dma_start(
                        out=wgq[:, :, 0:nch * 128], in_=sg_[:, c0:c1].rearrange("(k p) n -> p k n", p=128)), writes=[Bwgq])
                    A.dma(POOL, lambda e, wuq=wuq, su_=su_, c0=c0, c1=c1, nch=nch: e.dma_start(
                        out=wuq[:, :, 0:nch * 128], in_=su_[:, c0:c1].rearrange("(k p) n -> p k n", p=128)), writes=[Bwuq])
                    A.dma(POOL, lambda e, wdq=wdq, sd_=sd_, c0=c0, c1=c1, nch=nch: e.dma_start(
                        out=wdq[:, 0:nch, :], in_=sd_[c0:c1, :].rearrange("(k p) n -> p k n", p=128)), writes=[Bwdq])
                    firstw = (pi_ == 0)
                    at = 0
                    for bi in sbl:
                        t0, nti = BLOCKS[bi]
                        n = nti * 128
                        uT, BuT = uT_r[bctr % 2]
                        aT, BaT = aT_r[bctr % 2]
                        bctr += 1
                        A.dma(SP, lambda e, uT=uT, t0=t0, n=n: e.dma_start(
                            out=uT[:, :, 0:n], in_=uT_d[:, :, t0:t0 + n].rearrange("k p t -> p k t")), reads=[BuTd[bi]], writes=[BuT])
                        for fc in range(nch):
                            gb, ub = (fc % 2) * 2, (fc % 2) * 2 + 1
                            for (wt_, Bwt_, bank) in ((wgq, Bwgq, gb), (wuq, Bwuq, ub)):
                                for k in range(8):
                                    A.op(PE, lambda e, wt_=wt_, k=k, fc=fc, bank=bank, uT=uT, n=n: e.matmul(
                                        ps[:, bank, 0:n], lhsT=wt_[:, k, fc * 128:(fc + 1) * 128], rhs=uT[:, k, 0:n],
                                        start=(k == 0), stop=(k == 7)), reads=[Bwt_, BuT], writes=[Bps[bank]])
                            sg, Bsg = sg_r[fc % 2]
                            A.op(ACT, lambda e, sg=sg, gb=gb, n=n: e.activation(out=sg[:, 0:n], in_=ps[:, gb, 0:n], func=AF.Silu),
                                 reads=[Bps[gb]], writes=[Bsg])
                            A.op(DVE, lambda e, sg=sg, ub=ub, aT=aT, fc=fc, n=n: e.tensor_tensor(
                                out=aT[:, fc, 0:n], in0=ps[:, ub, 0:n], in1=sg[:, 0:n], op=ALU.mult),
                                reads=[Bps[ub], Bsg], writes=[BaT])
                            drain(2)
                        if firstw:
                            drain(6 * ((at + nti - 1) // 2 + 1) - n_drained[0])
                        for ti in range(nti):
                            gtile = t0 // 128 + ti
                            pb = 4 + 2 * (at % 2)
                            for nb in range(2):
                                for fc in range(nch):
                                    A.op(PE, lambda e, nb=nb, fc=fc, aT=aT, ti=ti, pb=pb, wdq=wdq, nch=nch: e.matmul(
                                        ps[:, pb + nb, :], lhsT=aT[:, fc, ti * 128:(ti + 1) * 128], rhs=wdq[:, fc, nb * 512:(nb + 1) * 512],
                                        start=(fc == 0), stop=(fc == nch - 1)), reads=[BaT, Bwdq], writes=[Bps[pb + nb]])
                            av = acc[:, at, :].rearrange("p (a c) -> p a c", a=2)
                            if moe:
                                cw_ = comb[:, gtile, ex_:ex_ + 1]
                                if firstw:
                                    A.op(DVE, lambda e, av=av, pb=pb, cw_=cw_: e.tensor_scalar(
                                        out=av, in0=ps[:, pb:pb + 2, :], scalar1=cw_, scalar2=None, op0=ALU.mult),
                                        reads=[Bps[pb], Bps[pb + 1], Bcomb], writes=[Bacc[at]])
                                else:
                                    A.op(DVE, lambda e, av=av, pb=pb, cw_=cw_: e.scalar_tensor_tensor(
                                        out=av, in0=ps[:, pb:pb + 2, :], scalar=cw_, in1=av, op0=ALU.mult, op1=ALU.add),
                                        reads=[Bps[pb], Bps[pb + 1], Bcomb, Bacc[at]], writes=[Bacc[at]])
                            else:
                                if firstw:
                                    A.op(ACT, lambda e, av=av, pb=pb: e.copy(out=av, in_=ps[:, pb:pb + 2, :]),
                                         reads=[Bps[pb], Bps[pb + 1]], writes=[Bacc[at]])
                                else:
                                    A.op(DVE, lambda e, av=av, pb=pb: e.tensor_tensor(out=av, in0=ps[:, pb:pb + 2, :], in1=av, op=ALU.add),
                                         reads=[Bps[pb], Bps[pb + 1], Bacc[at]], writes=[Bacc[at]])
                            at += 1
                drain(len(pending))
                pending.extend(finalize_steps(sbl))
                n_drained[0] = 0
            drain(len(pending))
            A.barrier()


    for l in range(n_layers):
        layer(l)

    A.barrier()
    A.emit()
    glob.close()
    return nc


_NC_CACHE = {}


def _in_maps(inputs):
    cst = _consts()
    maps = []
    shared = {}
    for k in INPUT_SHAPES:
        if k in ("x", "c", "ctx"):
            continue
        shared[k] = np.ascontiguousarray(np.asarray(inputs[k], dtype=np.float32))
    for b in range(8):
        m = dict(shared)
        m["x"] = np.ascontiguousarray(np.asarray(inputs["x"][b], dtype=np.float32))
        m["c"] = np.ascontiguousarray(np.asarray(inputs["c"][b], dtype=np.float32))
        m["ctx"] = np.ascontiguousarray(np.asarray(inputs["ctx"][b], dtype=np.float32))
        m.update(cst)
        maps.append(m)
    return maps


def kernel(**inputs):
    if "nc" not in _NC_CACHE:
        _NC_CACHE["nc"] = build()
    nc = _NC_CACHE["nc"]
    res = run_bass_kernel_spmd(nc, _in_maps(inputs), core_ids=list(range(8)))
    return np.stack([np.asarray(r["out"], dtype=np.float32) for r in res.results], axis=0)
```
